# Optimizing a Trainium2 kernel written in Bass

```python
import jax, jax.numpy as jnp
from jax import lax
import numpy as np

D_MODEL = 1024
BATCH = 8
SEQ = 2048
DEPTH = 4

GRID_W = 64
CTX_LEN = 256

A_HEADS = 4
A_DK = 128
A_DV = 128
A_WIDTH = A_HEADS * A_DK
B_HEADS = 8
B_HD = 64
B_WIDTH = B_HEADS * B_HD
D_MIX = A_WIDTH + B_WIDTH

A_Q = 0
A_FF = A_Q + A_WIDTH
A_FB = A_FF + A_WIDTH
A_I = A_FB + A_WIDTH
A_G = A_I + A_WIDTH
B_Q = A_G + A_WIDTH
B_K = B_Q + B_WIDTH
B_V = B_K + B_WIDTH
D_IN = B_V + B_WIDTH

CHUNK = 64
NA_ROWS = 8
NA_COLS = 16
ROPE_BASE = 10000.0

N_EXPERTS = 16
N_GROUPS = 4
EXPERTS_PER_GROUP = N_EXPERTS // N_GROUPS
TOP_K = 2
D_EXPERT = 512

ALPHA = (2 * DEPTH) ** 0.25
BETA = (8 * DEPTH) ** -0.25
LN_EPS = 1e-5
RMS_EPS = 1e-6
F_FLOOR = 1e-6
MASK_VALUE = -1e30

kernel_name = 'hybrid_hgrn2_natten_grouped_moe_dit'

f32 = jnp.float32


def cols(u, start, width):
    return u[..., start:start + width]


def split_heads(t, n_heads):
    b, L, w = t.shape
    return t.reshape(b, L, n_heads, w // n_heads).transpose(0, 2, 1, 3)


def merge_heads(t):
    b, h, L, d = t.shape
    return t.transpose(0, 2, 1, 3).reshape(b, L, h * d)


def layer_norm(x, g, b):
    xf = x.astype(f32)
    mu = jnp.mean(xf, axis=-1, keepdims=True)
    var = jnp.mean(jnp.square(xf - mu), axis=-1, keepdims=True)
    return ((xf - mu) * lax.rsqrt(var + LN_EPS) * g + b).astype(x.dtype)


def forget_gate(z, lb):
    z = z.astype(f32)
    f = lb + (1.0 - lb) * jax.nn.sigmoid(z)
    log_f = jnp.log(jnp.maximum(f, F_FLOOR))
    k = (1.0 - lb) * jax.nn.sigmoid(-z)
    return split_heads(k, A_HEADS), split_heads(log_f, A_HEADS)


def hgrn2_chunk_scan(q, k, v, log_f, s0):
    b, h, L, _ = q.shape
    nc = L // CHUNK

    def to_chunks(t):
        return jnp.moveaxis(t.reshape(b, h, nc, CHUNK, t.shape[-1]), 2, 0)

    causal = jnp.tril(jnp.ones((CHUNK, CHUNK), dtype=bool))[:, :, None]

    def step(S, inp):
        qc, kc, vc, lfc = inp
        cum = jnp.cumsum(lfc, axis=2)
        o_inter = jnp.einsum('bhck,bhkv->bhcv', qc * jnp.exp(cum), S)
        diff = cum[:, :, :, None, :] - cum[:, :, None, :, :]
        decay = jnp.where(causal, jnp.exp(jnp.where(causal, diff, 0.0)), 0.0)
        scores = jnp.einsum('bhtk,bhsk,bhtsk->bhts', qc, kc, decay)
        o_intra = jnp.einsum('bhts,bhsv->bhtv', scores, vc)
        last = cum[:, :, -1:, :]
        k_dec = kc * jnp.exp(last - cum)
        S_new = jnp.exp(last[:, :, 0, :])[..., None] * S + jnp.einsum('bhck,bhcv->bhkv', k_dec, vc)
        return S_new, o_inter + o_intra

    xs = (to_chunks(q.astype(f32)), to_chunks(k), to_chunks(v.astype(f32)), to_chunks(log_f))
    S_fin, o = lax.scan(step, s0, xs)
    o = jnp.moveaxis(o, 0, 2).reshape(b, h, L, -1)
    return o, S_fin


def hgrn2_final_state(k, v, log_f):
    cum = jnp.cumsum(log_f, axis=2)
    k_dec = k * jnp.exp(cum[:, :, -1:] - cum)
    return jnp.einsum('bhlk,bhlv->bhkv', k_dec, v.astype(f32))


def hgrn2_readout(o, g, norm_g, dtype):
    of = o.astype(f32)
    of = of * lax.rsqrt(jnp.mean(jnp.square(of), axis=-1, keepdims=True) + RMS_EPS) * norm_g
    return (merge_heads(of) * jax.nn.silu(g.astype(f32))).astype(dtype)


def hgrn2_mixer(u_ctx, u_lat, lb_f, lb_b, norm_g, ctx_out):
    flip = lambda t: jnp.flip(t, axis=2)
    b = u_lat.shape[0]
    zeros = jnp.zeros((b, A_HEADS, A_DK, A_DV), f32)
    ckf, clff = forget_gate(cols(u_ctx, A_FF, A_WIDTH), lb_f)
    ckb, clfb = forget_gate(cols(u_ctx, A_FB, A_WIDTH), lb_b)
    cv = split_heads(cols(u_ctx, A_I, A_WIDTH), A_HEADS)
    if ctx_out:
        cq = split_heads(jax.nn.silu(cols(u_ctx, A_Q, A_WIDTH)), A_HEADS)
        oc_f, s_f = hgrn2_chunk_scan(cq, ckf, cv, clff, zeros)
        oc_b, s_b = hgrn2_chunk_scan(flip(cq), flip(ckb), flip(cv), flip(clfb), zeros)
        a_ctx = hgrn2_readout(oc_f + flip(oc_b), cols(u_ctx, A_G, A_WIDTH), norm_g, u_ctx.dtype)
    else:
        s_f = hgrn2_final_state(ckf, cv, clff)
        s_b = hgrn2_final_state(flip(ckb), flip(cv), flip(clfb))
        a_ctx = None
    lq = split_heads(jax.nn.silu(cols(u_lat, A_Q, A_WIDTH)), A_HEADS)
    lkf, llff = forget_gate(cols(u_lat, A_FF, A_WIDTH), lb_f)
    lkb, llfb = forget_gate(cols(u_lat, A_FB, A_WIDTH), lb_b)
    lv = split_heads(cols(u_lat, A_I, A_WIDTH), A_HEADS)
    o_f, _ = hgrn2_chunk_scan(lq, lkf, lv, llff, s_f)
    o_b, _ = hgrn2_chunk_scan(flip(lq), flip(lkb), flip(lv), flip(llfb), s_b)
    a_lat = hgrn2_readout(o_f + flip(o_b), cols(u_lat, A_G, A_WIDTH), norm_g, u_lat.dtype)
    return a_ctx, a_lat


def rope_1d(t, pos):
    d = t.shape[-1] // 2
    inv = ROPE_BASE ** (-jnp.arange(d, dtype=f32) / d)
    ang = pos.astype(f32)[:, None] * inv[None, :]
    cos, sin = jnp.cos(ang), jnp.sin(ang)
    t1, t2 = t[..., :d].astype(f32), t[..., d:].astype(f32)
    return jnp.concatenate([t1 * cos - t2 * sin, t1 * sin + t2 * cos], axis=-1)


def axial_rope(t, pos_r, pos_c):
    half = t.shape[-1] // 2
    out = jnp.concatenate([rope_1d(t[..., :half], pos_r), rope_1d(t[..., half:], pos_c)], axis=-1)
    return out.astype(t.dtype)


def na_indices(rows):
    kr = min(NA_ROWS, rows)
    r = np.arange(rows)
    rs = np.clip(r - kr // 2, 0, rows - kr)
    key_rows = rs[:, None] + np.arange(kr)[None, :]
    dr = key_rows - r[:, None] + (NA_ROWS - 1)
    c = np.arange(GRID_W)
    cs = np.clip(c - NA_COLS // 2, 0, GRID_W - NA_COLS)
    in_win = (c[None, :] >= cs[:, None]) & (c[None, :] < cs[:, None] + NA_COLS)
    dc = np.clip(c[None, :] - c[:, None], -(NA_COLS - 1), NA_COLS - 1) + (NA_COLS - 1)
    return kr, key_rows, dr, in_win, dc


def neighbourhood_attention(q_rot, k_rot, v, q_plain, k_ctx, v_ctx, rpb_l):
    b, h, L, d = q_rot.shape
    rows = L // GRID_W
    kr, key_rows, dr, in_win, dc = na_indices(rows)
    n_lat = kr * GRID_W
    grid = lambda t: t.reshape(b, h, rows, GRID_W, d)
    k_blk = grid(k_rot)[:, :, key_rows].reshape(b, h, rows, n_lat, d)
    v_blk = grid(v)[:, :, key_rows].reshape(b, h, rows, n_lat, d)
    scale = d ** -0.5
    s_lat = jnp.einsum('bhrqd,bhrkd->bhrqk', grid(q_rot), k_blk).astype(f32) * scale
    bias = rpb_l[:, dr[:, None, :, None], dc[None, :, None, :]].reshape(h, rows, GRID_W, n_lat)
    mask = np.broadcast_to(in_win[:, None, :], (GRID_W, kr, GRID_W)).reshape(GRID_W, n_lat)
    s_lat = jnp.where(mask, s_lat + bias.astype(f32), MASK_VALUE)
    s_ctx = jnp.einsum('bhrqd,bhkd->bhrqk', grid(q_plain), k_ctx).astype(f32) * scale
    p = jax.nn.softmax(jnp.concatenate([s_lat, s_ctx], axis=-1), axis=-1).astype(v.dtype)
    o = (jnp.einsum('bhrqk,bhrkd->bhrqd', p[..., :n_lat], v_blk)
         + jnp.einsum('bhrqk,bhkd->bhrqd', p[..., n_lat:], v_ctx))
    return o.reshape(b, h, L, d)


def context_attention(q, k, v):
    s = jnp.einsum('bhqd,bhkd->bhqk', q, k).astype(f32) * (q.shape[-1] ** -0.5)
    p = jax.nn.softmax(s, axis=-1).astype(v.dtype)
    return jnp.einsum('bhqk,bhkd->bhqd', p, v)


def na_mixer(u_ctx, u_lat, rpb_l, pos_r, pos_c, ctx_out):
    kc = split_heads(cols(u_ctx, B_K, B_WIDTH), B_HEADS)
    vc = split_heads(cols(u_ctx, B_V, B_WIDTH), B_HEADS)
    q = split_heads(cols(u_lat, B_Q, B_WIDTH), B_HEADS)
    k = split_heads(cols(u_lat, B_K, B_WIDTH), B_HEADS)
    v = split_heads(cols(u_lat, B_V, B_WIDTH), B_HEADS)
    o_lat = neighbourhood_attention(axial_rope(q, pos_r, pos_c), axial_rope(k, pos_r, pos_c),
                                    v, q, kc, vc, rpb_l)
    b_lat = merge_heads(o_lat).astype(u_lat.dtype)
    if ctx_out:
        qc = split_heads(cols(u_ctx, B_Q, B_WIDTH), B_HEADS)
        b_ctx = merge_heads(context_attention(qc, kc, vc)).astype(u_ctx.dtype)
    else:
        b_ctx = None
    return b_ctx, b_lat


def grouped_moe(h, w_router, b_router, w_gate_l, w_up_l, w_down_l):
    shp = h.shape
    t = h.reshape(-1, shp[-1])
    n = t.shape[0]
    aff = jax.nn.sigmoid((t @ w_router).astype(f32))
    sel = (aff + b_router.astype(f32)).reshape(n, N_GROUPS, EXPERTS_PER_GROUP)
    group_score = lax.top_k(sel, TOP_K)[0].sum(-1)
    g_idx = jnp.argmax(group_score, axis=-1)
    in_grp = jnp.take_along_axis(sel, g_idx[:, None, None], axis=1)[:, 0]
    local = lax.top_k(in_grp, TOP_K)[1]
    e_idx = g_idx[:, None] * EXPERTS_PER_GROUP + local
    a_sel = jnp.take_along_axis(aff, e_idx, axis=1)
    gates = a_sel / jnp.sum(a_sel, axis=-1, keepdims=True)
    combine = jnp.sum(jax.nn.one_hot(e_idx, N_EXPERTS, dtype=f32) * gates[..., None], axis=1)
    out = jnp.zeros((n, shp[-1]), f32)
    for e in range(N_EXPERTS):
        hid = jax.nn.silu(t @ w_gate_l[e]) * (t @ w_up_l[e])
        out = out + combine[:, e:e + 1] * (hid @ w_down_l[e])
    return out.reshape(shp).astype(h.dtype)


def setup_inputs(seed: int = 0) -> dict:
    key = jax.random.key(seed)
    ks = jax.random.split(key, 20)
    D = D_MODEL
    nrm = lambda k, shape, s: jax.random.normal(k, shape, f32) * s
    return {
        'x': nrm(ks[0], (BATCH, SEQ, D), 1.0),
        'c': nrm(ks[1], (BATCH, D), 1.0),
        'ctx': nrm(ks[2], (BATCH, CTX_LEN, D), 1.0),
        'c_ctx': nrm(ks[3], (D,), 1.0),
        'w_ada': nrm(ks[4], (DEPTH, D, 6 * D), 0.5 * D ** -0.5),
        'b_ada': nrm(ks[5], (DEPTH, 6 * D), 0.02),
        'w_in': nrm(ks[6], (DEPTH, D, D_IN), D ** -0.5),
        'lb_logits': nrm(ks[7], (2, DEPTH, A_WIDTH), 0.5),
        'a_norm_g': 1.0 + nrm(ks[8], (DEPTH, A_DV), 0.02),
        'rpb': nrm(ks[9], (DEPTH, B_HEADS, 2 * NA_ROWS - 1, 2 * NA_COLS - 1), 0.05),
        'w_out': nrm(ks[10], (DEPTH, D_MIX, D), BETA * D_MIX ** -0.5),
        'ln1_g': 1.0 + nrm(ks[11], (DEPTH, D), 0.02),
        'ln1_b': nrm(ks[12], (DEPTH, D), 0.02),
        'w_router': nrm(ks[13], (D, N_EXPERTS), D ** -0.5),
        'b_router': nrm(ks[14], (N_EXPERTS,), 0.01),
        'w_gate': nrm(ks[15], (DEPTH, N_EXPERTS, D, D_EXPERT), D ** -0.5),
        'w_up': nrm(ks[16], (DEPTH, N_EXPERTS, D, D_EXPERT), D ** -0.5),
        'w_down': nrm(ks[17], (DEPTH, N_EXPERTS, D_EXPERT, D), BETA * D_EXPERT ** -0.5),
        'ln2_g': 1.0 + nrm(ks[18], (DEPTH, D), 0.02),
        'ln2_b': nrm(ks[19], (DEPTH, D), 0.02),
    }


def reference(x, c, ctx, c_ctx, w_ada, b_ada, w_in, lb_logits, a_norm_g, rpb, w_out, ln1_g, ln1_b,
              w_router, b_router, w_gate, w_up, w_down, ln2_g, ln2_b):
    seq = x.shape[1]
    t = jnp.arange(seq, dtype=jnp.int32)
    pos_r = t // GRID_W
    pos_c = t % GRID_W
    sm = jax.nn.softmax(lb_logits.astype(f32), axis=1)
    lower = jnp.cumsum(sm, axis=1) - sm[:, :1]
    silu_c = jax.nn.silu(c)
    silu_cc = jax.nn.silu(c_ctx)
    x_lat, x_ctx = x, ctx
    for l in range(DEPTH):
        ctx_out = l < DEPTH - 1
        mod_lat = (silu_c @ w_ada[l] + b_ada[l])[:, None, :]
        mod_ctx = silu_cc @ w_ada[l] + b_ada[l]
        sh1, sc1, g1, sh2, sc2, g2 = jnp.split(mod_lat, 6, axis=-1)
        ch1, cc1, cg1, ch2, cc2, cg2 = jnp.split(mod_ctx, 6, axis=-1)
        u_lat = (x_lat * (1.0 + sc1) + sh1) @ w_in[l]
        u_ctx = (x_ctx * (1.0 + cc1) + ch1) @ w_in[l]
        a_ctx, a_lat = hgrn2_mixer(u_ctx, u_lat, lower[0, l], lower[1, l], a_norm_g[l], ctx_out)
        b_ctx, b_lat = na_mixer(u_ctx, u_lat, rpb[l], pos_r, pos_c, ctx_out)
        y_lat = jnp.concatenate([a_lat, b_lat], axis=-1) @ w_out[l]
        x_lat = layer_norm(ALPHA * x_lat + g1 * y_lat, ln1_g[l], ln1_b[l])
        f_lat = grouped_moe(x_lat * (1.0 + sc2) + sh2, w_router, b_router, w_gate[l], w_up[l], w_down[l])
        x_lat = layer_norm(ALPHA * x_lat + g2 * f_lat, ln2_g[l], ln2_b[l])
        if ctx_out:
            y_ctx = jnp.concatenate([a_ctx, b_ctx], axis=-1) @ w_out[l]
            x_ctx = layer_norm(ALPHA * x_ctx + cg1 * y_ctx, ln1_g[l], ln1_b[l])
            f_ctx = grouped_moe(x_ctx * (1.0 + cc2) + ch2, w_router, b_router, w_gate[l], w_up[l], w_down[l])
            x_ctx = layer_norm(ALPHA * x_ctx + cg2 * f_ctx, ln2_g[l], ln2_b[l])
    return x_lat
```

```python
import os
import numpy as np
import concourse.bass as bass
import concourse.mybir as mybir
from concourse.bass_utils import run_bass_kernel_spmd

F32 = mybir.dt.float32
BF16 = mybir.dt.bfloat16
AF = mybir.ActivationFunctionType
ALU = mybir.AluOpType
AX = mybir.AxisListType

D = 1024
SEQ = 2048
CTX = 256
T = SEQ + CTX
NT = T // 128
NCH = T // 64
DEPTH = 4
A_Q, A_FF, A_FB, A_I, A_G, B_Q, B_K, B_V = 0, 512, 1024, 1536, 2048, 2560, 3072, 3584
ALPHA = (2 * DEPTH) ** 0.25
LN_EPS = 1e-5
RMS_EPS = 1e-6
NEXP = 16


class Buf:
    __slots__ = ("name", "writer", "readers")

    def __init__(self, name=""):
        self.name = name
        self.writer = None
        self.readers = []


class K:
    def __init__(self, nc):
        self.nc = nc
        self.h = {"pe": nc.tensor, "act": nc.scalar, "dve": nc.vector, "pool": nc.gpsimd, "sp": nc.sync}
        self.sem = {}
        self.cnt = {}
        self.seen = {}
        for e in self.h:
            self.sem[e] = nc.semaphore("s_" + e).__enter__()
            self.cnt[e] = 0
            self.seen[e] = {}
        self.ND = 8
        self.dsem = {}
        self.dcnt = {}
        self.dnext = {}
        for q in ("sp", "pool"):
            self.dsem[q] = [nc.semaphore(f"d_{q}{i}").__enter__() for i in range(self.ND)]
            self.dcnt[q] = [0] * self.ND
            self.dnext[q] = 0
        self.ninst = 0

    def _wait(self, e, ev):
        key, sem, val = ev
        if self.seen[e].get(key, 0) >= val:
            return
        self.h[e].wait_ge(sem, val)
        self.seen[e][key] = val

    def _deps(self, e, R, W):
        mx = {}
        for b in R:
            ev = b.writer
            if ev is not None and (ev[0] not in mx or mx[ev[0]][2] < ev[2]):
                mx[ev[0]] = ev
        for b in W:
            ev = b.writer
            if ev is not None and (ev[0] not in mx or mx[ev[0]][2] < ev[2]):
                mx[ev[0]] = ev
            for ev in b.readers:
                if ev[0] not in mx or mx[ev[0]][2] < ev[2]:
                    mx[ev[0]] = ev
        for key, ev in mx.items():
            if e == "pe" and key == "pe":
                continue
            self._wait(e, ev)

    def _mark(self, ev, R, W):
        for b in W:
            b.writer = ev
            b.readers = []
        for b in R:
            rs = b.readers
            for i, r in enumerate(rs):
                if r[0] == ev[0]:
                    rs[i] = ev
                    break
            else:
                rs.append(ev)

    def op(self, e, fn, R=(), W=()):
        self._deps(e, R, W)
        inst = fn(self.h[e])
        self.cnt[e] += 1
        inst.then_inc(self.sem[e], 1)
        self._mark((e, self.sem[e], self.cnt[e]), R, W)
        self.ninst += 1
        return inst

    def dma(self, q, out, in_, R=(), W=(), **kw):
        i = self.dnext[q]
        self.dnext[q] = (i + 1) % self.ND
        key = f"d_{q}{i}"
        sem = self.dsem[q][i]
        if self.dcnt[q][i] > 0:
            self._wait(q, (key, sem, self.dcnt[q][i]))
        self._deps(q, R, W)
        inst = self.h[q].dma_start(out=out, in_=in_, **kw)
        self.dcnt[q][i] += 16
        inst.then_inc(sem, 16)
        self._mark((key, sem, self.dcnt[q][i]), R, W)
        self.ninst += 1
        return inst

    def barrier(self):
        evs = [(e, self.sem[e], self.cnt[e]) for e in self.h if self.cnt[e] > 0]
        for q in self.dsem:
            for i in range(self.ND):
                if self.dcnt[q][i] > 0:
                    evs.append((f"d_{q}{i}", self.dsem[q][i], self.dcnt[q][i]))
        for e in self.h:
            for ev in evs:
                if not (e == "pe" and ev[0] == "pe"):
                    self._wait(e, ev)


class Rot:
    def __init__(self, items):
        self.items = items
        self.i = 0

    def next(self):
        it = self.items[self.i]
        self.i = (self.i + 1) % len(self.items)
        return it


def host_consts():
    cst = np.zeros((128, 704), np.float32)
    cst[:, 0:128] = np.eye(128, dtype=np.float32)
    J = np.zeros((128, 128), np.float32)
    P = np.zeros((128, 128), np.float32)
    for m in range(128):
        blk, q = divmod(m, 64)
        J[blk * 64 + (63 - q), m] = 1.0
        d = m % 64
        partner = d + 16 if (d % 32) < 16 else d - 16
        P[blk * 64 + partner, m] = 1.0
    cst[:, 128:256] = J
    cst[:, 256:384] = P
    s = np.arange(64)[:, None]
    t = np.arange(64)[None, :]
    cst[0:64, 384:448] = (s <= t).astype(np.float32)
    cst[0:64, 448:512] = (s >= t).astype(np.float32)
    c = np.arange(64)
    cs = np.clip(c - 8, 0, 48)
    inwin = (c[None, :] >= cs[:, None]) & (c[None, :] < cs[:, None] + 16)
    wm = np.where(inwin, 0.0, -1e30).astype(np.float32)
    cst[0:64, 512:576] = wm
    cst[64:128, 512:576] = wm
    cst[:, 576:704] = 1.0 / 128.0
    inv = 10000.0 ** (-np.arange(16, dtype=np.float32) / 16.0)
    tt = np.arange(SEQ)
    pos_r = (tt // 64).astype(np.float32)
    pos_c = (tt % 64).astype(np.float32)
    rope = np.zeros((128, 2 * SEQ), np.float32)
    for p in range(128):
        d = p % 64
        j = d % 16
        pos = pos_r if d < 32 else pos_c
        ang = pos * inv[j]
        sign = -1.0 if (d % 32) < 16 else 1.0
        rope[p, :SEQ] = np.cos(ang)
        rope[p, SEQ:] = sign * np.sin(ang)
    return cst, rope


class _Stop(Exception):
    pass


def build(nlayers=DEPTH, stop=None, dbg=()):
    nc = bass.Bass("TRN2", target_bir_lowering=False)
    k = K(nc)
    dbg_out = {}
    try:
        _build(nc, k, dbg_out, nlayers, stop, dbg)
    except _Stop:
        pass
    k.barrier()
    return nc, dbg_out


def _build(nc, k, dbg_out, nlayers, stop, dbg):
    def ck(name):
        if stop == name:
            raise _Stop()

    def dram(name, shape, dt=F32, kind="ExternalInput"):
        return nc.dram_tensor(name, list(shape), dt, kind=kind).ap()

    uid = [0]

    class Scope:
        def __init__(self):
            self.items = []

        def sb(self, name, shape, dt=F32):
            uid[0] += 1
            t = nc.sbuf_tensor(f"{name}_{uid[0]}", list(shape), dt)
            h = t.__enter__()
            self.items.append(t)
            return h

        def close(self):
            k.barrier()
            for t in reversed(self.items):
                t.__exit__(None, None, None)
            self.items = []

    x_in = dram("x", [SEQ, D])
    ctx_in = dram("ctx", [CTX, D])
    c_in = dram("c", [1, D])
    cc_in = dram("c_ctx", [1, D])
    w_ada = dram("w_ada", [DEPTH, D, 6 * D])
    b_ada = dram("b_ada", [DEPTH, 6 * D])
    w_in = dram("w_in", [DEPTH, D, 4096])
    lb_logits = dram("lb_logits", [2, DEPTH, 512])
    a_norm_g = dram("a_norm_g", [DEPTH, 128])
    rpb = dram("rpb", [DEPTH, 8, 15, 31])
    w_out = dram("w_out", [DEPTH, D, D])
    ln1_g = dram("ln1_g", [DEPTH, D])
    ln1_b = dram("ln1_b", [DEPTH, D])
    w_router = dram("w_router", [D, NEXP])
    b_router = dram("b_router", [1, NEXP])
    w_gate = dram("w_gate", [DEPTH, NEXP, D, 512])
    w_up = dram("w_up", [DEPTH, NEXP, D, 512])
    w_down = dram("w_down", [DEPTH, NEXP, 512, D])
    ln2_g = dram("ln2_g", [DEPTH, D])
    ln2_b = dram("ln2_b", [DEPTH, D])
    cst_in = dram("cst", [128, 704])
    rope_in = dram("rope", [128, 2 * SEQ])
    out = dram("out", [SEQ, D], kind="ExternalOutput")
    xs = dram("xs_scr", [T, D], kind="Internal")
    modd = dram("mod_scr", [DEPTH, 2, 6 * D], kind="Internal")
    rpbpad = dram("rpb_scr", [120, 128], kind="Internal")
    combT_d = dram("combT_scr", [16, T], kind="Internal")

    def dbg_dump(name, src_ap, src_buf, shape, dt=F32):
        if name not in dbg:
            return
        o = dram("dbg_" + name, shape, dt=dt, kind="ExternalOutput")
        b = Buf()
        srcs = src_buf if isinstance(src_buf, list) else [src_buf]
        k.dma("sp", o, src_ap, R=srcs, W=[b])
        dbg_out[name] = b

    ps2 = [nc.psum_tensor(f"ps{i}", [128, 1024], F32).__enter__() for i in range(4)]
    bbank = [Buf(f"bank{i}") for i in range(8)]

    def bank(i):
        return ps2[i // 2][:, (i % 2) * 512:(i % 2) * 512 + 512]

    G = Scope()
    cst = G.sb("cst_sb", [128, 704])
    b_cst = Buf()
    ident32 = cst[:, 0:128]
    J2 = cst[:, 128:256]
    cmf = cst[0:64, 384:448]
    cmb = cst[0:64, 448:512]
    winm = cst[:, 512:576]
    ident16 = G.sb("ident16", [128, 128], BF16)
    Pm16 = G.sb("Pm16", [128, 128], BF16)
    onesm = G.sb("onesm", [128, 128], BF16)
    cmi = G.sb("cmi", [64, 2, 64], mybir.dt.int32)
    rmask = G.sb("rmask", [128, 512], BF16)
    XT = G.sb("XT", [128, 8, T], BF16)
    bXT = [Buf(f"xt{i}") for i in range(NT)]
    modc = G.sb("modc", [128, DEPTH, 2, 6, 8])
    b_modc = Buf()
    lbv = G.sb("lbv", [128, 2, DEPTH, 4])
    oml = G.sb("oml", [128, 2, DEPTH, 4])
    noml = G.sb("noml", [128, 2, DEPTH, 4])
    b_lb = Buf()
    ang = G.sb("ang", [128, DEPTH])
    b_ang = Buf()
    wr32 = G.sb("wr32", [128, 8, NEXP])
    b_wr = Buf()
    brb = G.sb("brb", [128, NEXP])
    b_brb = Buf()
    b_combT = Buf()

    k.dma("sp", cst[:], cst_in[:, :], W=[b_cst])
    k.op("dve", lambda e: e.tensor_copy(out=ident16[:], in_=cst[:, 0:128]), R=[b_cst], W=[b_cst])
    k.op("dve", lambda e: e.tensor_copy(out=Pm16[:], in_=cst[:, 256:384]), R=[b_cst], W=[b_cst])
    k.op("dve", lambda e: e.tensor_copy(out=onesm[:], in_=cst[:, 576:704]), R=[b_cst], W=[b_cst])
    k.op("dve", lambda e: e.tensor_copy(out=cmi[:].rearrange("p a b -> p (a b)"), in_=cst[0:64, 384:512]), R=[b_cst], W=[b_cst])
    k.op("pool", lambda e: e.memset(rmask[:], 1.0), W=[b_cst])
    k.op("pool", lambda e: e.memset(rmask[:, 0:512:64], 0.0), R=[b_cst], W=[b_cst])
    k.dma("sp", wr32[:], w_router.rearrange("(ch p) e -> p ch e", p=128), W=[b_wr])
    k.dma("sp", brb[:], bass.AP(tensor=b_router.tensor, offset=0, ap=[[0, 128], [1, NEXP]]), W=[b_brb])

    ck("P0")
    PR = Scope()
    craw = PR.sb("craw", [128, 2, 8])
    csil = PR.sb("csil", [128, 2, 8])
    b_c = Buf()
    k.dma("sp", craw[:, 0, :], c_in.rearrange("o (ch p) -> p (o ch)", p=128), W=[b_c], allow_slow_non_contiguous=True)
    k.dma("sp", craw[:, 1, :], cc_in.rearrange("o (ch p) -> p (o ch)", p=128), W=[b_c], allow_slow_non_contiguous=True)
    k.op("act", lambda e: e.activation(out=csil[:], in_=craw[:], func=AF.Silu), R=[b_c], W=[b_c])
    ck("P1")
    wa = [PR.sb(f"wa{i}", [128, 8, 512]) for i in range(2)]
    b_wa = [Buf(), Buf()]
    modrow = PR.sb("modrow", [2, 6 * D])
    b_modrow = Buf()
    badar = PR.sb("badar", [2, 6 * D])
    b_bada = Buf()
    b_modd_all = []
    mt48 = PR.sb("mt48", [48, 2, 128])
    b_mt48 = Buf()
    for l in range(nlayers):
        for j in range(2):
            k.dma("sp", badar[j:j + 1, :], b_ada[l:l + 1, :], W=[b_bada])
        for cb in range(12):
            i = (l * 12 + cb) % 2
            k.dma("sp", wa[i][:], w_ada[l, :, cb * 512:(cb + 1) * 512].rearrange("(ch p) c -> p ch c", p=128), W=[b_wa[i]])
            pb = 6 + (cb % 2)
            for ch in range(8):
                k.op("pe", lambda e: e.matmul(bank(pb)[0:2, :], lhsT=csil[:, :, ch], rhs=wa[i][:, ch, :],
                                              start=(ch == 0), stop=(ch == 7)), R=[b_c, b_wa[i]], W=[bbank[pb]])
            k.op("dve", lambda e: e.tensor_tensor(out=modrow[:, cb * 512:(cb + 1) * 512], in0=bank(pb)[0:2, :],
                                                  in1=badar[:, cb * 512:(cb + 1) * 512], op=ALU.add),
                 R=[bbank[pb], b_bada], W=[b_modrow])
        ck("P2")
        b_modd = Buf()
        b_modd_all.append(b_modd)
        k.dma("sp", modd[l, :, :], modrow[:], R=[b_modrow], W=[b_modd])
        for j in range(2):
            k.dma("sp", mt48[:, j, :], modd[l, j, :].rearrange("(r p) -> r p", p=128), R=[b_modd], W=[b_mt48])
            k.op("pe", lambda e: e.transpose(out=bank(5)[:, j * 48:(j + 1) * 48], in_=mt48[:, j, :], identity=ident32[0:48, 0:48]),
                 R=[b_mt48, b_cst], W=[bbank[5]])
        k.op("dve", lambda e: e.tensor_copy(out=modc[:, l, :, :, :].rearrange("p j w c -> p (j w c)"), in_=bank(5)[:, 0:96]),
             R=[bbank[5]], W=[b_modc])
        for w_ in (1, 4):
            k.op("dve", lambda e: e.tensor_scalar(out=modc[:, l, :, w_, :], in0=modc[:, l, :, w_, :], scalar1=1.0, scalar2=None, op0=ALU.add),
                 R=[b_modc], W=[b_modc])
    if "modrow" in dbg:
        dbg_dump("modrow", modrow[:], b_modrow, [2, 6 * D])
    ck("P3")
    lraw = PR.sb("lraw", [128, 2, DEPTH, 4])
    lexp = PR.sb("lexp", [128, 2, DEPTH, 4])
    lsum = PR.sb("lsum", [128, 2, 4])
    lmx = PR.sb("lmx", [128, 2, 4])
    b_l = Buf()
    for d_ in range(2):
        for l_ in range(DEPTH):
            k.dma("sp", lraw[:, d_, l_, :], lb_logits[d_, l_, :].rearrange("(h p) -> p h", p=128), W=[b_l], allow_slow_non_contiguous=True)
    k.op("dve", lambda e: e.tensor_tensor(out=lmx[:], in0=lraw[:, :, 0, :], in1=lraw[:, :, 1, :], op=ALU.max), R=[b_l], W=[b_l])
    k.op("dve", lambda e: e.tensor_tensor(out=lmx[:], in0=lmx[:], in1=lraw[:, :, 2, :], op=ALU.max), R=[b_l], W=[b_l])
    k.op("dve", lambda e: e.tensor_tensor(out=lmx[:], in0=lmx[:], in1=lraw[:, :, 3, :], op=ALU.max), R=[b_l], W=[b_l])
    for l_ in range(DEPTH):
        k.op("dve", lambda e: e.tensor_tensor(out=lexp[:, :, l_, :], in0=lraw[:, :, l_, :], in1=lmx[:], op=ALU.subtract), R=[b_l], W=[b_l])
    k.op("act", lambda e: e.activation(out=lexp[:], in_=lexp[:], func=AF.Exp), R=[b_l], W=[b_l])
    k.op("dve", lambda e: e.tensor_tensor(out=lsum[:], in0=lexp[:, :, 0, :], in1=lexp[:, :, 1, :], op=ALU.add), R=[b_l], W=[b_l])
    k.op("dve", lambda e: e.tensor_tensor(out=lsum[:], in0=lsum[:], in1=lexp[:, :, 2, :], op=ALU.add), R=[b_l], W=[b_l])
    k.op("dve", lambda e: e.tensor_tensor(out=lsum[:], in0=lsum[:], in1=lexp[:, :, 3, :], op=ALU.add), R=[b_l], W=[b_l])
    k.op("dve", lambda e: e.reciprocal(out=lsum[:], in_=lsum[:]), R=[b_l], W=[b_l])
    k.op("dve", lambda e: e.memset(lbv[:, :, 0, :], 0.0), R=[b_l], W=[b_lb])
    for l_ in range(1, DEPTH):
        k.op("dve", lambda e: e.tensor_tensor(out=lexp[:, :, l_, :], in0=lexp[:, :, l_, :], in1=lsum[:], op=ALU.mult), R=[b_l], W=[b_l])
        k.op("dve", lambda e: e.tensor_tensor(out=lbv[:, :, l_, :], in0=lbv[:, :, l_ - 1, :], in1=lexp[:, :, l_, :], op=ALU.add), R=[b_l, b_lb], W=[b_lb])
    k.op("dve", lambda e: e.tensor_scalar(out=oml[:], in0=lbv[:], scalar1=-1.0, scalar2=1.0, op0=ALU.mult, op1=ALU.add), R=[b_lb], W=[b_lb])
    k.op("dve", lambda e: e.tensor_scalar(out=noml[:], in0=lbv[:], scalar1=1.0, scalar2=-1.0, op0=ALU.mult, op1=ALU.add), R=[b_lb], W=[b_lb])
    ck("P4")
    k.dma("sp", ang[:], a_norm_g.rearrange("l p -> p l"), W=[b_ang], allow_slow_non_contiguous=True)
    zt = PR.sb("zt", [120, 128])
    b_zt = Buf()
    b_rpbpad = Buf()
    k.op("pool", lambda e: e.memset(zt[:], 0.0), W=[b_zt])
    k.dma("sp", rpbpad[:, :], zt[:], R=[b_zt], W=[b_rpbpad])
    dbg_dump("modc", modc[:].rearrange("p l j w c -> p (l j w c)"), b_modc, [128, DEPTH * 96])
    dbg_dump("lbv", lbv[:].rearrange("p a b c -> p (a b c)"), b_lb, [128, 32])
    PR.close()
    ck("P")

    psT = Rot([0, 1])

    def xt_bufs(t0, n):
        return [bXT[i] for i in range(t0 // 128, (t0 + n + 127) // 128)]

    def transpose_tile(xtile, bx, tt, l, which_sc, which_sh, tf32=None, b_tf=None):
        j = 1 if tt < 2 else 0
        for half in range(2):
            pb = psT.next()
            for q in range(4):
                ch = half * 4 + q
                k.op("pe", lambda e: e.transpose(out=bank(pb)[:, q * 128:(q + 1) * 128], in_=xtile[:, ch * 128:(ch + 1) * 128],
                                                 identity=ident32), R=[bx, b_cst], W=[bbank[pb]])
            for q in range(4):
                ch = half * 4 + q
                sc = modc[:, l, j, which_sc, ch:ch + 1]
                sh = modc[:, l, j, which_sh, ch:ch + 1]
                if tf32 is None:
                    k.op("act", lambda e: e.activation(out=XT[:, ch, tt * 128:(tt + 1) * 128], in_=bank(pb)[:, q * 128:(q + 1) * 128],
                                                       func=AF.Identity, scale=sc, bias=sh), R=[bbank[pb], b_modc], W=[bXT[tt]])
                else:
                    k.op("act", lambda e: e.activation(out=tf32[:, ch, :], in_=bank(pb)[:, q * 128:(q + 1) * 128],
                                                       func=AF.Identity, scale=sc, bias=sh), R=[bbank[pb], b_modc], W=[b_tf])
                    k.op("act", lambda e: e.activation(out=XT[:, ch, tt * 128:(tt + 1) * 128], in_=bank(pb)[:, q * 128:(q + 1) * 128],
                                                       func=AF.Identity, scale=sc, bias=sh), R=[bbank[pb], b_modc], W=[bXT[tt]])

    def proj_fm(pb, W, bW, wsl, t0, n):
        for ch in range(8):
            k.op("pe", lambda e: e.matmul(bank(pb)[:, 0:n], lhsT=W[(slice(None), ch) + tuple(wsl)], rhs=XT[:, ch, t0:t0 + n],
                                          start=(ch == 0), stop=(ch == 7)), R=[bW] + xt_bufs(t0, n), W=[bbank[pb]])

    BLKS = [(0, 512), (512, 512), (1024, 512), (1536, 512), (2048, 256)]

    S0 = Scope()
    xin = [S0.sb(f"xin{i}", [128, D]) for i in range(2)]
    b_xin = [Buf(), Buf()]
    for tt in range(NT):
        i = tt % 2
        src = ctx_in[tt * 128:(tt + 1) * 128, :] if tt < 2 else x_in[(tt - 2) * 128:(tt - 1) * 128, :]
        k.dma("sp", xin[i][:], src, W=[b_xin[i]])
        transpose_tile(xin[i], b_xin[i], tt, 0, 1, 0)
    S0.close()
    if "xt0" in dbg:
        dbg_dump("xt0", XT[:, :, :].rearrange("p c t -> p (c t)"), bXT, [128, 8 * T], dt=BF16)

    ck("T0")
    b_xs = [Buf(f"xs{i}") for i in range(NT)]

    for l in range(nlayers):
        last = (l == DEPTH - 1)
        MS = Scope()
        MIX = MS.sb("MIX", [128, 8, T], BF16)
        b_mix = [Buf(f"mix{i}") for i in range(8)]

        SA = Scope()
        Wh = SA.sb("Wh", [128, 8, 5, 128], BF16)
        b_Wh = Buf()
        nT = 2
        QSb = [SA.sb(f"QSb{i}", [128, 512]) for i in range(nT)]
        Xb2 = [[SA.sb(f"Xb{d}{i}", [128, 512]) for i in range(nT)] for d in range(2)]
        bX2 = [[Buf() for _ in range(nT)] for d in range(2)]
        Fb = [SA.sb(f"Fb{i}", [128, 512]) for i in range(nT)]
        Cb = [SA.sb(f"Cb{i}", [128, 512]) for i in range(nT)]
        Db = [SA.sb(f"Db{i}", [128, 512]) for i in range(nT)]
        Eb = [SA.sb(f"Eb{i}", [128, 512]) for i in range(nT)]
        D2b = [SA.sb(f"D2b{i}", [128, 512]) for i in range(nT)]
        E2b = [SA.sb(f"E2b{i}", [128, 512]) for i in range(nT)]
        bD2 = [Buf() for _ in range(nT)]
        bE2 = [Buf() for _ in range(nT)]
        bQS = [Buf() for _ in range(nT)]
        bX = [Buf() for _ in range(nT)]
        bF = [Buf() for _ in range(nT)]
        bC = [Buf() for _ in range(nT)]
        bD = [Buf() for _ in range(nT)]
        bE = [Buf() for _ in range(nT)]
        QIN = [SA.sb(f"QIN{d}", [128, T], BF16) for d in range(2)]
        QS2 = [SA.sb(f"QS2{d}", [128, T], BF16) for d in range(2)]
        KDEC = [SA.sb(f"KDEC{d}", [128, T], BF16) for d in range(2)]
        KD2 = [SA.sb(f"KD2{d}", [128, T], BF16) for d in range(2)]
        b_q3 = [Buf(), Buf()]
        GS = SA.sb("GS", [128, T], BF16)
        b_gs = Buf()
        TOT = [SA.sb(f"TOT{d}", [128, NCH]) for d in range(2)]
        b_tot = [Buf(), Buf()]
        V64 = SA.sb("V64", [64, NCH, 128], BF16)
        b_v64 = Buf()
        OD = [SA.sb(f"OD{d}", [128, T]) for d in range(2)]
        b_od = [Buf(), Buf()]
        S32 = [SA.sb(f"S32{d}", [128, 128]) for d in range(2)]
        S16 = [SA.sb(f"S16{d}", [128, 128], BF16) for d in range(2)]
        S16b = [[SA.sb(f"S16b{d}{j}", [128, 128], BF16) for j in range(2)] for d in range(2)]
        b_s16b = [[Buf(), Buf()] for d in range(2)]
        b_s32 = [Buf(), Buf()]
        b_s16 = [Buf(), Buf()]
        kdT = [SA.sb(f"kdT{i}", [64, 128], BF16) for i in range(4)]
        b_kdT = [Buf() for _ in range(4)]
        scm = [SA.sb(f"scm{i}", [64, 64], BF16) for i in range(4)]
        b_scm = [Buf() for _ in range(4)]
        sqb = [SA.sb(f"sqb{i}", [128, 512], BF16) for i in range(2)]
        b_sq = [Buf(), Buf()]
        pA = Rot([2, 3, 4, 5])
        for h in range(0 if not os.environ.get('SKIPA') else 4, 4):
            for fi, c0 in enumerate([A_Q, A_G, A_FF, A_FB, A_I]):
                k.dma("pool", Wh[:, :, fi, :], w_in[l, :, c0 + h * 128:c0 + h * 128 + 128].rearrange("(ch p) c -> p ch c", p=128), W=[b_Wh])
            ck("A0")
            for bi, (t0, n) in enumerate(BLKS):
                i = bi % nT
                nch = n // 64
                ck0 = t0 // 64
                if bi == 1:
                    ck("A2")
                pb = pA.next()
                proj_fm(pb, Wh, b_Wh, (0, slice(None)), t0, n)
                k.op("act", lambda e: e.activation(out=QSb[i][:, 0:n], in_=bank(pb)[:, 0:n], func=AF.Silu), R=[bbank[pb]], W=[bQS[i]])
                pb = pA.next()
                proj_fm(pb, Wh, b_Wh, (1, slice(None)), t0, n)
                k.op("act", lambda e: e.activation(out=GS[:, t0:t0 + n], in_=bank(pb)[:, 0:n], func=AF.Silu), R=[bbank[pb]], W=[b_gs])
                ck("A1")
                for d in range(2):
                    pb = pA.next()
                    proj_fm(pb, Wh, b_Wh, (2 + d, slice(None)), t0, n)
                    k.op("act", lambda e: e.activation(out=Xb2[d][i][:, 0:n], in_=bank(pb)[:, 0:n], func=AF.Sigmoid), R=[bbank[pb]], W=[bX2[d][i]])
                for d in range(2):
                    lb_ap = lbv[:, d, l, h:h + 1]
                    oml_ap = oml[:, d, l, h:h + 1]
                    noml_ap = noml[:, d, l, h:h + 1]
                    X_, F_, C_, D_, E_ = Xb2[d][i][:, 0:n], Fb[i][:, 0:n], Cb[i][:, 0:n], Db[i][:, 0:n], Eb[i][:, 0:n]
                    bX = bX2[d]
                    k.op("dve", lambda e: e.tensor_scalar(out=F_, in0=X_, scalar1=1e-6, scalar2=None, op0=ALU.max), R=[bX[i]], W=[bF[i]])
                    k.op("act", lambda e: e.activation(out=F_, in_=F_, func=AF.Ln, scale=oml_ap, bias=lb_ap), R=[bF[i], b_lb], W=[bF[i]])
                    k.op("pool", lambda e: e.tensor_scalar(out=X_, in0=X_, scalar1=noml_ap, scalar2=oml_ap, op0=ALU.mult, op1=ALU.add),
                         R=[bX[i], b_lb], W=[bX[i]])
                    k.op("dve", lambda e: e.tensor_tensor_scan(out=C_, data0=rmask[:, 0:n], data1=F_, initial=0.0, op0=ALU.mult, op1=ALU.add),
                         R=[bF[i], b_cst], W=[bC[i]])
                    C3 = C_.rearrange("p (c s) -> p c s", s=64)
                    Ctb = C3[:, :, 63:64].broadcast_to([128, nch, 64])
                    D3 = D_.rearrange("p (c s) -> p c s", s=64)
                    D2_ = D2b[i][:, 0:n]
                    D23 = D2_.rearrange("p (c s) -> p c s", s=64)
                    if d == 0:
                        Cmb = C3[:, :, 31:32].broadcast_to([128, nch, 64])
                        k.op("dve", lambda e: e.tensor_tensor(out=D3, in0=C3, in1=Ctb, op=ALU.subtract), R=[bC[i]], W=[bD[i]])
                        k.op("pool", lambda e: e.tensor_tensor(out=D23, in0=C3, in1=Cmb, op=ALU.subtract), R=[bC[i]], W=[bD2[i]])
                        a1, a1b, a1s = C_, bC[i], 1.0
                        a3, a3b, a3s = D_, bD[i], -1.0
                    else:
                        k.op("dve", lambda e: e.tensor_tensor(out=F_, in0=C_, in1=F_, op=ALU.subtract), R=[bC[i], bF[i]], W=[bF[i]])
                        F3 = F_.rearrange("p (c s) -> p c s", s=64)
                        Pmb = F3[:, :, 32:33].broadcast_to([128, nch, 64])
                        k.op("dve", lambda e: e.tensor_tensor(out=D3, in0=Ctb, in1=F3, op=ALU.subtract), R=[bC[i], bF[i]], W=[bD[i]])
                        k.op("pool", lambda e: e.tensor_tensor(out=D23, in0=Pmb, in1=F3, op=ALU.subtract), R=[bF[i]], W=[bD2[i]])
                        a1, a1b, a1s = D_, bD[i], 1.0
                        a3, a3b, a3s = F_, bF[i], 1.0
                    k.op("act", lambda e: e.activation(out=TOT[d][:, ck0:ck0 + nch], in_=C_[:, 63:n:64], func=AF.Exp), R=[bC[i]], W=[b_tot[d]])
                    k.op("act", lambda e: e.activation(out=E_, in_=a1, func=AF.Exp, scale=a1s), R=[a1b], W=[bE[i]])
                    k.op("dve", lambda e: e.tensor_tensor(out=QIN[d][:, t0:t0 + n], in0=QSb[i][:, 0:n], in1=E_, op=ALU.mult),
                         R=[bQS[i], bE[i]], W=[b_q3[d]])
                    k.op("act", lambda e: e.activation(out=E_, in_=a3, func=AF.Exp, scale=a3s), R=[a3b], W=[bE[i]])
                    k.op("dve", lambda e: e.tensor_tensor(out=KDEC[d][:, t0:t0 + n], in0=X_, in1=E_, op=ALU.mult),
                         R=[bX[i], bE[i]], W=[b_q3[d]])
                    E2_ = E2b[i][:, 0:n]
                    k.op("act", lambda e: e.activation(out=E2_, in_=D2_, func=AF.Exp, scale=1.0), R=[bD2[i]], W=[bE2[i]])
                    k.op("pool", lambda e: e.tensor_tensor(out=QS2[d][:, t0:t0 + n], in0=QSb[i][:, 0:n], in1=E2_, op=ALU.mult),
                         R=[bQS[i], bE2[i]], W=[b_q3[d]])
                    k.op("act", lambda e: e.activation(out=E_, in_=D2_, func=AF.Exp, scale=-1.0), R=[bD2[i]], W=[bE[i]])
                    k.op("dve", lambda e: e.tensor_tensor(out=KD2[d][:, t0:t0 + n], in0=X_, in1=E_, op=ALU.mult),
                         R=[bX[i], bE[i]], W=[b_q3[d]])
            ck("A3")
            for c4 in range(NCH // 4):
                pb = pA.next()
                for q in range(4):
                    c = c4 * 4 + q
                    for ch in range(8):
                        k.op("pe", lambda e: e.matmul(bank(pb)[0:64, q * 128:(q + 1) * 128], lhsT=XT[:, ch, c * 64:(c + 1) * 64],
                                                      rhs=Wh[:, ch, 4, :], start=(ch == 0), stop=(ch == 7)),
                             R=[b_Wh] + xt_bufs(c * 64, 64), W=[bbank[pb]])
                k.op("act", lambda e: e.activation(out=V64[:, c4 * 4:(c4 + 1) * 4, :].rearrange("p c v -> p (c v)"), in_=bank(pb)[0:64, :],
                                                   func=AF.Copy), R=[bbank[pb]], W=[b_v64])
            ck("A4")
            for d in range(2):
                k.op("pool", lambda e: e.memset(S32[d][:], 0.0), W=[b_s32[d]])
                k.op("pool", lambda e: e.memset(S16[d][:], 0.0), W=[b_s16[d]])
                k.op("pool", lambda e: e.memset(S16b[d][0][:], 0.0), W=[b_s16b[d][0]])
            orders = [list(range(NCH)), [3, 2, 1, 0] + list(range(NCH - 1, 3, -1))]
            cms = [cmf, cmb]
            ri = int(os.environ.get('RI0', 0))
            b_pt = [bbank[0], bbank[4]]
            b_psc = [bbank[1], bbank[5]]
            b_po = [bbank[2], bbank[6]]
            b_pds = [bbank[3], bbank[7]]
            for step in range(int(os.environ.get('NSTEPS', NCH))):
                for d in [int(x) for x in os.environ.get('DIRS', '01')]:
                    c = orders[d][step]
                    cs_ = slice(c * 64, (c + 1) * 64)
                    r4 = ri % 4
                    ri += 1
                    pt_ap = bank(4 * d).bitcast(BF16)[0:64, 0:128]
                    psc_ap = bank(4 * d + 1)[0:64, 0:64]
                    po_ap = bank(4 * d + 2)[:, 0:64]
                    pds_ap = bank(4 * d + 3)[:, 0:128]
                    k.op("pe", lambda e: e.transpose(out=pt_ap, in_=KDEC[d][:, cs_], identity=ident16[:]), R=[b_q3[d], b_cst], W=[b_pt[d]])
                    if step == 0 and d == 0: ck("R0")
                    k.op("act", lambda e: e.activation(out=kdT[r4][:], in_=pt_ap, func=AF.Copy), R=[b_pt[d]], W=[b_kdT[r4]])
                    if step == 0 and d == 0: ck("R1")
                    k.op("pe", lambda e: e.matmul(psc_ap, lhsT=KD2[d][:, cs_], rhs=QS2[d][:, cs_], start=True, stop=True),
                         R=[b_q3[d]], W=[b_psc[d]])
                    if step == 0 and d == 0: ck("R2")
                    k.op("pool", lambda e: e.memset(scm[r4][:], 0.0), W=[b_scm[r4]])
                    k.op("dve", lambda e: e.copy_predicated(out=scm[r4][:], mask=cmi[:, d, :], data=psc_ap), R=[b_psc[d], b_cst, b_scm[r4]], W=[b_scm[r4]])
                    if step == 0 and d == 0: ck("R3")
                    if os.environ.get("S32MASTER"):
                        k.op("pe", lambda e: e.matmul(po_ap, lhsT=S16[d][:], rhs=QIN[d][:, cs_], start=True, stop=False),
                             R=[b_s16[d], b_q3[d]], W=[b_po[d]])
                    else:
                        k.op("pe", lambda e: e.matmul(po_ap, lhsT=S16b[d][step % 2][:], rhs=QIN[d][:, cs_], start=True, stop=False),
                             R=[b_s16b[d][step % 2], b_q3[d]], W=[b_po[d]])
                    if step == 0 and d == 0: ck("R4")
                    k.op("pe", lambda e: e.matmul(po_ap, lhsT=V64[:, c, :], rhs=scm[r4][:], start=False, stop=True),
                         R=[b_v64, b_scm[r4]], W=[b_po[d]])
                    if step == 0 and d == 0: ck("R5")
                    k.op("act", lambda e: e.activation(out=OD[d][:, cs_], in_=po_ap, func=AF.Copy), R=[b_po[d]], W=[b_od[d]])
                    if step == 0 and d == 0: ck("R6")
                    k.op("pe", lambda e: e.matmul(pds_ap, lhsT=kdT[r4][:], rhs=V64[:, c, :], start=True, stop=True),
                         R=[b_kdT[r4], b_v64], W=[b_pds[d]])
                    if step == 0 and d == 0: ck("R7")
                    if os.environ.get("S32MASTER"):
                        k.op("dve", lambda e: e.scalar_tensor_tensor(out=S16[d][:], in0=S32[d][:], scalar=TOT[d][:, c:c + 1], in1=pds_ap,
                                                                     op0=ALU.mult, op1=ALU.add), R=[b_s32[d], b_tot[d], b_pds[d]], W=[b_s16[d]])
                        k.op("dve", lambda e: e.scalar_tensor_tensor(out=S32[d][:], in0=S32[d][:], scalar=TOT[d][:, c:c + 1], in1=pds_ap,
                                                                     op0=ALU.mult, op1=ALU.add), R=[b_s32[d], b_tot[d], b_pds[d]], W=[b_s32[d]])
                    else:
                        sn = S16b[d][(step + 1) % 2]
                        so = S16b[d][step % 2]
                        k.op("dve", lambda e: e.scalar_tensor_tensor(out=sn[:], in0=so[:], scalar=TOT[d][:, c:c + 1], in1=pds_ap,
                                                                     op0=ALU.mult, op1=ALU.add), R=[b_s16b[d][step % 2], b_tot[d], b_pds[d]], W=[b_s16b[d][(step + 1) % 2]])
                    if step == 0 and d == 0: ck("R8")
                    pass
                    if step == 0 and d == 0: ck("R9")
            ck("A5")
            for bi, (t0, n) in enumerate(BLKS):
                i = bi % 2
                O_ = Cb[i][:, 0:n]
                k.op("pool", lambda e: e.tensor_tensor(out=O_, in0=OD[0][:, t0:t0 + n], in1=OD[1][:, t0:t0 + n], op=ALU.add),
                     R=[b_od[0], b_od[1]], W=[bC[i]])
                k.op("act", lambda e: e.activation(out=sqb[i][:, 0:n], in_=O_, func=AF.Square), R=[bC[i]], W=[b_sq[i]])
                pb = pA.next()
                k.op("pe", lambda e: e.matmul(bank(pb)[:, 0:n], lhsT=onesm[:], rhs=sqb[i][:, 0:n], start=True, stop=True),
                     R=[b_sq[i], b_cst], W=[bbank[pb]])
                R_ = Db[i][:, 0:n]
                k.op("act", lambda e: e.activation(out=R_, in_=bank(pb)[:, 0:n], func=AF.Ln, bias=RMS_EPS, scale=1.0), R=[bbank[pb]], W=[bD[i]])
                k.op("act", lambda e: e.activation(out=R_, in_=R_, func=AF.Exp, scale=-0.5), R=[bD[i]], W=[bD[i]])
                k.op("dve", lambda e: e.tensor_tensor(out=O_, in0=O_, in1=R_, op=ALU.mult), R=[bC[i], bD[i]], W=[bC[i]])
                k.op("dve", lambda e: e.scalar_tensor_tensor(out=MIX[:, h, t0:t0 + n], in0=O_, scalar=ang[:, l:l + 1], in1=GS[:, t0:t0 + n],
                                                             op0=ALU.mult, op1=ALU.mult), R=[bC[i], b_ang, b_gs], W=[b_mix[h]])
        SA.close()
        dbg_dump("mixA", MIX[:, 0:4, :].rearrange("p c t -> p (c t)"), b_mix[0:4], [128, 4 * T], dt=BF16)
        ck("A")

        SB = Scope()
        ropeS = SB.sb("rope_sb", [128, 2 * SEQ])
        b_rope = Buf()
        k.dma("sp", ropeS[:], rope_in[:, :], W=[b_rope])
        HB = []
        for par in range(2):
            HB.append(dict(
                Wq=SB.sb(f"Wq{par}", [128, 8, 3, 128], BF16), b_Wq=Buf(),
                Qp=SB.sb(f"Qp{par}", [128, T], BF16), Kp=SB.sb(f"Kp{par}", [128, T], BF16),
                Qr=SB.sb(f"Qr{par}", [128, SEQ], BF16), Kr=SB.sb(f"Kr{par}", [128, SEQ], BF16),
                b_Qp=Buf(), b_Kp=Buf(), b_Qr=Buf(), b_Kr=Buf(),
                Va=SB.sb(f"Va{par}", [128, NT, 128], BF16), Vb=SB.sb(f"Vb{par}", [128, NT - 1, 128], BF16),
                b_Va=Buf(), b_Vb=Buf(),
                Hh=SB.sb(f"Hh{par}", [128, 15, 64]), b_Hh=Buf(),
                Tb=SB.sb(f"Tb{par}", [128, 960], BF16), b_Tb=Buf()))
        rp_sb = SB.sb("rp_sb", [120, 31])
        b_rp = Buf()
        rt1 = [SB.sb(f"rt1{i}", [128, 512]) for i in range(2)]
        rt2 = [SB.sb(f"rt2{i}", [128, 512]) for i in range(2)]
        b_rt1 = [Buf(), Buf()]
        b_rt2 = [Buf(), Buf()]
        Pt_ = [SB.sb(f"Pt{i}", [128, 768], BF16) for i in range(3)]
        PTs = [SB.sb(f"PTs{i}", [128, 768], BF16) for i in range(3)]
        b_P = [Buf(), Buf(), Buf()]
        b_PTs = [Buf(), Buf(), Buf()]
        stt_ = [SB.sb(f"stt{i}", [128, 4]) for i in range(3)]
        b_st = [Buf(), Buf(), Buf()]
        Dg = [SB.sb(f"Dg{i}", [128, 128], BF16) for i in range(3)]
        b_Dg = [Buf(), Buf(), Buf()]
        pB = Rot([6, 7])
        k.dma("sp", rp_sb[:], rpb[l].rearrange("h r c -> (h r) c"), W=[b_rp])
        k.dma("sp", rpbpad[:, 48:79], rp_sb[:], R=[b_rp], W=[b_rpbpad])
        rope_ctr = [0]

        def setup_gen(hp):
            hb = HB[hp % 2]
            Wq, b_Wq, Qp, Kp, Qr, Kr = hb["Wq"], hb["b_Wq"], hb["Qp"], hb["Kp"], hb["Qr"], hb["Kr"]
            b_Qp, b_Kp, b_Qr, b_Kr = hb["b_Qp"], hb["b_Kp"], hb["b_Qr"], hb["b_Kr"]
            Va, Vb, b_Va, b_Vb, Hh, b_Hh, Tb, b_Tb = hb["Va"], hb["Vb"], hb["b_Va"], hb["b_Vb"], hb["Hh"], hb["b_Hh"], hb["Tb"], hb["b_Tb"]
            for fi, c0 in enumerate([B_Q, B_K, B_V]):
                k.dma("pool", Wq[:, :, fi, :], w_in[l, :, c0 + hp * 128:c0 + hp * 128 + 128].rearrange("(ch p) c -> p ch c", p=128), W=[b_Wq])
            for hh in range(2):
                src = bass.AP(tensor=rpbpad.tensor, offset=((2 * hp + hh) * 15) * 128, ap=[[1, 64], [128, 15], [1, 64]])
                k.dma("sp", Hh[hh * 64:(hh + 1) * 64, :, :], src, R=[b_rpbpad], W=[b_Hh])
            Hf = Hh[:].rearrange("p a b -> p (a b)")
            for (c0, ncol) in ((0, 512), (512, 448)):
                pb = pB.next()
                k.op("pe", lambda e: e.matmul(bank(pb)[:, 0:ncol], lhsT=J2, rhs=Hf[:, c0:c0 + ncol], start=True, stop=True),
                     R=[b_cst, b_Hh], W=[bbank[pb]])
                ndr = ncol // 64
                k.op("dve", lambda e: e.tensor_tensor(out=Tb[:, c0:c0 + ncol].rearrange("p (a b) -> p a b", b=64),
                                                      in0=bank(pb)[:, 0:ncol].rearrange("p (a b) -> p a b", b=64),
                                                      in1=winm.unsqueeze(1).broadcast_to([128, ndr, 64]), op=ALU.add),
                     R=[bbank[pb], b_cst], W=[b_Tb])
                yield
            for bi, (t0, n) in enumerate(BLKS):
                pb = pB.next()
                proj_fm(pb, Wq, b_Wq, (0, slice(None)), t0, n)
                k.op("act", lambda e: e.activation(out=Qp[:, t0:t0 + n], in_=bank(pb)[:, 0:n], func=AF.Copy, scale=0.125), R=[bbank[pb]], W=[b_Qp])
                pb = pB.next()
                proj_fm(pb, Wq, b_Wq, (1, slice(None)), t0, n)
                k.op("dve", lambda e: e.tensor_copy(out=Kp[:, t0:t0 + n], in_=bank(pb)[:, 0:n]), R=[bbank[pb]], W=[b_Kp])
                yield
            for lbk in range(4):
                t0 = CTX + lbk * 512
                tl = lbk * 512
                for (src_, bsrc, dst_, bdst) in ((Qp, b_Qp, Qr, b_Qr), (Kp, b_Kp, Kr, b_Kr)):
                    i = rope_ctr[0] % 2
                    rope_ctr[0] += 1
                    pb = pB.next()
                    k.op("pe", lambda e: e.matmul(bank(pb)[:, :], lhsT=Pm16[:], rhs=src_[:, t0:t0 + 512], start=True, stop=True),
                         R=[b_cst, bsrc], W=[bbank[pb]])
                    k.op("pool", lambda e: e.tensor_tensor(out=rt1[i][:], in0=src_[:, t0:t0 + 512], in1=ropeS[:, tl:tl + 512], op=ALU.mult),
                         R=[bsrc, b_rope], W=[b_rt1[i]])
                    k.op("dve", lambda e: e.tensor_tensor(out=rt2[i][:], in0=bank(pb)[:, :], in1=ropeS[:, SEQ + tl:SEQ + tl + 512], op=ALU.mult),
                         R=[bbank[pb], b_rope], W=[b_rt2[i]])
                    k.op("pool", lambda e: e.tensor_tensor(out=dst_[:, tl:tl + 512], in0=rt1[i][:], in1=rt2[i][:], op=ALU.add),
                         R=[b_rt1[i], b_rt2[i]], W=[bdst])
                    yield
            for (Vt, bV, ntile, off) in ((Va, b_Va, NT, 0), (Vb, b_Vb, NT - 1, 64)):
                j0 = 0
                while j0 < ntile:
                    g = min(4, ntile - j0)
                    pb = pB.next()
                    for q in range(g):
                        tk = off + (j0 + q) * 128
                        for ch in range(8):
                            k.op("pe", lambda e: e.matmul(bank(pb)[:, q * 128:(q + 1) * 128], lhsT=XT[:, ch, tk:tk + 128], rhs=Wq[:, ch, 2, :],
                                                          start=(ch == 0), stop=(ch == 7)), R=[b_Wq] + xt_bufs(tk, 128), W=[bbank[pb]])
                    k.op("act", lambda e: e.activation(out=Vt[:, j0:j0 + g, :].rearrange("p a b -> p (a b)"), in_=bank(pb)[:, 0:g * 128], func=AF.Copy),
                         R=[bbank[pb]], W=[bV])
                    j0 += g
                    yield

        gens = [setup_gen(hp_) for hp_ in range(4)]
        for _ in gens[0]:
            pass
        for hp in range(4):
            hb = HB[hp % 2]
            Qp, Kp, Qr, Kr = hb["Qp"], hb["Kp"], hb["Qr"], hb["Kr"]
            b_Qp, b_Kp, b_Qr, b_Kr = hb["b_Qp"], hb["b_Kp"], hb["b_Qr"], hb["b_Kr"]
            Va, Vb, b_Va, b_Vb, Tb, b_Tb = hb["Va"], hb["Vb"], hb["b_Va"], hb["b_Vb"], hb["Tb"], hb["b_Tb"]
            gnext = gens[hp + 1] if hp + 1 < 4 else None
            tiles = [("ctx", qb) for qb in range(4)] + [("lat", r) for r in range(32)]
            NTL = len(tiles)
            tst = [None] * NTL

            def ph_a(t):
                kind, idx = tiles[t]
                i3 = t % 3
                S2 = ps2[i3]
                bS = [bbank[2 * i3], bbank[2 * i3 + 1]]
                if kind == "lat":
                    r = idx
                    rs = min(max(r - 4, 0), 24)
                    dr0 = rs - r + 7
                    qt = CTX + r * 64
                    nk = 768
                    k.op("pe", lambda e: e.matmul(S2[:, 0:512], lhsT=ident16[:], rhs=Tb[:, dr0 * 64:dr0 * 64 + 512], start=True, stop=False),
                         R=[b_Tb, b_cst], W=[bS[0]])
                    for hh in range(2):
                        hs = slice(hh * 64, (hh + 1) * 64)
                        k.op("pe", lambda e: e.matmul(S2[hs, 0:512], lhsT=Qr[hs, r * 64:(r + 1) * 64], rhs=Kr[hs, rs * 64:rs * 64 + 512],
                                                      start=False, stop=True), R=[b_Qr, b_Kr], W=[bS[0]])
                    for hh in range(2):
                        hs = slice(hh * 64, (hh + 1) * 64)
                        k.op("pe", lambda e: e.matmul(S2[hs, 512:768], lhsT=Qp[hs, qt:qt + 64], rhs=Kp[hs, 0:CTX], start=True, stop=True),
                             R=[b_Qp, b_Kp], W=[bS[1]])
                    vts = []
                    for b in range(4):
                        if rs % 2 == 0:
                            vts.append((Va, 2 + rs // 2 + b, b_Va))
                        else:
                            vts.append((Vb, (3 + rs) // 2 + b, b_Vb))
                    vts += [(Va, 0, b_Va), (Va, 1, b_Va)]
                else:
                    qt = idx * 64
                    nk = 256
                    for hh in range(2):
                        hs = slice(hh * 64, (hh + 1) * 64)
                        k.op("pe", lambda e: e.matmul(S2[hs, 0:256], lhsT=Qp[hs, qt:qt + 64], rhs=Kp[hs, 0:CTX], start=True, stop=True),
                             R=[b_Qp, b_Kp], W=[bS[0]])
                    vts = [(Va, 0, b_Va), (Va, 1, b_Va)]
                tst[t] = (S2, bS, qt, nk, vts)

            def ph_b(t):
                S2, bS, qt, nk, vts = tst[t]
                i = t % 3
                bSr = bS if nk > 512 else bS[0:1]
                st_ = stt_[i]
                k.op("dve", lambda e: e.tensor_reduce(out=st_[:, 0:1], in_=S2[:, 0:nk], axis=AX.X, op=ALU.max), R=bSr, W=[b_st[i]])
                k.op("dve", lambda e: e.tensor_scalar(out=st_[:, 1:2], in0=st_[:, 0:1], scalar1=-1.0, scalar2=None, op0=ALU.mult), R=[b_st[i]], W=[b_st[i]])
                k.op("act", lambda e: e.activation(out=Pt_[i][:, 0:nk], in_=S2[:, 0:nk], func=AF.Exp, bias=st_[:, 1:2], scale=1.0, accum_out=st_[:, 2:3]),
                     R=bSr + [b_st[i]], W=[b_P[i], b_st[i]])
                k.op("dve", lambda e: e.reciprocal(out=st_[:, 3:4], in_=st_[:, 2:3]), R=[b_st[i]], W=[b_st[i]])
                k.op("dve", lambda e: e.tensor_scalar(out=Dg[i][:], in0=ident32, scalar1=st_[:, 3:4], scalar2=None, op0=ALU.mult), R=[b_st[i], b_cst], W=[b_Dg[i]])

            def ph_c(t):
                S2, bS, qt, nk, vts = tst[t]
                i = t % 3
                nb = nk // 128
                PT2 = ps2[3]
                for b in range(nb):
                    k.op("pe", lambda e: e.matmul(PT2[:, b * 128:(b + 1) * 128], lhsT=Pt_[i][:, b * 128:(b + 1) * 128], rhs=Dg[i][:], start=True, stop=True),
                         R=[b_P[i], b_Dg[i]], W=[bbank[6 + b // 4]])
                bPT = [bbank[6], bbank[7]] if nb > 4 else [bbank[6]]
                if t % 2 == 0:
                    k.op("act", lambda e: e.activation(out=PTs[i][:, 0:nk], in_=PT2[:, 0:nk], func=AF.Copy), R=bPT, W=[b_PTs[i]])
                else:
                    k.op("dve", lambda e: e.tensor_copy(out=PTs[i][:, 0:nk], in_=PT2[:, 0:nk]), R=bPT, W=[b_PTs[i]])

            def ph_d(t):
                S2, bS, qt, nk, vts = tst[t]
                i = t % 3
                nb = nk // 128
                for hh in range(2):
                    hs = slice(hh * 64, (hh + 1) * 64)
                    for b in range(nb):
                        Vt, vj, bV = vts[b]
                        k.op("pe", lambda e: e.matmul(S2[hs, 768:832], lhsT=Vt[:, vj, hs], rhs=PTs[i][:, b * 128 + hh * 64:b * 128 + hh * 64 + 64],
                                                      start=(b == 0), stop=(b == nb - 1)), R=[bV, b_PTs[i]], W=[bS[1]])
                k.op("act", lambda e: e.activation(out=MIX[:, 4 + hp, qt:qt + 64], in_=S2[:, 768:832], func=AF.Copy), R=[bS[1]], W=[b_mix[4 + hp]])

            for t in range(NTL + 2):
                if t < NTL:
                    ph_a(t)
                if gnext is not None:
                    next(gnext, None)
                if 1 <= t <= NTL:
                    ph_b(t - 1)
                    ph_c(t - 1)
                if t >= 2:
                    ph_d(t - 2)
            if gnext is not None:
                for _ in gnext:
                    pass
        SB.close()
        dbg_dump("mixB", MIX[:, 4:8, :].rearrange("p c t -> p (c t)"), b_mix[4:8], [128, 4 * T], dt=BF16)
        ck("B")

        SO = Scope()
        Wo = SO.sb("Wo", [128, 8, D], BF16)
        b_Wo = Buf()
        for hf in range(2):
            k.dma("pool", Wo[:, hf * 4:(hf + 1) * 4, :], w_out[l, hf * 512:(hf + 1) * 512, :].rearrange("(ch p) c -> p ch c", p=128), W=[b_Wo])
        gbc = SO.sb("gbc", [128, 2, D])
        lngb = SO.sb("lngb", [128, 2, D])
        b_bc = Buf()
        for j in range(2):
            k.dma("sp", gbc[:, j, :], bass.AP(tensor=modd.tensor, offset=(l * 2 + j) * 6 * D + 2 * D, ap=[[0, 128], [1, D]]), R=[b_modd_all[l]], W=[b_bc])
        k.dma("sp", lngb[:, 0, :], bass.AP(tensor=ln1_g.tensor, offset=l * D, ap=[[0, 128], [1, D]]), W=[b_bc])
        k.dma("sp", lngb[:, 1, :], bass.AP(tensor=ln1_b.tensor, offset=l * D, ap=[[0, 128], [1, D]]), W=[b_bc])
        xr = [SO.sb(f"xr{i}", [128, D]) for i in range(3)]
        zt = [SO.sb(f"zt{i}", [128, D]) for i in range(3)]
        x1t = [SO.sb(f"x1t{i}", [128, D]) for i in range(3)]
        tf32 = [SO.sb(f"tf32{i}", [128, 8, 128]) for i in range(3)]
        b_xr, b_zt, b_x1t, b_tf = [Buf(), Buf(), Buf()], [Buf(), Buf(), Buf()], [Buf(), Buf(), Buf()], [Buf(), Buf(), Buf()]
        lnst = [SO.sb(f"lnst{i}", [128, 2, 6]) for i in range(3)]
        lnmv = [SO.sb(f"lnmv{i}", [128, 4]) for i in range(3)]
        b_ln = [Buf(), Buf(), Buf()]
        rt = [SO.sb(f"rt{i}", [128, 8, 16]) for i in range(3)]
        rcmp = [SO.sb(f"rcmp{i}", [128, 4, 4, 4]) for i in range(3)]
        b_rt = [Buf(), Buf(), Buf()]
        cTs = [SO.sb(f"cTs{i}", [16, 128]) for i in range(3)]
        b_cTs = [Buf(), Buf(), Buf()]
        pY = Rot([2, 4])

        def layer_norm_tile(st, mv, bln, z, bz, xo, bxo, g_ap, b_ap, bgb):
            for hf in range(2):
                k.op("dve", lambda e: e.bn_stats(out=st[:, hf, :], in_=z[:, hf * 512:(hf + 1) * 512]), R=[bz], W=[bln])
            k.op("dve", lambda e: e.bn_aggr(out=mv[:, 0:2], in_=st[:].rearrange("p a b -> p (a b)")), R=[bln], W=[bln])
            k.op("act", lambda e: e.activation(out=mv[:, 2:3], in_=mv[:, 1:2], func=AF.Ln, bias=LN_EPS, scale=1.0), R=[bln], W=[bln])
            k.op("act", lambda e: e.activation(out=mv[:, 2:3], in_=mv[:, 2:3], func=AF.Exp, scale=-0.5), R=[bln], W=[bln])
            k.op("dve", lambda e: e.tensor_scalar(out=mv[:, 3:4], in0=mv[:, 0:1], scalar1=mv[:, 2:3], scalar2=-1.0, op0=ALU.mult, op1=ALU.mult),
                 R=[bln], W=[bln])
            k.op("act", lambda e: e.activation(out=xo[:], in_=z[:], func=AF.Identity, scale=mv[:, 2:3], bias=mv[:, 3:4]), R=[bz, bln], W=[bxo])
            k.op("dve", lambda e: e.tensor_tensor(out=xo[:], in0=xo[:], in1=g_ap, op=ALU.mult), R=[bxo, bgb], W=[bxo])
            k.op("pool", lambda e: e.tensor_tensor(out=xo[:], in0=xo[:], in1=b_ap, op=ALU.add), R=[bxo, bgb], W=[bxo])

        def stageO_p1(tt):
            i = tt % 3
            j = 1 if tt < 2 else 0
            pb0 = pY.next()
            if l == 0:
                src = ctx_in[tt * 128:(tt + 1) * 128, :] if tt < 2 else x_in[(tt - 2) * 128:(tt - 1) * 128, :]
                k.dma("sp", xr[i][:], src, W=[b_xr[i]])
            else:
                k.dma("sp", xr[i][:], xs[tt * 128:(tt + 1) * 128, :], R=[b_xs[tt]], W=[b_xr[i]])
            for hf in range(2):
                pb = pb0 + hf
                for ch in range(8):
                    k.op("pe", lambda e: e.matmul(bank(pb)[:, :], lhsT=MIX[:, ch, tt * 128:(tt + 1) * 128], rhs=Wo[:, ch, hf * 512:(hf + 1) * 512],
                                                  start=(ch == 0), stop=(ch == 7)), R=[b_mix[ch], b_Wo], W=[bbank[pb]])
                k.op("dve", lambda e: e.tensor_tensor(out=zt[i][:, hf * 512:(hf + 1) * 512], in0=bank(pb)[:, :], in1=gbc[:, j, hf * 512:(hf + 1) * 512], op=ALU.mult),
                     R=[bbank[pb], b_bc], W=[b_zt[i]])
            k.op("dve", lambda e: e.scalar_tensor_tensor(out=zt[i][:], in0=xr[i][:], scalar=ALPHA, in1=zt[i][:], op0=ALU.mult, op1=ALU.add),
                 R=[b_xr[i], b_zt[i]], W=[b_zt[i]])
            yield
            layer_norm_tile(lnst[i], lnmv[i], b_ln[i], zt[i], b_zt[i], x1t[i], b_x1t[i], lngb[:, 0, :], lngb[:, 1, :], b_bc)
            k.dma("sp", xs[tt * 128:(tt + 1) * 128, :], x1t[i][:], R=[b_x1t[i]], W=[b_xs[tt]])

        def stageO_p2(tt):
            i = tt % 3
            j = 1 if tt < 2 else 0
            transpose_tile(x1t[i], b_x1t[i], tt, l, 4, 3, tf32=tf32[i], b_tf=b_tf[i])
            yield
            for ch in range(8):
                k.op("pe", lambda e: e.matmul(bank(6)[:, 0:16], lhsT=tf32[i][:, ch, :], rhs=wr32[:, ch, :], start=(ch == 0), stop=(ch == 7)),
                     R=[b_tf[i], b_wr], W=[bbank[6]])
            yield
            R_ = rt[i]
            e1, aff, sel, m2, gs4, gm4, asel = R_[:, 0, :], R_[:, 1, :], R_[:, 2, :], R_[:, 3, :], R_[:, 4, 0:4], R_[:, 4, 4:8], R_[:, 5, :]
            gmx, den = R_[:, 4, 8:9], R_[:, 4, 9:10]
            k.op("act", lambda e: e.activation(out=e1, in_=bank(6)[:, 0:16], func=AF.Exp, scale=-1.0), R=[bbank[6]], W=[b_rt[i]])
            k.op("dve", lambda e: e.tensor_scalar(out=e1, in0=e1, scalar1=1.0, scalar2=None, op0=ALU.add), R=[b_rt[i]], W=[b_rt[i]])
            k.op("dve", lambda e: e.reciprocal(out=aff, in_=e1), R=[b_rt[i]], W=[b_rt[i]])
            k.op("dve", lambda e: e.tensor_tensor(out=sel, in0=aff, in1=brb[:], op=ALU.add), R=[b_rt[i], b_brb], W=[b_rt[i]])
            selv = sel.rearrange("p (g i) -> p g i", g=4)
            k.op("dve", lambda e: e.tensor_tensor(out=rcmp[i][:], in0=selv.unsqueeze(2).broadcast_to([128, 4, 4, 4]),
                                                  in1=selv.unsqueeze(3).broadcast_to([128, 4, 4, 4]), op=ALU.is_gt), R=[b_rt[i]], W=[b_rt[i]])
            m2v = m2.rearrange("p (g i) -> p g i", g=4)
            k.op("dve", lambda e: e.tensor_reduce(out=m2v, in_=rcmp[i][:], axis=AX.X, op=ALU.add), R=[b_rt[i]], W=[b_rt[i]])
            k.op("dve", lambda e: e.tensor_scalar(out=m2, in0=m2, scalar1=2.0, scalar2=None, op0=ALU.is_lt), R=[b_rt[i]], W=[b_rt[i]])
            k.op("dve", lambda e: e.tensor_tensor(out=asel, in0=m2, in1=sel, op=ALU.mult), R=[b_rt[i]], W=[b_rt[i]])
            k.op("dve", lambda e: e.tensor_reduce(out=gs4, in_=asel.rearrange("p (g i) -> p g i", g=4), axis=AX.X, op=ALU.add), R=[b_rt[i]], W=[b_rt[i]])
            k.op("dve", lambda e: e.tensor_reduce(out=gmx, in_=gs4, axis=AX.X, op=ALU.max), R=[b_rt[i]], W=[b_rt[i]])
            k.op("dve", lambda e: e.tensor_scalar(out=gm4, in0=gs4, scalar1=gmx, scalar2=None, op0=ALU.is_ge), R=[b_rt[i]], W=[b_rt[i]])
            k.op("dve", lambda e: e.tensor_tensor(out=m2v, in0=m2v, in1=gm4.unsqueeze(2).broadcast_to([128, 4, 4]), op=ALU.mult), R=[b_rt[i]], W=[b_rt[i]])
            k.op("dve", lambda e: e.tensor_tensor(out=asel, in0=m2, in1=aff, op=ALU.mult), R=[b_rt[i]], W=[b_rt[i]])
            k.op("dve", lambda e: e.tensor_reduce(out=den, in_=asel, axis=AX.X, op=ALU.add), R=[b_rt[i]], W=[b_rt[i]])
            k.op("dve", lambda e: e.reciprocal(out=den, in_=den), R=[b_rt[i]], W=[b_rt[i]])
            k.op("dve", lambda e: e.tensor_scalar(out=asel, in0=asel, scalar1=den, scalar2=None, op0=ALU.mult), R=[b_rt[i]], W=[b_rt[i]])
            k.op("pe", lambda e: e.transpose(out=bank(7)[0:16, 0:128], in_=asel, identity=ident32), R=[b_rt[i], b_cst], W=[bbank[7]])
            k.op("act", lambda e: e.activation(out=cTs[i][:], in_=bank(7)[0:16, 0:128], func=AF.Copy), R=[bbank[7]], W=[b_cTs[i]])
            k.dma("sp", combT_d[:, tt * 128:(tt + 1) * 128], cTs[i][:], R=[b_cTs[i]], W=[b_combT])
            if tt == 3:
                dbg_dump("x1t3", x1t[i][:], b_x1t[i], [128, D])
                dbg_dump("comb3", asel, b_rt[i], [128, 16])
        tt0 = 2 if last else 0
        for tt in range(tt0, NT + 1):
            g1 = stageO_p1(tt) if tt < NT else None
            g2 = stageO_p2(tt - 1) if tt > tt0 else None
            if g2:
                next(g2)
            if g1:
                next(g1)
            if g2:
                next(g2)
            if g1:
                next(g1, None)
            if g2:
                next(g2, None)
        SO.close()
        dbg_dump("xt2", XT[:, :, :].rearrange("p c t -> p (c t)"), bXT, [128, 8 * T], dt=BF16)
        ck("O")
        MS.close()

        SML = Scope()
        ACC = SML.sb("ACC", [128, NT, D])
        b_acc = [Buf(f"acc{i}") for i in range(NT)]
        SM = Scope()
        CT = SM.sb("CT", [16, T])
        b_CT = Buf()
        k.dma("sp", CT[:], combT_d[:, :], R=[b_combT], W=[b_CT])
        CB = SM.sb("CB", [128, T])
        b_CB = Buf()
        wg = [SM.sb(f"wg{i}", [128, 8, 128], BF16) for i in range(2)]
        wu = [SM.sb(f"wu{i}", [128, 8, 128], BF16) for i in range(2)]
        b_wg, b_wu = [Buf(), Buf()], [Buf(), Buf()]
        wd = SM.sb("wd", [128, 4, D], BF16)
        b_wd = Buf()
        HID = SM.sb("HID", [128, 4, T], BF16)
        b_hid = [Buf() for _ in range(4)]
        sgt = [SM.sb(f"sgt{i}", [128, 512]) for i in range(2)]
        tmt = [SM.sb(f"tmt{i}", [128, 512]) for i in range(2)]
        b_sg, b_tm = [Buf(), Buf()], [Buf(), Buf()]
        pM = Rot([0, 1, 2, 3, 4, 5, 6, 7])
        wi = 0
        ti = 0
        NEX_ = int(os.environ.get("NEXPS", NEXP))
        MBLKS = [(256, 512), (768, 512), (1280, 512), (1792, 512)] if last else BLKS
        mt0 = 2 if last else 0

        def load_gu(ex_, fc_, wi_):
            k.dma("pool", wg[wi_][:], w_gate[l, ex_, :, fc_ * 128:(fc_ + 1) * 128].rearrange("(ch p) c -> p ch c", p=128), W=[b_wg[wi_]])
            k.dma("pool", wu[wi_][:], w_up[l, ex_, :, fc_ * 128:(fc_ + 1) * 128].rearrange("(ch p) c -> p ch c", p=128), W=[b_wu[wi_]])

        for ex in range(NEX_):
            for bi, (t0, n) in enumerate(MBLKS):
                pb = pM.next()
                k.op("pe", lambda e: e.matmul(bank(pb)[:, 0:n], lhsT=ident32[0:16, ex:ex + 1].broadcast_to([16, 128]), rhs=CT[:, t0:t0 + n], start=True, stop=True),
                     R=[b_cst, b_CT], W=[bbank[pb]])
                k.op("act", lambda e: e.activation(out=CB[:, t0:t0 + n], in_=bank(pb)[:, 0:n], func=AF.Copy), R=[bbank[pb]], W=[b_CB])
            for fc in range(4):
                w_i = wi % 2
                wi += 1
                if ex == 0 and fc == 0:
                    load_gu(ex, fc, w_i)
                nfc, nex = (fc + 1, ex) if fc < 3 else (0, ex + 1)
                if nex < NEX_:
                    load_gu(nex, nfc, 1 - w_i)
                if fc == 1:
                    k.dma("pool", wd[:], w_down[l, ex, :, :].rearrange("(fc p) c -> p fc c", p=128), W=[b_wd])
                for bi, (t0, n) in enumerate(MBLKS):
                    i = ti % 2
                    ti += 1
                    pg = pM.next()
                    for ch in range(8):
                        k.op("pe", lambda e: e.matmul(bank(pg)[:, 0:n], lhsT=wg[w_i][:, ch, :], rhs=XT[:, ch, t0:t0 + n], start=(ch == 0), stop=(ch == 7)),
                             R=[b_wg[w_i]] + xt_bufs(t0, n), W=[bbank[pg]])
                    pu = pM.next()
                    for ch in range(8):
                        k.op("pe", lambda e: e.matmul(bank(pu)[:, 0:n], lhsT=wu[w_i][:, ch, :], rhs=XT[:, ch, t0:t0 + n], start=(ch == 0), stop=(ch == 7)),
                             R=[b_wu[w_i]] + xt_bufs(t0, n), W=[bbank[pu]])
                    k.op("act", lambda e: e.activation(out=sgt[i][:, 0:n], in_=bank(pg)[:, 0:n], func=AF.Silu), R=[bbank[pg]], W=[b_sg[i]])
                    k.op("dve", lambda e: e.tensor_tensor(out=tmt[i][:, 0:n], in0=bank(pu)[:, 0:n], in1=CB[:, t0:t0 + n], op=ALU.mult),
                         R=[bbank[pu], b_CB], W=[b_tm[i]])
                    k.op("pool", lambda e: e.tensor_tensor(out=HID[:, fc, t0:t0 + n], in0=sgt[i][:, 0:n], in1=tmt[i][:, 0:n], op=ALU.mult),
                         R=[b_sg[i], b_tm[i]], W=[b_hid[fc]])
            for tt in range(mt0, NT):
                for hf in range(2):
                    pb = pM.next()
                    for fc in range(4):
                        k.op("pe", lambda e: e.matmul(bank(pb)[:, :], lhsT=HID[:, fc, tt * 128:(tt + 1) * 128], rhs=wd[:, fc, hf * 512:(hf + 1) * 512],
                                                      start=(fc == 0), stop=(fc == 3)), R=[b_hid[fc], b_wd], W=[bbank[pb]])
                    dst = ACC[:, tt, hf * 512:(hf + 1) * 512]
                    if ex == 0:
                        k.op("act", lambda e: e.activation(out=dst, in_=bank(pb)[:, :], func=AF.Copy), R=[bbank[pb]], W=[b_acc[tt]])
                    else:
                        k.op("dve", lambda e: e.tensor_tensor(out=dst, in0=bank(pb)[:, :], in1=dst, op=ALU.add), R=[bbank[pb], b_acc[tt]], W=[b_acc[tt]])
        SM.close()
        dbg_dump("acc3", ACC[:, 3, :], b_acc[3], [128, D])
        ck("M")

        SO = Scope()
        gbc = SO.sb("gbc2", [128, 2, D])
        lngb = SO.sb("lngb2", [128, 2, D])
        b_bc = Buf()
        for j in range(2):
            k.dma("sp", gbc[:, j, :], bass.AP(tensor=modd.tensor, offset=(l * 2 + j) * 6 * D + 5 * D, ap=[[0, 128], [1, D]]), R=[b_modd_all[l]], W=[b_bc])
        k.dma("sp", lngb[:, 0, :], bass.AP(tensor=ln2_g.tensor, offset=l * D, ap=[[0, 128], [1, D]]), W=[b_bc])
        k.dma("sp", lngb[:, 1, :], bass.AP(tensor=ln2_b.tensor, offset=l * D, ap=[[0, 128], [1, D]]), W=[b_bc])
        xr = [SO.sb(f"xr2{i}", [128, D]) for i in range(3)]
        zt = [SO.sb(f"zt2{i}", [128, D]) for i in range(3)]
        x1t = [SO.sb(f"x2t{i}", [128, D]) for i in range(3)]
        b_xr, b_zt, b_x1t = [Buf(), Buf(), Buf()], [Buf(), Buf(), Buf()], [Buf(), Buf(), Buf()]
        lnst = [SO.sb(f"lnst2{i}", [128, 2, 6]) for i in range(3)]
        lnmv = [SO.sb(f"lnmv2{i}", [128, 4]) for i in range(3)]
        b_ln = [Buf(), Buf(), Buf()]
        pendT = [None]
        for tt in range(2 if last else 0, NT):
            i = tt % 3
            j = 1 if tt < 2 else 0
            k.dma("sp", xr[i][:], xs[tt * 128:(tt + 1) * 128, :], R=[b_xs[tt]], W=[b_xr[i]])
            k.op("dve", lambda e: e.tensor_tensor(out=zt[i][:], in0=ACC[:, tt, :], in1=gbc[:, j, :], op=ALU.mult), R=[b_acc[tt], b_bc], W=[b_zt[i]])
            k.op("dve", lambda e: e.scalar_tensor_tensor(out=zt[i][:], in0=xr[i][:], scalar=ALPHA, in1=zt[i][:], op0=ALU.mult, op1=ALU.add),
                 R=[b_xr[i], b_zt[i]], W=[b_zt[i]])
            layer_norm_tile(lnst[i], lnmv[i], b_ln[i], zt[i], b_zt[i], x1t[i], b_x1t[i], lngb[:, 0, :], lngb[:, 1, :], b_bc)
            if last:
                bo = Buf()
                k.dma("sp", out[(tt - 2) * 128:(tt - 1) * 128, :], x1t[i][:], R=[b_x1t[i]], W=[bo])
            else:
                k.dma("sp", xs[tt * 128:(tt + 1) * 128, :], x1t[i][:], R=[b_x1t[i]], W=[b_xs[tt]])
                if l + 1 < nlayers:
                    if pendT[0] is not None:
                        pt_, pi_ = pendT[0]
                        transpose_tile(x1t[pi_], b_x1t[pi_], pt_, l + 1, 1, 0)
                    pendT[0] = (tt, i)
            if tt == 3:
                dbg_dump("x2t3_%d" % l, x1t[i][:], b_x1t[i], [128, D])
        if pendT[0] is not None:
            pt_, pi_ = pendT[0]
            transpose_tile(x1t[pi_], b_x1t[pi_], pt_, l + 1, 1, 0)
        SO.close()
        SML.close()
        ck("L%d" % l)


def make_in_map(inp, b, cst, rope):
    f = lambda a: np.ascontiguousarray(np.asarray(a, dtype=np.float32))
    m = {
        "x": f(inp["x"][b]), "ctx": f(inp["ctx"][b]), "c": f(inp["c"][b]).reshape(1, D),
        "c_ctx": f(inp["c_ctx"]).reshape(1, D), "b_router": f(inp["b_router"]).reshape(1, NEXP),
        "cst": cst, "rope": rope,
    }
    for n in ("w_ada", "b_ada", "w_in", "lb_logits", "a_norm_g", "rpb", "w_out", "ln1_g", "ln1_b", "w_router",
              "w_gate", "w_up", "w_down", "ln2_g", "ln2_b"):
        m[n] = f(inp[n])
    return m


_NC = None


def kernel(**inputs):
    global _NC
    if _NC is None:
        _NC = build()[0]
    nc = _NC
    cst, rope = host_consts()
    nb = np.asarray(inputs["x"]).shape[0]
    shared = make_in_map(inputs, 0, cst, rope)
    in_maps = []
    for b in range(nb):
        m = dict(shared)
        m["x"] = np.ascontiguousarray(np.asarray(inputs["x"][b], dtype=np.float32))
        m["ctx"] = np.ascontiguousarray(np.asarray(inputs["ctx"][b], dtype=np.float32))
        m["c"] = np.ascontiguousarray(np.asarray(inputs["c"][b], dtype=np.float32)).reshape(1, D)
        in_maps.append(m)
    res = run_bass_kernel_spmd(nc, in_maps, core_ids=list(range(nb)))
    return np.stack([np.asarray(r["out"], dtype=np.float32) for r in res.results], axis=0)
```

```python
import os
import numpy as np
import concourse.bass as bass
import concourse.mybir as mybir
from concourse.bass_utils import run_bass_kernel_spmd

F32 = mybir.dt.float32
BF16 = mybir.dt.bfloat16
AF = mybir.ActivationFunctionType
ALU = mybir.AluOpType
AX = mybir.AxisListType

D = 1024
SEQ = 2048
CTX = 256
T = SEQ + CTX
NT = T // 128
NCH = T // 64
DEPTH = 4
A_Q, A_FF, A_FB, A_I, A_G, B_Q, B_K, B_V = 0, 512, 1024, 1536, 2048, 2560, 3072, 3584
ALPHA = (2 * DEPTH) ** 0.25
LN_EPS = 1e-5
RMS_EPS = 1e-6
NEXP = 16


class Buf:
    __slots__ = ("name", "writer", "readers")

    def __init__(self, name=""):
        self.name = name
        self.writer = None
        self.readers = []


class K:
    def __init__(self, nc):
        self.nc = nc
        self.h = {"pe": nc.tensor, "act": nc.scalar, "dve": nc.vector, "pool": nc.gpsimd, "sp": nc.sync}
        self.sem = {}
        self.cnt = {}
        self.seen = {}
        for e in self.h:
            self.sem[e] = nc.semaphore("s_" + e).__enter__()
            self.cnt[e] = 0
            self.seen[e] = {}
        self.ND = 8
        self.dsem = {}
        self.dcnt = {}
        self.dnext = {}
        for q in ("sp", "pool"):
            self.dsem[q] = [nc.semaphore(f"d_{q}{i}").__enter__() for i in range(self.ND)]
            self.dcnt[q] = [0] * self.ND
            self.dnext[q] = 0
        self.ninst = 0

    def _wait(self, e, ev):
        key, sem, val = ev
        if self.seen[e].get(key, 0) >= val:
            return
        self.h[e].wait_ge(sem, val)
        self.seen[e][key] = val

    def _deps(self, e, R, W):
        mx = {}
        for b in R:
            ev = b.writer
            if ev is not None and (ev[0] not in mx or mx[ev[0]][2] < ev[2]):
                mx[ev[0]] = ev
        for b in W:
            ev = b.writer
            if ev is not None and (ev[0] not in mx or mx[ev[0]][2] < ev[2]):
                mx[ev[0]] = ev
            for ev in b.readers:
                if ev[0] not in mx or mx[ev[0]][2] < ev[2]:
                    mx[ev[0]] = ev
        for key, ev in mx.items():
            if e == "pe" and key == "pe":
                continue
            self._wait(e, ev)

    def _mark(self, ev, R, W):
        for b in W:
            b.writer = ev
            b.readers = []
        for b in R:
            rs = b.readers
            for i, r in enumerate(rs):
                if r[0] == ev[0]:
                    rs[i] = ev
                    break
            else:
                rs.append(ev)

    def op(self, e, fn, R=(), W=()):
        self._deps(e, R, W)
        inst = fn(self.h[e])
        self.cnt[e] += 1
        inst.then_inc(self.sem[e], 1)
        self._mark((e, self.sem[e], self.cnt[e]), R, W)
        self.ninst += 1
        return inst

    def dma(self, q, out, in_, R=(), W=(), **kw):
        i = self.dnext[q]
        self.dnext[q] = (i + 1) % self.ND
        key = f"d_{q}{i}"
        sem = self.dsem[q][i]
        if self.dcnt[q][i] > 0:
            self._wait(q, (key, sem, self.dcnt[q][i]))
        self._deps(q, R, W)
        inst = self.h[q].dma_start(out=out, in_=in_, **kw)
        self.dcnt[q][i] += 16
        inst.then_inc(sem, 16)
        self._mark((key, sem, self.dcnt[q][i]), R, W)
        self.ninst += 1
        return inst

    def barrier(self):
        evs = [(e, self.sem[e], self.cnt[e]) for e in self.h if self.cnt[e] > 0]
        for q in self.dsem:
            for i in range(self.ND):
                if self.dcnt[q][i] > 0:
                    evs.append((f"d_{q}{i}", self.dsem[q][i], self.dcnt[q][i]))
        for e in self.h:
            for ev in evs:
                if not (e == "pe" and ev[0] == "pe"):
                    self._wait(e, ev)


class Rot:
    def __init__(self, items):
        self.items = items
        self.i = 0

    def next(self):
        it = self.items[self.i]
        self.i = (self.i + 1) % len(self.items)
        return it


def host_consts():
    cst = np.zeros((128, 704), np.float32)
    cst[:, 0:128] = np.eye(128, dtype=np.float32)
    J = np.zeros((128, 128), np.float32)
    P = np.zeros((128, 128), np.float32)
    for m in range(128):
        blk, q = divmod(m, 64)
        J[blk * 64 + (63 - q), m] = 1.0
        d = m % 64
        partner = d + 16 if (d % 32) < 16 else d - 16
        P[blk * 64 + partner, m] = 1.0
    cst[:, 128:256] = J
    cst[:, 256:384] = P
    s = np.arange(64)[:, None]
    t = np.arange(64)[None, :]
    cst[0:64, 384:448] = (s <= t).astype(np.float32)
    cst[0:64, 448:512] = (s >= t).astype(np.float32)
    c = np.arange(64)
    cs = np.clip(c - 8, 0, 48)
    inwin = (c[None, :] >= cs[:, None]) & (c[None, :] < cs[:, None] + 16)
    wm = np.where(inwin, 0.0, -1e30).astype(np.float32)
    cst[0:64, 512:576] = wm
    cst[64:128, 512:576] = wm
    cst[:, 576:704] = 1.0 / 128.0
    inv = 10000.0 ** (-np.arange(16, dtype=np.float32) / 16.0)
    tt = np.arange(SEQ)
    pos_r = (tt // 64).astype(np.float32)
    pos_c = (tt % 64).astype(np.float32)
    rope = np.zeros((128, 2 * SEQ), np.float32)
    for p in range(128):
        d = p % 64
        j = d % 16
        pos = pos_r if d < 32 else pos_c
        ang = pos * inv[j]
        sign = -1.0 if (d % 32) < 16 else 1.0
        rope[p, :SEQ] = np.cos(ang)
        rope[p, SEQ:] = sign * np.sin(ang)
    return cst, rope


class _Stop(Exception):
    pass


def build(nlayers=DEPTH, stop=None, dbg=()):
    nc = bass.Bass("TRN2", target_bir_lowering=False)
    k = K(nc)
    dbg_out = {}
    try:
        _build(nc, k, dbg_out, nlayers, stop, dbg)
    except _Stop:
        pass
    k.barrier()
    return nc, dbg_out


def _build(nc, k, dbg_out, nlayers, stop, dbg):
    def ck(name):
        if stop == name:
            raise _Stop()

    def dram(name, shape, dt=F32, kind="ExternalInput"):
        return nc.dram_tensor(name, list(shape), dt, kind=kind).ap()

    uid = [0]

    class Scope:
        def __init__(self):
            self.items = []

        def sb(self, name, shape, dt=F32):
            uid[0] += 1
            t = nc.sbuf_tensor(f"{name}_{uid[0]}", list(shape), dt)
            h = t.__enter__()
            self.items.append(t)
            return h

        def close(self):
            k.barrier()
            for t in reversed(self.items):
                t.__exit__(None, None, None)
            self.items = []

    x_in = dram("x", [SEQ, D])
    ctx_in = dram("ctx", [CTX, D])
    c_in = dram("c", [1, D])
    cc_in = dram("c_ctx", [1, D])
    w_ada = dram("w_ada", [DEPTH, D, 6 * D])
    b_ada = dram("b_ada", [DEPTH, 6 * D])
    w_in = dram("w_in", [DEPTH, D, 4096])
    lb_logits = dram("lb_logits", [2, DEPTH, 512])
    a_norm_g = dram("a_norm_g", [DEPTH, 128])
    rpb = dram("rpb", [DEPTH, 8, 15, 31])
    w_out = dram("w_out", [DEPTH, D, D])
    ln1_g = dram("ln1_g", [DEPTH, D])
    ln1_b = dram("ln1_b", [DEPTH, D])
    w_router = dram("w_router", [D, NEXP])
    b_router = dram("b_router", [1, NEXP])
    w_gate = dram("w_gate", [DEPTH, NEXP, D, 512])
    w_up = dram("w_up", [DEPTH, NEXP, D, 512])
    w_down = dram("w_down", [DEPTH, NEXP, 512, D])
    ln2_g = dram("ln2_g", [DEPTH, D])
    ln2_b = dram("ln2_b", [DEPTH, D])
    cst_in = dram("cst", [128, 704])
    rope_in = dram("rope", [128, 2 * SEQ])
    out = dram("out", [SEQ, D], kind="ExternalOutput")
    xs = dram("xs_scr", [T, D], kind="Internal")
    modd = dram("mod_scr", [DEPTH, 2, 6 * D], kind="Internal")
    rpbpad = dram("rpb_scr", [120, 128], kind="Internal")
    combT_d = dram("combT_scr", [16, T], kind="Internal")

    def dbg_dump(name, src_ap, src_buf, shape, dt=F32):
        if name not in dbg:
            return
        o = dram("dbg_" + name, shape, dt=dt, kind="ExternalOutput")
        b = Buf()
        srcs = src_buf if isinstance(src_buf, list) else [src_buf]
        k.dma("sp", o, src_ap, R=srcs, W=[b])
        dbg_out[name] = b

    ps2 = [nc.psum_tensor(f"ps{i}", [128, 1024], F32).__enter__() for i in range(4)]
    bbank = [Buf(f"bank{i}") for i in range(8)]

    def bank(i):
        return ps2[i // 2][:, (i % 2) * 512:(i % 2) * 512 + 512]

    G = Scope()
    cst = G.sb("cst_sb", [128, 704])
    b_cst = Buf()
    ident32 = cst[:, 0:128]
    J2 = cst[:, 128:256]
    cmf = cst[0:64, 384:448]
    cmb = cst[0:64, 448:512]
    winm = cst[:, 512:576]
    ident16 = G.sb("ident16", [128, 128], BF16)
    Pm16 = G.sb("Pm16", [128, 128], BF16)
    onesm = G.sb("onesm", [128, 128], BF16)
    cmi = G.sb("cmi", [64, 2, 64], mybir.dt.int32)
    rmask = G.sb("rmask", [128, 512], BF16)
    XT = G.sb("XT", [128, 8, T], BF16)
    bXT = [Buf(f"xt{i}") for i in range(NT)]
    modc = G.sb("modc", [128, DEPTH, 2, 6, 8])
    b_modc = Buf()
    lbv = G.sb("lbv", [128, 2, DEPTH, 4])
    oml = G.sb("oml", [128, 2, DEPTH, 4])
    noml = G.sb("noml", [128, 2, DEPTH, 4])
    b_lb = Buf()
    ang = G.sb("ang", [128, DEPTH])
    b_ang = Buf()
    wr32 = G.sb("wr32", [128, 8, NEXP])
    b_wr = Buf()
    brb = G.sb("brb", [128, NEXP])
    b_brb = Buf()
    b_combT = Buf()

    k.dma("sp", cst[:], cst_in[:, :], W=[b_cst])
    k.op("dve", lambda e: e.tensor_copy(out=ident16[:], in_=cst[:, 0:128]), R=[b_cst], W=[b_cst])
    k.op("dve", lambda e: e.tensor_copy(out=Pm16[:], in_=cst[:, 256:384]), R=[b_cst], W=[b_cst])
    k.op("dve", lambda e: e.tensor_copy(out=onesm[:], in_=cst[:, 576:704]), R=[b_cst], W=[b_cst])
    k.op("dve", lambda e: e.tensor_copy(out=cmi[:].rearrange("p a b -> p (a b)"), in_=cst[0:64, 384:512]), R=[b_cst], W=[b_cst])
    k.op("pool", lambda e: e.memset(rmask[:], 1.0), W=[b_cst])
    k.op("pool", lambda e: e.memset(rmask[:, 0:512:64], 0.0), R=[b_cst], W=[b_cst])
    k.dma("sp", wr32[:], w_router.rearrange("(ch p) e -> p ch e", p=128), W=[b_wr])
    k.dma("sp", brb[:], bass.AP(tensor=b_router.tensor, offset=0, ap=[[0, 128], [1, NEXP]]), W=[b_brb])

    ck("P0")
    PR = Scope()
    craw = PR.sb("craw", [128, 2, 8])
    csil = PR.sb("csil", [128, 2, 8])
    b_c = Buf()
    k.dma("sp", craw[:, 0, :], c_in.rearrange("o (ch p) -> p (o ch)", p=128), W=[b_c], allow_slow_non_contiguous=True)
    k.dma("sp", craw[:, 1, :], cc_in.rearrange("o (ch p) -> p (o ch)", p=128), W=[b_c], allow_slow_non_contiguous=True)
    k.op("act", lambda e: e.activation(out=csil[:], in_=craw[:], func=AF.Silu), R=[b_c], W=[b_c])
    ck("P1")
    wa = [PR.sb(f"wa{i}", [128, 8, 512]) for i in range(2)]
    b_wa = [Buf(), Buf()]
    modrow = PR.sb("modrow", [2, 6 * D])
    b_modrow = Buf()
    badar = PR.sb("badar", [2, 6 * D])
    b_bada = Buf()
    b_modd_all = []
    mt48 = PR.sb("mt48", [48, 2, 128])
    b_mt48 = Buf()
    for l in range(nlayers):
        for j in range(2):
            k.dma("sp", badar[j:j + 1, :], b_ada[l:l + 1, :], W=[b_bada])
        for cb in range(12):
            i = (l * 12 + cb) % 2
            k.dma("sp", wa[i][:], w_ada[l, :, cb * 512:(cb + 1) * 512].rearrange("(ch p) c -> p ch c", p=128), W=[b_wa[i]])
            pb = 6 + (cb % 2)
            for ch in range(8):
                k.op("pe", lambda e: e.matmul(bank(pb)[0:2, :], lhsT=csil[:, :, ch], rhs=wa[i][:, ch, :],
                                              start=(ch == 0), stop=(ch == 7)), R=[b_c, b_wa[i]], W=[bbank[pb]])
            k.op("dve", lambda e: e.tensor_tensor(out=modrow[:, cb * 512:(cb + 1) * 512], in0=bank(pb)[0:2, :],
                                                  in1=badar[:, cb * 512:(cb + 1) * 512], op=ALU.add),
                 R=[bbank[pb], b_bada], W=[b_modrow])
        ck("P2")
        b_modd = Buf()
        b_modd_all.append(b_modd)
        k.dma("sp", modd[l, :, :], modrow[:], R=[b_modrow], W=[b_modd])
        for j in range(2):
            k.dma("sp", mt48[:, j, :], modd[l, j, :].rearrange("(r p) -> r p", p=128), R=[b_modd], W=[b_mt48])
            k.op("pe", lambda e: e.transpose(out=bank(5)[:, j * 48:(j + 1) * 48], in_=mt48[:, j, :], identity=ident32[0:48, 0:48]),
                 R=[b_mt48, b_cst], W=[bbank[5]])
        k.op("dve", lambda e: e.tensor_copy(out=modc[:, l, :, :, :].rearrange("p j w c -> p (j w c)"), in_=bank(5)[:, 0:96]),
             R=[bbank[5]], W=[b_modc])
        for w_ in (1, 4):
            k.op("dve", lambda e: e.tensor_scalar(out=modc[:, l, :, w_, :], in0=modc[:, l, :, w_, :], scalar1=1.0, scalar2=None, op0=ALU.add),
                 R=[b_modc], W=[b_modc])
    if "modrow" in dbg:
        dbg_dump("modrow", modrow[:], b_modrow, [2, 6 * D])
    ck("P3")
    lraw = PR.sb("lraw", [128, 2, DEPTH, 4])
    lexp = PR.sb("lexp", [128, 2, DEPTH, 4])
    lsum = PR.sb("lsum", [128, 2, 4])
    lmx = PR.sb("lmx", [128, 2, 4])
    b_l = Buf()
    for d_ in range(2):
        for l_ in range(DEPTH):
            k.dma("sp", lraw[:, d_, l_, :], lb_logits[d_, l_, :].rearrange("(h p) -> p h", p=128), W=[b_l], allow_slow_non_contiguous=True)
    k.op("dve", lambda e: e.tensor_tensor(out=lmx[:], in0=lraw[:, :, 0, :], in1=lraw[:, :, 1, :], op=ALU.max), R=[b_l], W=[b_l])
    k.op("dve", lambda e: e.tensor_tensor(out=lmx[:], in0=lmx[:], in1=lraw[:, :, 2, :], op=ALU.max), R=[b_l], W=[b_l])
    k.op("dve", lambda e: e.tensor_tensor(out=lmx[:], in0=lmx[:], in1=lraw[:, :, 3, :], op=ALU.max), R=[b_l], W=[b_l])
    for l_ in range(DEPTH):
        k.op("dve", lambda e: e.tensor_tensor(out=lexp[:, :, l_, :], in0=lraw[:, :, l_, :], in1=lmx[:], op=ALU.subtract), R=[b_l], W=[b_l])
    k.op("act", lambda e: e.activation(out=lexp[:], in_=lexp[:], func=AF.Exp), R=[b_l], W=[b_l])
    k.op("dve", lambda e: e.tensor_tensor(out=lsum[:], in0=lexp[:, :, 0, :], in1=lexp[:, :, 1, :], op=ALU.add), R=[b_l], W=[b_l])
    k.op("dve", lambda e: e.tensor_tensor(out=lsum[:], in0=lsum[:], in1=lexp[:, :, 2, :], op=ALU.add), R=[b_l], W=[b_l])
    k.op("dve", lambda e: e.tensor_tensor(out=lsum[:], in0=lsum[:], in1=lexp[:, :, 3, :], op=ALU.add), R=[b_l], W=[b_l])
    k.op("dve", lambda e: e.reciprocal(out=lsum[:], in_=lsum[:]), R=[b_l], W=[b_l])
    k.op("dve", lambda e: e.memset(lbv[:, :, 0, :], 0.0), R=[b_l], W=[b_lb])
    for l_ in range(1, DEPTH):
        k.op("dve", lambda e: e.tensor_tensor(out=lexp[:, :, l_, :], in0=lexp[:, :, l_, :], in1=lsum[:], op=ALU.mult), R=[b_l], W=[b_l])
        k.op("dve", lambda e: e.tensor_tensor(out=lbv[:, :, l_, :], in0=lbv[:, :, l_ - 1, :], in1=lexp[:, :, l_, :], op=ALU.add), R=[b_l, b_lb], W=[b_lb])
    k.op("dve", lambda e: e.tensor_scalar(out=oml[:], in0=lbv[:], scalar1=-1.0, scalar2=1.0, op0=ALU.mult, op1=ALU.add), R=[b_lb], W=[b_lb])
    k.op("dve", lambda e: e.tensor_scalar(out=noml[:], in0=lbv[:], scalar1=1.0, scalar2=-1.0, op0=ALU.mult, op1=ALU.add), R=[b_lb], W=[b_lb])
    ck("P4")
    k.dma("sp", ang[:], a_norm_g.rearrange("l p -> p l"), W=[b_ang], allow_slow_non_contiguous=True)
    zt = PR.sb("zt", [120, 128])
    b_zt = Buf()
    b_rpbpad = Buf()
    k.op("pool", lambda e: e.memset(zt[:], 0.0), W=[b_zt])
    k.dma("sp", rpbpad[:, :], zt[:], R=[b_zt], W=[b_rpbpad])
    dbg_dump("modc", modc[:].rearrange("p l j w c -> p (l j w c)"), b_modc, [128, DEPTH * 96])
    dbg_dump("lbv", lbv[:].rearrange("p a b c -> p (a b c)"), b_lb, [128, 32])
    PR.close()
    ck("P")

    psT = Rot([0, 1])

    def xt_bufs(t0, n):
        return [bXT[i] for i in range(t0 // 128, (t0 + n + 127) // 128)]

    def transpose_tile(xtile, bx, tt, l, which_sc, which_sh, tf32=None, b_tf=None):
        j = 1 if tt < 2 else 0
        for half in range(2):
            pb = psT.next()
            for q in range(4):
                ch = half * 4 + q
                k.op("pe", lambda e: e.transpose(out=bank(pb)[:, q * 128:(q + 1) * 128], in_=xtile[:, ch * 128:(ch + 1) * 128],
                                                 identity=ident32), R=[bx, b_cst], W=[bbank[pb]])
            for q in range(4):
                ch = half * 4 + q
                sc = modc[:, l, j, which_sc, ch:ch + 1]
                sh = modc[:, l, j, which_sh, ch:ch + 1]
                if tf32 is None:
                    k.op("act", lambda e: e.activation(out=XT[:, ch, tt * 128:(tt + 1) * 128], in_=bank(pb)[:, q * 128:(q + 1) * 128],
                                                       func=AF.Identity, scale=sc, bias=sh), R=[bbank[pb], b_modc], W=[bXT[tt]])
                else:
                    k.op("act", lambda e: e.activation(out=tf32[:, ch, :], in_=bank(pb)[:, q * 128:(q + 1) * 128],
                                                       func=AF.Identity, scale=sc, bias=sh), R=[bbank[pb], b_modc], W=[b_tf])
                    k.op("act", lambda e: e.activation(out=XT[:, ch, tt * 128:(tt + 1) * 128], in_=bank(pb)[:, q * 128:(q + 1) * 128],
                                                       func=AF.Identity, scale=sc, bias=sh), R=[bbank[pb], b_modc], W=[bXT[tt]])

    def proj_fm(pb, W, bW, wsl, t0, n):
        for ch in range(8):
            k.op("pe", lambda e: e.matmul(bank(pb)[:, 0:n], lhsT=W[(slice(None), ch) + tuple(wsl)], rhs=XT[:, ch, t0:t0 + n],
                                          start=(ch == 0), stop=(ch == 7)), R=[bW] + xt_bufs(t0, n), W=[bbank[pb]])

    BLKS = [(0, 512), (512, 512), (1024, 512), (1536, 512), (2048, 256)]

    S0 = Scope()
    xin = [S0.sb(f"xin{i}", [128, D]) for i in range(2)]
    b_xin = [Buf(), Buf()]
    for tt in range(NT):
        i = tt % 2
        src = ctx_in[tt * 128:(tt + 1) * 128, :] if tt < 2 else x_in[(tt - 2) * 128:(tt - 1) * 128, :]
        k.dma("sp", xin[i][:], src, W=[b_xin[i]])
        transpose_tile(xin[i], b_xin[i], tt, 0, 1, 0)
    S0.close()
    if "xt0" in dbg:
        dbg_dump("xt0", XT[:, :, :].rearrange("p c t -> p (c t)"), bXT, [128, 8 * T], dt=BF16)

    ck("T0")
    b_xs = [Buf(f"xs{i}") for i in range(NT)]

    for l in range(nlayers):
        last = (l == DEPTH - 1)
        MS = Scope()
        MIX = MS.sb("MIX", [128, 8, T], BF16)
        b_mix = [Buf(f"mix{i}") for i in range(8)]

        SA = Scope()
        Wh = SA.sb("Wh", [128, 8, 5, 128], BF16)
        b_Wh = Buf()
        nT = 2
        QSb = [SA.sb(f"QSb{i}", [128, 512]) for i in range(nT)]
        Xb2 = [[SA.sb(f"Xb{d}{i}", [128, 512]) for i in range(nT)] for d in range(2)]
        bX2 = [[Buf() for _ in range(nT)] for d in range(2)]
        Fb = [SA.sb(f"Fb{i}", [128, 512]) for i in range(nT)]
        Cb = [SA.sb(f"Cb{i}", [128, 512]) for i in range(nT)]
        Db = [SA.sb(f"Db{i}", [128, 512]) for i in range(nT)]
        Eb = [SA.sb(f"Eb{i}", [128, 512]) for i in range(nT)]
        D2b = [SA.sb(f"D2b{i}", [128, 512]) for i in range(nT)]
        E2b = [SA.sb(f"E2b{i}", [128, 512]) for i in range(nT)]
        bD2 = [Buf() for _ in range(nT)]
        bE2 = [Buf() for _ in range(nT)]
        bQS = [Buf() for _ in range(nT)]
        bX = [Buf() for _ in range(nT)]
        bF = [Buf() for _ in range(nT)]
        bC = [Buf() for _ in range(nT)]
        bD = [Buf() for _ in range(nT)]
        bE = [Buf() for _ in range(nT)]
        QIN = [SA.sb(f"QIN{d}", [128, T], BF16) for d in range(2)]
        QS2 = [SA.sb(f"QS2{d}", [128, T], BF16) for d in range(2)]
        KDEC = [SA.sb(f"KDEC{d}", [128, T], BF16) for d in range(2)]
        KD2 = [SA.sb(f"KD2{d}", [128, T], BF16) for d in range(2)]
        b_q3 = [Buf(), Buf()]
        GS = SA.sb("GS", [128, T], BF16)
        b_gs = Buf()
        TOT = [SA.sb(f"TOT{d}", [128, NCH]) for d in range(2)]
        b_tot = [Buf(), Buf()]
        V64 = SA.sb("V64", [64, NCH, 128], BF16)
        b_v64 = Buf()
        OD = [SA.sb(f"OD{d}", [128, T]) for d in range(2)]
        b_od = [Buf(), Buf()]
        S32 = [SA.sb(f"S32{d}", [128, 128]) for d in range(2)]
        S16 = [SA.sb(f"S16{d}", [128, 128], BF16) for d in range(2)]
        S16b = [[SA.sb(f"S16b{d}{j}", [128, 128], BF16) for j in range(2)] for d in range(2)]
        b_s16b = [[Buf(), Buf()] for d in range(2)]
        b_s32 = [Buf(), Buf()]
        b_s16 = [Buf(), Buf()]
        kdT = [SA.sb(f"kdT{i}", [64, 128], BF16) for i in range(4)]
        b_kdT = [Buf() for _ in range(4)]
        scm = [SA.sb(f"scm{i}", [64, 64], BF16) for i in range(4)]
        b_scm = [Buf() for _ in range(4)]
        sqb = [SA.sb(f"sqb{i}", [128, 512], BF16) for i in range(2)]
        b_sq = [Buf(), Buf()]
        pA = Rot([2, 3, 4, 5])
        for h in range(0 if not os.environ.get('SKIPA') else 4, 4):
            for fi, c0 in enumerate([A_Q, A_G, A_FF, A_FB, A_I]):
                k.dma("pool", Wh[:, :, fi, :], w_in[l, :, c0 + h * 128:c0 + h * 128 + 128].rearrange("(ch p) c -> p ch c", p=128), W=[b_Wh])
            ck("A0")
            for bi, (t0, n) in enumerate(BLKS):
                i = bi % nT
                nch = n // 64
                ck0 = t0 // 64
                if bi == 1:
                    ck("A2")
                pb = pA.next()
                proj_fm(pb, Wh, b_Wh, (0, slice(None)), t0, n)
                k.op("act", lambda e: e.activation(out=QSb[i][:, 0:n], in_=bank(pb)[:, 0:n], func=AF.Silu), R=[bbank[pb]], W=[bQS[i]])
                pb = pA.next()
                proj_fm(pb, Wh, b_Wh, (1, slice(None)), t0, n)
                k.op("act", lambda e: e.activation(out=GS[:, t0:t0 + n], in_=bank(pb)[:, 0:n], func=AF.Silu), R=[bbank[pb]], W=[b_gs])
                ck("A1")
                for d in range(2):
                    pb = pA.next()
                    proj_fm(pb, Wh, b_Wh, (2 + d, slice(None)), t0, n)
                    k.op("act", lambda e: e.activation(out=Xb2[d][i][:, 0:n], in_=bank(pb)[:, 0:n], func=AF.Sigmoid), R=[bbank[pb]], W=[bX2[d][i]])
                for d in range(2):
                    lb_ap = lbv[:, d, l, h:h + 1]
                    oml_ap = oml[:, d, l, h:h + 1]
                    noml_ap = noml[:, d, l, h:h + 1]
                    X_, F_, C_, D_, E_ = Xb2[d][i][:, 0:n], Fb[i][:, 0:n], Cb[i][:, 0:n], Db[i][:, 0:n], Eb[i][:, 0:n]
                    bX = bX2[d]
                    k.op("dve", lambda e: e.tensor_scalar(out=F_, in0=X_, scalar1=1e-6, scalar2=None, op0=ALU.max), R=[bX[i]], W=[bF[i]])
                    k.op("act", lambda e: e.activation(out=F_, in_=F_, func=AF.Ln, scale=oml_ap, bias=lb_ap), R=[bF[i], b_lb], W=[bF[i]])
                    k.op("pool", lambda e: e.tensor_scalar(out=X_, in0=X_, scalar1=noml_ap, scalar2=oml_ap, op0=ALU.mult, op1=ALU.add),
                         R=[bX[i], b_lb], W=[bX[i]])
                    k.op("dve", lambda e: e.tensor_tensor_scan(out=C_, data0=rmask[:, 0:n], data1=F_, initial=0.0, op0=ALU.mult, op1=ALU.add),
                         R=[bF[i], b_cst], W=[bC[i]])
                    C3 = C_.rearrange("p (c s) -> p c s", s=64)
                    Ctb = C3[:, :, 63:64].broadcast_to([128, nch, 64])
                    D3 = D_.rearrange("p (c s) -> p c s", s=64)
                    D2_ = D2b[i][:, 0:n]
                    D23 = D2_.rearrange("p (c s) -> p c s", s=64)
                    if d == 0:
                        Cmb = C3[:, :, 31:32].broadcast_to([128, nch, 64])
                        k.op("dve", lambda e: e.tensor_tensor(out=D3, in0=C3, in1=Ctb, op=ALU.subtract), R=[bC[i]], W=[bD[i]])
                        k.op("pool", lambda e: e.tensor_tensor(out=D23, in0=C3, in1=Cmb, op=ALU.subtract), R=[bC[i]], W=[bD2[i]])
                        a1, a1b, a1s = C_, bC[i], 1.0
                        a3, a3b, a3s = D_, bD[i], -1.0
                    else:
                        k.op("dve", lambda e: e.tensor_tensor(out=F_, in0=C_, in1=F_, op=ALU.subtract), R=[bC[i], bF[i]], W=[bF[i]])
                        F3 = F_.rearrange("p (c s) -> p c s", s=64)
                        Pmb = F3[:, :, 32:33].broadcast_to([128, nch, 64])
                        k.op("dve", lambda e: e.tensor_tensor(out=D3, in0=Ctb, in1=F3, op=ALU.subtract), R=[bC[i], bF[i]], W=[bD[i]])
                        k.op("pool", lambda e: e.tensor_tensor(out=D23, in0=Pmb, in1=F3, op=ALU.subtract), R=[bF[i]], W=[bD2[i]])
                        a1, a1b, a1s = D_, bD[i], 1.0
                        a3, a3b, a3s = F_, bF[i], 1.0
                    k.op("act", lambda e: e.activation(out=TOT[d][:, ck0:ck0 + nch], in_=C_[:, 63:n:64], func=AF.Exp), R=[bC[i]], W=[b_tot[d]])
                    k.op("act", lambda e: e.activation(out=E_, in_=a1, func=AF.Exp, scale=a1s), R=[a1b], W=[bE[i]])
                    k.op("dve", lambda e: e.tensor_tensor(out=QIN[d][:, t0:t0 + n], in0=QSb[i][:, 0:n], in1=E_, op=ALU.mult),
                         R=[bQS[i], bE[i]], W=[b_q3[d]])
                    k.op("act", lambda e: e.activation(out=E_, in_=a3, func=AF.Exp, scale=a3s), R=[a3b], W=[bE[i]])
                    k.op("dve", lambda e: e.tensor_tensor(out=KDEC[d][:, t0:t0 + n], in0=X_, in1=E_, op=ALU.mult),
                         R=[bX[i], bE[i]], W=[b_q3[d]])
                    E2_ = E2b[i][:, 0:n]
                    k.op("act", lambda e: e.activation(out=E2_, in_=D2_, func=AF.Exp, scale=1.0), R=[bD2[i]], W=[bE2[i]])
                    k.op("pool", lambda e: e.tensor_tensor(out=QS2[d][:, t0:t0 + n], in0=QSb[i][:, 0:n], in1=E2_, op=ALU.mult),
                         R=[bQS[i], bE2[i]], W=[b_q3[d]])
                    k.op("act", lambda e: e.activation(out=E_, in_=D2_, func=AF.Exp, scale=-1.0), R=[bD2[i]], W=[bE[i]])
                    k.op("dve", lambda e: e.tensor_tensor(out=KD2[d][:, t0:t0 + n], in0=X_, in1=E_, op=ALU.mult),
                         R=[bX[i], bE[i]], W=[b_q3[d]])
            ck("A3")
            for c4 in range(NCH // 4):
                pb = pA.next()
                for q in range(4):
                    c = c4 * 4 + q
                    for ch in range(8):
                        k.op("pe", lambda e: e.matmul(bank(pb)[0:64, q * 128:(q + 1) * 128], lhsT=XT[:, ch, c * 64:(c + 1) * 64],
                                                      rhs=Wh[:, ch, 4, :], start=(ch == 0), stop=(ch == 7)),
                             R=[b_Wh] + xt_bufs(c * 64, 64), W=[bbank[pb]])
                k.op("act", lambda e: e.activation(out=V64[:, c4 * 4:(c4 + 1) * 4, :].rearrange("p c v -> p (c v)"), in_=bank(pb)[0:64, :],
                                                   func=AF.Copy), R=[bbank[pb]], W=[b_v64])
            ck("A4")
            for d in range(2):
                k.op("pool", lambda e: e.memset(S32[d][:], 0.0), W=[b_s32[d]])
                k.op("pool", lambda e: e.memset(S16[d][:], 0.0), W=[b_s16[d]])
                k.op("pool", lambda e: e.memset(S16b[d][0][:], 0.0), W=[b_s16b[d][0]])
            orders = [list(range(NCH)), [3, 2, 1, 0] + list(range(NCH - 1, 3, -1))]
            cms = [cmf, cmb]
            ri = int(os.environ.get('RI0', 0))
            b_pt = [bbank[0], bbank[4]]
            b_psc = [bbank[1], bbank[5]]
            b_po = [bbank[2], bbank[6]]
            b_pds = [bbank[3], bbank[7]]
            for step in range(int(os.environ.get('NSTEPS', NCH))):
                for d in [int(x) for x in os.environ.get('DIRS', '01')]:
                    c = orders[d][step]
                    cs_ = slice(c * 64, (c + 1) * 64)
                    r4 = ri % 4
                    ri += 1
                    pt_ap = bank(4 * d).bitcast(BF16)[0:64, 0:128]
                    psc_ap = bank(4 * d + 1)[0:64, 0:64]
                    po_ap = bank(4 * d + 2)[:, 0:64]
                    pds_ap = bank(4 * d + 3)[:, 0:128]
                    k.op("pe", lambda e: e.transpose(out=pt_ap, in_=KDEC[d][:, cs_], identity=ident16[:]), R=[b_q3[d], b_cst], W=[b_pt[d]])
                    if step == 0 and d == 0: ck("R0")
                    k.op("act", lambda e: e.activation(out=kdT[r4][:], in_=pt_ap, func=AF.Copy), R=[b_pt[d]], W=[b_kdT[r4]])
                    if step == 0 and d == 0: ck("R1")
                    k.op("pe", lambda e: e.matmul(psc_ap, lhsT=KD2[d][:, cs_], rhs=QS2[d][:, cs_], start=True, stop=True),
                         R=[b_q3[d]], W=[b_psc[d]])
                    if step == 0 and d == 0: ck("R2")
                    k.op("pool", lambda e: e.memset(scm[r4][:], 0.0), W=[b_scm[r4]])
                    k.op("dve", lambda e: e.copy_predicated(out=scm[r4][:], mask=cmi[:, d, :], data=psc_ap), R=[b_psc[d], b_cst, b_scm[r4]], W=[b_scm[r4]])
                    if step == 0 and d == 0: ck("R3")
                    if os.environ.get("S32MASTER"):
                        k.op("pe", lambda e: e.matmul(po_ap, lhsT=S16[d][:], rhs=QIN[d][:, cs_], start=True, stop=False),
                             R=[b_s16[d], b_q3[d]], W=[b_po[d]])
                    else:
                        k.op("pe", lambda e: e.matmul(po_ap, lhsT=S16b[d][step % 2][:], rhs=QIN[d][:, cs_], start=True, stop=False),
                             R=[b_s16b[d][step % 2], b_q3[d]], W=[b_po[d]])
                    if step == 0 and d == 0: ck("R4")
                    k.op("pe", lambda e: e.matmul(po_ap, lhsT=V64[:, c, :], rhs=scm[r4][:], start=False, stop=True),
                         R=[b_v64, b_scm[r4]], W=[b_po[d]])
                    if step == 0 and d == 0: ck("R5")
                    k.op("act", lambda e: e.activation(out=OD[d][:, cs_], in_=po_ap, func=AF.Copy), R=[b_po[d]], W=[b_od[d]])
                    if step == 0 and d == 0: ck("R6")
                    k.op("pe", lambda e: e.matmul(pds_ap, lhsT=kdT[r4][:], rhs=V64[:, c, :], start=True, stop=True),
                         R=[b_kdT[r4], b_v64], W=[b_pds[d]])
                    if step == 0 and d == 0: ck("R7")
                    if os.environ.get("S32MASTER"):
                        k.op("dve", lambda e: e.scalar_tensor_tensor(out=S16[d][:], in0=S32[d][:], scalar=TOT[d][:, c:c + 1], in1=pds_ap,
                                                                     op0=ALU.mult, op1=ALU.add), R=[b_s32[d], b_tot[d], b_pds[d]], W=[b_s16[d]])
                        k.op("dve", lambda e: e.scalar_tensor_tensor(out=S32[d][:], in0=S32[d][:], scalar=TOT[d][:, c:c + 1], in1=pds_ap,
                                                                     op0=ALU.mult, op1=ALU.add), R=[b_s32[d], b_tot[d], b_pds[d]], W=[b_s32[d]])
                    else:
                        sn = S16b[d][(step + 1) % 2]
                        so = S16b[d][step % 2]
                        k.op("dve", lambda e: e.scalar_tensor_tensor(out=sn[:], in0=so[:], scalar=TOT[d][:, c:c + 1], in1=pds_ap,
                                                                     op0=ALU.mult, op1=ALU.add), R=[b_s16b[d][step % 2], b_tot[d], b_pds[d]], W=[b_s16b[d][(step + 1) % 2]])
                    if step == 0 and d == 0: ck("R8")
                    pass
                    if step == 0 and d == 0: ck("R9")
            ck("A5")
            for bi, (t0, n) in enumerate(BLKS):
                i = bi % 2
                O_ = Cb[i][:, 0:n]
                k.op("pool", lambda e: e.tensor_tensor(out=O_, in0=OD[0][:, t0:t0 + n], in1=OD[1][:, t0:t0 + n], op=ALU.add),
                     R=[b_od[0], b_od[1]], W=[bC[i]])
                k.op("act", lambda e: e.activation(out=sqb[i][:, 0:n], in_=O_, func=AF.Square), R=[bC[i]], W=[b_sq[i]])
                pb = pA.next()
                k.op("pe", lambda e: e.matmul(bank(pb)[:, 0:n], lhsT=onesm[:], rhs=sqb[i][:, 0:n], start=True, stop=True),
                     R=[b_sq[i], b_cst], W=[bbank[pb]])
                R_ = Db[i][:, 0:n]
                k.op("act", lambda e: e.activation(out=R_, in_=bank(pb)[:, 0:n], func=AF.Ln, bias=RMS_EPS, scale=1.0), R=[bbank[pb]], W=[bD[i]])
                k.op("act", lambda e: e.activation(out=R_, in_=R_, func=AF.Exp, scale=-0.5), R=[bD[i]], W=[bD[i]])
                k.op("dve", lambda e: e.tensor_tensor(out=O_, in0=O_, in1=R_, op=ALU.mult), R=[bC[i], bD[i]], W=[bC[i]])
                k.op("dve", lambda e: e.scalar_tensor_tensor(out=MIX[:, h, t0:t0 + n], in0=O_, scalar=ang[:, l:l + 1], in1=GS[:, t0:t0 + n],
                                                             op0=ALU.mult, op1=ALU.mult), R=[bC[i], b_ang, b_gs], W=[b_mix[h]])
        SA.close()
        dbg_dump("mixA", MIX[:, 0:4, :].rearrange("p c t -> p (c t)"), b_mix[0:4], [128, 4 * T], dt=BF16)
        ck("A")

        SB = Scope()
        ropeS = SB.sb("rope_sb", [128, 2 * SEQ])
        b_rope = Buf()
        k.dma("sp", ropeS[:], rope_in[:, :], W=[b_rope])
        HB = []
        for par in range(2):
            HB.append(dict(
                Wq=SB.sb(f"Wq{par}", [128, 8, 3, 128], BF16), b_Wq=Buf(),
                Qp=SB.sb(f"Qp{par}", [128, T], BF16), Kp=SB.sb(f"Kp{par}", [128, T], BF16),
                Qr=SB.sb(f"Qr{par}", [128, SEQ], BF16), Kr=SB.sb(f"Kr{par}", [128, SEQ], BF16),
                b_Qp=Buf(), b_Kp=Buf(), b_Qr=Buf(), b_Kr=Buf(),
                Va=SB.sb(f"Va{par}", [128, NT, 128], BF16), Vb=SB.sb(f"Vb{par}", [128, NT - 1, 128], BF16),
                b_Va=Buf(), b_Vb=Buf(),
                Hh=SB.sb(f"Hh{par}", [128, 15, 64]), b_Hh=Buf(),
                Tb=SB.sb(f"Tb{par}", [128, 960], BF16), b_Tb=Buf()))
        rp_sb = SB.sb("rp_sb", [120, 31])
        b_rp = Buf()
        rt1 = [SB.sb(f"rt1{i}", [128, 512]) for i in range(2)]
        rt2 = [SB.sb(f"rt2{i}", [128, 512]) for i in range(2)]
        b_rt1 = [Buf(), Buf()]
        b_rt2 = [Buf(), Buf()]
        Pt_ = [SB.sb(f"Pt{i}", [128, 768], BF16) for i in range(3)]
        PTs = [SB.sb(f"PTs{i}", [128, 768], BF16) for i in range(3)]
        b_P = [Buf(), Buf(), Buf()]
        b_PTs = [Buf(), Buf(), Buf()]
        stt_ = [SB.sb(f"stt{i}", [128, 4]) for i in range(3)]
        b_st = [Buf(), Buf(), Buf()]
        Dg = [SB.sb(f"Dg{i}", [128, 128], BF16) for i in range(3)]
        b_Dg = [Buf(), Buf(), Buf()]
        pB = Rot([6, 7])
        k.dma("sp", rp_sb[:], rpb[l].rearrange("h r c -> (h r) c"), W=[b_rp])
        k.dma("sp", rpbpad[:, 48:79], rp_sb[:], R=[b_rp], W=[b_rpbpad])
        rope_ctr = [0]

        def setup_gen(hp):
            hb = HB[hp % 2]
            Wq, b_Wq, Qp, Kp, Qr, Kr = hb["Wq"], hb["b_Wq"], hb["Qp"], hb["Kp"], hb["Qr"], hb["Kr"]
            b_Qp, b_Kp, b_Qr, b_Kr = hb["b_Qp"], hb["b_Kp"], hb["b_Qr"], hb["b_Kr"]
            Va, Vb, b_Va, b_Vb, Hh, b_Hh, Tb, b_Tb = hb["Va"], hb["Vb"], hb["b_Va"], hb["b_Vb"], hb["Hh"], hb["b_Hh"], hb["Tb"], hb["b_Tb"]
            for fi, c0 in enumerate([B_Q, B_K, B_V]):
                k.dma("pool", Wq[:, :, fi, :], w_in[l, :, c0 + hp * 128:c0 + hp * 128 + 128].rearrange("(ch p) c -> p ch c", p=128), W=[b_Wq])
            for hh in range(2):
                src = bass.AP(tensor=rpbpad.tensor, offset=((2 * hp + hh) * 15) * 128, ap=[[1, 64], [128, 15], [1, 64]])
                k.dma("sp", Hh[hh * 64:(hh + 1) * 64, :, :], src, R=[b_rpbpad], W=[b_Hh])
            Hf = Hh[:].rearrange("p a b -> p (a b)")
            for (c0, ncol) in ((0, 512), (512, 448)):
                pb = pB.next()
                k.op("pe", lambda e: e.matmul(bank(pb)[:, 0:ncol], lhsT=J2, rhs=Hf[:, c0:c0 + ncol], start=True, stop=True),
                     R=[b_cst, b_Hh], W=[bbank[pb]])
                ndr = ncol // 64
                k.op("dve", lambda e: e.tensor_tensor(out=Tb[:, c0:c0 + ncol].rearrange("p (a b) -> p a b", b=64),
                                                      in0=bank(pb)[:, 0:ncol].rearrange("p (a b) -> p a b", b=64),
                                                      in1=winm.unsqueeze(1).broadcast_to([128, ndr, 64]), op=ALU.add),
                     R=[bbank[pb], b_cst], W=[b_Tb])
                yield
            for bi, (t0, n) in enumerate(BLKS):
                pb = pB.next()
                proj_fm(pb, Wq, b_Wq, (0, slice(None)), t0, n)
                k.op("act", lambda e: e.activation(out=Qp[:, t0:t0 + n], in_=bank(pb)[:, 0:n], func=AF.Copy, scale=0.125), R=[bbank[pb]], W=[b_Qp])
                pb = pB.next()
                proj_fm(pb, Wq, b_Wq, (1, slice(None)), t0, n)
                k.op("dve", lambda e: e.tensor_copy(out=Kp[:, t0:t0 + n], in_=bank(pb)[:, 0:n]), R=[bbank[pb]], W=[b_Kp])
                yield
            for lbk in range(4):
                t0 = CTX + lbk * 512
                tl = lbk * 512
                for (src_, bsrc, dst_, bdst) in ((Qp, b_Qp, Qr, b_Qr), (Kp, b_Kp, Kr, b_Kr)):
                    i = rope_ctr[0] % 2
                    rope_ctr[0] += 1
                    pb = pB.next()
                    k.op("pe", lambda e: e.matmul(bank(pb)[:, :], lhsT=Pm16[:], rhs=src_[:, t0:t0 + 512], start=True, stop=True),
                         R=[b_cst, bsrc], W=[bbank[pb]])
                    k.op("pool", lambda e: e.tensor_tensor(out=rt1[i][:], in0=src_[:, t0:t0 + 512], in1=ropeS[:, tl:tl + 512], op=ALU.mult),
                         R=[bsrc, b_rope], W=[b_rt1[i]])
                    k.op("dve", lambda e: e.tensor_tensor(out=rt2[i][:], in0=bank(pb)[:, :], in1=ropeS[:, SEQ + tl:SEQ + tl + 512], op=ALU.mult),
                         R=[bbank[pb], b_rope], W=[b_rt2[i]])
                    k.op("pool", lambda e: e.tensor_tensor(out=dst_[:, tl:tl + 512], in0=rt1[i][:], in1=rt2[i][:], op=ALU.add),
                         R=[b_rt1[i], b_rt2[i]], W=[bdst])
                    yield
            for (Vt, bV, ntile, off) in ((Va, b_Va, NT, 0), (Vb, b_Vb, NT - 1, 64)):
                j0 = 0
                while j0 < ntile:
                    g = min(4, ntile - j0)
                    pb = pB.next()
                    for q in range(g):
                        tk = off + (j0 + q) * 128
                        for ch in range(8):
                            k.op("pe", lambda e: e.matmul(bank(pb)[:, q * 128:(q + 1) * 128], lhsT=XT[:, ch, tk:tk + 128], rhs=Wq[:, ch, 2, :],
                                                          start=(ch == 0), stop=(ch == 7)), R=[b_Wq] + xt_bufs(tk, 128), W=[bbank[pb]])
                    k.op("act", lambda e: e.activation(out=Vt[:, j0:j0 + g, :].rearrange("p a b -> p (a b)"), in_=bank(pb)[:, 0:g * 128], func=AF.Copy),
                         R=[bbank[pb]], W=[bV])
                    j0 += g
                    yield

        gens = [setup_gen(hp_) for hp_ in range(4)]
        for hp in range(4):
            for _ in gens[hp]:
                pass
            hb = HB[hp % 2]
            Qp, Kp, Qr, Kr = hb["Qp"], hb["Kp"], hb["Qr"], hb["Kr"]
            b_Qp, b_Kp, b_Qr, b_Kr = hb["b_Qp"], hb["b_Kp"], hb["b_Qr"], hb["b_Kr"]
            Va, Vb, b_Va, b_Vb, Tb, b_Tb = hb["Va"], hb["Vb"], hb["b_Va"], hb["b_Vb"], hb["Tb"], hb["b_Tb"]
            gnext = None
            tiles = [("ctx", qb) for qb in range(4)] + [("lat", r) for r in range(32)]
            NTL = len(tiles)
            tst = [None] * NTL

            def ph_a(t):
                kind, idx = tiles[t]
                i3 = t % 3
                S2 = ps2[i3]
                bS = [bbank[2 * i3], bbank[2 * i3 + 1]]
                if kind == "lat":
                    r = idx
                    rs = min(max(r - 4, 0), 24)
                    dr0 = rs - r + 7
                    qt = CTX + r * 64
                    nk = 768
                    k.op("pe", lambda e: e.matmul(S2[:, 0:512], lhsT=ident16[:], rhs=Tb[:, dr0 * 64:dr0 * 64 + 512], start=True, stop=False),
                         R=[b_Tb, b_cst], W=[bS[0]])
                    for hh in range(2):
                        hs = slice(hh * 64, (hh + 1) * 64)
                        k.op("pe", lambda e: e.matmul(S2[hs, 0:512], lhsT=Qr[hs, r * 64:(r + 1) * 64], rhs=Kr[hs, rs * 64:rs * 64 + 512],
                                                      start=False, stop=True), R=[b_Qr, b_Kr], W=[bS[0]])
                    for hh in range(2):
                        hs = slice(hh * 64, (hh + 1) * 64)
                        k.op("pe", lambda e: e.matmul(S2[hs, 512:768], lhsT=Qp[hs, qt:qt + 64], rhs=Kp[hs, 0:CTX], start=True, stop=True),
                             R=[b_Qp, b_Kp], W=[bS[1]])
                    vts = []
                    for b in range(4):
                        if rs % 2 == 0:
                            vts.append((Va, 2 + rs // 2 + b, b_Va))
                        else:
                            vts.append((Vb, (3 + rs) // 2 + b, b_Vb))
                    vts += [(Va, 0, b_Va), (Va, 1, b_Va)]
                else:
                    qt = idx * 64
                    nk = 256
                    for hh in range(2):
                        hs = slice(hh * 64, (hh + 1) * 64)
                        k.op("pe", lambda e: e.matmul(S2[hs, 0:256], lhsT=Qp[hs, qt:qt + 64], rhs=Kp[hs, 0:CTX], start=True, stop=True),
                             R=[b_Qp, b_Kp], W=[bS[0]])
                    vts = [(Va, 0, b_Va), (Va, 1, b_Va)]
                tst[t] = (S2, bS, qt, nk, vts)

            def ph_b(t):
                S2, bS, qt, nk, vts = tst[t]
                i = t % 3
                bSr = bS if nk > 512 else bS[0:1]
                st_ = stt_[i]
                k.op("dve", lambda e: e.tensor_reduce(out=st_[:, 0:1], in_=S2[:, 0:nk], axis=AX.X, op=ALU.max), R=bSr, W=[b_st[i]])
                k.op("dve", lambda e: e.tensor_scalar(out=st_[:, 1:2], in0=st_[:, 0:1], scalar1=-1.0, scalar2=None, op0=ALU.mult), R=[b_st[i]], W=[b_st[i]])
                k.op("act", lambda e: e.activation(out=Pt_[i][:, 0:nk], in_=S2[:, 0:nk], func=AF.Exp, bias=st_[:, 1:2], scale=1.0, accum_out=st_[:, 2:3]),
                     R=bSr + [b_st[i]], W=[b_P[i], b_st[i]])
                k.op("dve", lambda e: e.reciprocal(out=st_[:, 3:4], in_=st_[:, 2:3]), R=[b_st[i]], W=[b_st[i]])
                k.op("dve", lambda e: e.tensor_scalar(out=Dg[i][:], in0=ident32, scalar1=st_[:, 3:4], scalar2=None, op0=ALU.mult), R=[b_st[i], b_cst], W=[b_Dg[i]])

            def ph_c(t):
                S2, bS, qt, nk, vts = tst[t]
                i = t % 3
                nb = nk // 128
                PT2 = ps2[3]
                for b in range(nb):
                    k.op("pe", lambda e: e.matmul(PT2[:, b * 128:(b + 1) * 128], lhsT=Pt_[i][:, b * 128:(b + 1) * 128], rhs=Dg[i][:], start=True, stop=True),
                         R=[b_P[i], b_Dg[i]], W=[bbank[6 + b // 4]])
                bPT = [bbank[6], bbank[7]] if nb > 4 else [bbank[6]]
                if t % 2 == 0:
                    k.op("act", lambda e: e.activation(out=PTs[i][:, 0:nk], in_=PT2[:, 0:nk], func=AF.Copy), R=bPT, W=[b_PTs[i]])
                else:
                    k.op("dve", lambda e: e.tensor_copy(out=PTs[i][:, 0:nk], in_=PT2[:, 0:nk]), R=bPT, W=[b_PTs[i]])

            def ph_d(t):
                S2, bS, qt, nk, vts = tst[t]
                i = t % 3
                nb = nk // 128
                for hh in range(2):
                    hs = slice(hh * 64, (hh + 1) * 64)
                    for b in range(nb):
                        Vt, vj, bV = vts[b]
                        k.op("pe", lambda e: e.matmul(S2[hs, 768:832], lhsT=Vt[:, vj, hs], rhs=PTs[i][:, b * 128 + hh * 64:b * 128 + hh * 64 + 64],
                                                      start=(b == 0), stop=(b == nb - 1)), R=[bV, b_PTs[i]], W=[bS[1]])
                k.op("act", lambda e: e.activation(out=MIX[:, 4 + hp, qt:qt + 64], in_=S2[:, 768:832], func=AF.Copy), R=[bS[1]], W=[b_mix[4 + hp]])

            for t in range(NTL + 2):
                if t < NTL:
                    ph_a(t)
                if gnext is not None:
                    next(gnext, None)
                if 1 <= t <= NTL:
                    ph_b(t - 1)
                    ph_c(t - 1)
                if t >= 2:
                    ph_d(t - 2)
            if gnext is not None:
                for _ in gnext:
                    pass
        SB.close()
        dbg_dump("mixB", MIX[:, 4:8, :].rearrange("p c t -> p (c t)"), b_mix[4:8], [128, 4 * T], dt=BF16)
        ck("B")

        SO = Scope()
        Wo = SO.sb("Wo", [128, 8, D], BF16)
        b_Wo = Buf()
        for hf in range(2):
            k.dma("pool", Wo[:, hf * 4:(hf + 1) * 4, :], w_out[l, hf * 512:(hf + 1) * 512, :].rearrange("(ch p) c -> p ch c", p=128), W=[b_Wo])
        gbc = SO.sb("gbc", [128, 2, D])
        lngb = SO.sb("lngb", [128, 2, D])
        b_bc = Buf()
        for j in range(2):
            k.dma("sp", gbc[:, j, :], bass.AP(tensor=modd.tensor, offset=(l * 2 + j) * 6 * D + 2 * D, ap=[[0, 128], [1, D]]), R=[b_modd_all[l]], W=[b_bc])
        k.dma("sp", lngb[:, 0, :], bass.AP(tensor=ln1_g.tensor, offset=l * D, ap=[[0, 128], [1, D]]), W=[b_bc])
        k.dma("sp", lngb[:, 1, :], bass.AP(tensor=ln1_b.tensor, offset=l * D, ap=[[0, 128], [1, D]]), W=[b_bc])
        xr = [SO.sb(f"xr{i}", [128, D]) for i in range(3)]
        zt = [SO.sb(f"zt{i}", [128, D]) for i in range(3)]
        x1t = [SO.sb(f"x1t{i}", [128, D]) for i in range(3)]
        tf32 = [SO.sb(f"tf32{i}", [128, 8, 128]) for i in range(3)]
        b_xr, b_zt, b_x1t, b_tf = [Buf(), Buf(), Buf()], [Buf(), Buf(), Buf()], [Buf(), Buf(), Buf()], [Buf(), Buf(), Buf()]
        lnst = [SO.sb(f"lnst{i}", [128, 2, 6]) for i in range(3)]
        lnmv = [SO.sb(f"lnmv{i}", [128, 4]) for i in range(3)]
        b_ln = [Buf(), Buf(), Buf()]
        rt = [SO.sb(f"rt{i}", [128, 8, 16]) for i in range(3)]
        rcmp = [SO.sb(f"rcmp{i}", [128, 4, 4, 4]) for i in range(3)]
        b_rt = [Buf(), Buf(), Buf()]
        cTs = [SO.sb(f"cTs{i}", [16, 128]) for i in range(3)]
        b_cTs = [Buf(), Buf(), Buf()]
        pY = Rot([2, 4])

        def layer_norm_tile(st, mv, bln, z, bz, xo, bxo, g_ap, b_ap, bgb):
            for hf in range(2):
                k.op("dve", lambda e: e.bn_stats(out=st[:, hf, :], in_=z[:, hf * 512:(hf + 1) * 512]), R=[bz], W=[bln])
            k.op("dve", lambda e: e.bn_aggr(out=mv[:, 0:2], in_=st[:].rearrange("p a b -> p (a b)")), R=[bln], W=[bln])
            k.op("act", lambda e: e.activation(out=mv[:, 2:3], in_=mv[:, 1:2], func=AF.Ln, bias=LN_EPS, scale=1.0), R=[bln], W=[bln])
            k.op("act", lambda e: e.activation(out=mv[:, 2:3], in_=mv[:, 2:3], func=AF.Exp, scale=-0.5), R=[bln], W=[bln])
            k.op("dve", lambda e: e.tensor_scalar(out=mv[:, 3:4], in0=mv[:, 0:1], scalar1=mv[:, 2:3], scalar2=-1.0, op0=ALU.mult, op1=ALU.mult),
                 R=[bln], W=[bln])
            k.op("act", lambda e: e.activation(out=xo[:], in_=z[:], func=AF.Identity, scale=mv[:, 2:3], bias=mv[:, 3:4]), R=[bz, bln], W=[bxo])
            k.op("dve", lambda e: e.tensor_tensor(out=xo[:], in0=xo[:], in1=g_ap, op=ALU.mult), R=[bxo, bgb], W=[bxo])
            k.op("pool", lambda e: e.tensor_tensor(out=xo[:], in0=xo[:], in1=b_ap, op=ALU.add), R=[bxo, bgb], W=[bxo])

        tt0 = 2 if last else 0

        def load_res(t_):
            i_ = t_ % 3
            if l == 0:
                src = ctx_in[t_ * 128:(t_ + 1) * 128, :] if t_ < 2 else x_in[(t_ - 2) * 128:(t_ - 1) * 128, :]
                k.dma("sp", xr[i_][:], src, W=[b_xr[i_]])
            else:
                k.dma("sp", xr[i_][:], xs[t_ * 128:(t_ + 1) * 128, :], R=[b_xs[t_]], W=[b_xr[i_]])

        def stageO_p1(tt):
            i = tt % 3
            j = 1 if tt < 2 else 0
            pb0 = pY.next()
            if tt == tt0:
                load_res(tt)
            if tt + 1 < NT:
                load_res(tt + 1)
            for hf in range(2):
                pb = pb0 + hf
                for ch in range(8):
                    k.op("pe", lambda e: e.matmul(bank(pb)[:, :], lhsT=MIX[:, ch, tt * 128:(tt + 1) * 128], rhs=Wo[:, ch, hf * 512:(hf + 1) * 512],
                                                  start=(ch == 0), stop=(ch == 7)), R=[b_mix[ch], b_Wo], W=[bbank[pb]])
                k.op("dve", lambda e: e.tensor_tensor(out=zt[i][:, hf * 512:(hf + 1) * 512], in0=bank(pb)[:, :], in1=gbc[:, j, hf * 512:(hf + 1) * 512], op=ALU.mult),
                     R=[bbank[pb], b_bc], W=[b_zt[i]])
            k.op("dve", lambda e: e.scalar_tensor_tensor(out=zt[i][:], in0=xr[i][:], scalar=ALPHA, in1=zt[i][:], op0=ALU.mult, op1=ALU.add),
                 R=[b_xr[i], b_zt[i]], W=[b_zt[i]])
            yield
            layer_norm_tile(lnst[i], lnmv[i], b_ln[i], zt[i], b_zt[i], x1t[i], b_x1t[i], lngb[:, 0, :], lngb[:, 1, :], b_bc)
            k.dma("sp", xs[tt * 128:(tt + 1) * 128, :], x1t[i][:], R=[b_x1t[i]], W=[b_xs[tt]])

        def stageO_p2(tt):
            i = tt % 3
            j = 1 if tt < 2 else 0
            transpose_tile(x1t[i], b_x1t[i], tt, l, 4, 3, tf32=tf32[i], b_tf=b_tf[i])
            yield
            for ch in range(8):
                k.op("pe", lambda e: e.matmul(bank(6)[:, 0:16], lhsT=tf32[i][:, ch, :], rhs=wr32[:, ch, :], start=(ch == 0), stop=(ch == 7)),
                     R=[b_tf[i], b_wr], W=[bbank[6]])
            yield
            R_ = rt[i]
            e1, aff, sel, m2, gs4, gm4, asel = R_[:, 0, :], R_[:, 1, :], R_[:, 2, :], R_[:, 3, :], R_[:, 4, 0:4], R_[:, 4, 4:8], R_[:, 5, :]
            gmx, den = R_[:, 4, 8:9], R_[:, 4, 9:10]
            k.op("act", lambda e: e.activation(out=e1, in_=bank(6)[:, 0:16], func=AF.Exp, scale=-1.0), R=[bbank[6]], W=[b_rt[i]])
            k.op("dve", lambda e: e.tensor_scalar(out=e1, in0=e1, scalar1=1.0, scalar2=None, op0=ALU.add), R=[b_rt[i]], W=[b_rt[i]])
            k.op("dve", lambda e: e.reciprocal(out=aff, in_=e1), R=[b_rt[i]], W=[b_rt[i]])
            k.op("dve", lambda e: e.tensor_tensor(out=sel, in0=aff, in1=brb[:], op=ALU.add), R=[b_rt[i], b_brb], W=[b_rt[i]])
            selv = sel.rearrange("p (g i) -> p g i", g=4)
            k.op("dve", lambda e: e.tensor_tensor(out=rcmp[i][:], in0=selv.unsqueeze(2).broadcast_to([128, 4, 4, 4]),
                                                  in1=selv.unsqueeze(3).broadcast_to([128, 4, 4, 4]), op=ALU.is_gt), R=[b_rt[i]], W=[b_rt[i]])
            m2v = m2.rearrange("p (g i) -> p g i", g=4)
            k.op("dve", lambda e: e.tensor_reduce(out=m2v, in_=rcmp[i][:], axis=AX.X, op=ALU.add), R=[b_rt[i]], W=[b_rt[i]])
            k.op("dve", lambda e: e.tensor_scalar(out=m2, in0=m2, scalar1=2.0, scalar2=None, op0=ALU.is_lt), R=[b_rt[i]], W=[b_rt[i]])
            k.op("dve", lambda e: e.tensor_tensor(out=asel, in0=m2, in1=sel, op=ALU.mult), R=[b_rt[i]], W=[b_rt[i]])
            k.op("dve", lambda e: e.tensor_reduce(out=gs4, in_=asel.rearrange("p (g i) -> p g i", g=4), axis=AX.X, op=ALU.add), R=[b_rt[i]], W=[b_rt[i]])
            k.op("dve", lambda e: e.tensor_reduce(out=gmx, in_=gs4, axis=AX.X, op=ALU.max), R=[b_rt[i]], W=[b_rt[i]])
            k.op("dve", lambda e: e.tensor_scalar(out=gm4, in0=gs4, scalar1=gmx, scalar2=None, op0=ALU.is_ge), R=[b_rt[i]], W=[b_rt[i]])
            k.op("dve", lambda e: e.tensor_tensor(out=m2v, in0=m2v, in1=gm4.unsqueeze(2).broadcast_to([128, 4, 4]), op=ALU.mult), R=[b_rt[i]], W=[b_rt[i]])
            k.op("dve", lambda e: e.tensor_tensor(out=asel, in0=m2, in1=aff, op=ALU.mult), R=[b_rt[i]], W=[b_rt[i]])
            k.op("dve", lambda e: e.tensor_reduce(out=den, in_=asel, axis=AX.X, op=ALU.add), R=[b_rt[i]], W=[b_rt[i]])
            k.op("dve", lambda e: e.reciprocal(out=den, in_=den), R=[b_rt[i]], W=[b_rt[i]])
            k.op("dve", lambda e: e.tensor_scalar(out=asel, in0=asel, scalar1=den, scalar2=None, op0=ALU.mult), R=[b_rt[i]], W=[b_rt[i]])
            k.op("pe", lambda e: e.transpose(out=bank(7)[0:16, 0:128], in_=asel, identity=ident32), R=[b_rt[i], b_cst], W=[bbank[7]])
            k.op("act", lambda e: e.activation(out=cTs[i][:], in_=bank(7)[0:16, 0:128], func=AF.Copy), R=[bbank[7]], W=[b_cTs[i]])
            k.dma("sp", combT_d[:, tt * 128:(tt + 1) * 128], cTs[i][:], R=[b_cTs[i]], W=[b_combT])
            if tt == 3:
                dbg_dump("x1t3", x1t[i][:], b_x1t[i], [128, D])
                dbg_dump("comb3", asel, b_rt[i], [128, 16])
        for tt in range(tt0, NT + 1):
            g1 = stageO_p1(tt) if tt < NT else None
            g2 = stageO_p2(tt - 1) if tt > tt0 else None
            if g2:
                next(g2)
            if g1:
                next(g1)
            if g2:
                next(g2)
            if g1:
                next(g1, None)
            if g2:
                next(g2, None)
        SO.close()
        dbg_dump("xt2", XT[:, :, :].rearrange("p c t -> p (c t)"), bXT, [128, 8 * T], dt=BF16)
        ck("O")
        MS.close()

        SML = Scope()
        ACC = SML.sb("ACC", [128, NT, D])
        b_acc = [Buf(f"acc{i}") for i in range(NT)]
        SM = Scope()
        CT = SM.sb("CT", [16, T])
        b_CT = Buf()
        k.dma("sp", CT[:], combT_d[:, :], R=[b_combT], W=[b_CT])
        CB = SM.sb("CB", [128, T])
        b_CB = Buf()
        wg = [SM.sb(f"wg{i}", [128, 8, 128], BF16) for i in range(2)]
        wu = [SM.sb(f"wu{i}", [128, 8, 128], BF16) for i in range(2)]
        b_wg, b_wu = [Buf(), Buf()], [Buf(), Buf()]
        wd = SM.sb("wd", [128, 4, D], BF16)
        b_wd = Buf()
        HID = SM.sb("HID", [128, 4, T], BF16)
        b_hid = [Buf() for _ in range(4)]
        sgt = [SM.sb(f"sgt{i}", [128, 512]) for i in range(2)]
        tmt = [SM.sb(f"tmt{i}", [128, 512]) for i in range(2)]
        b_sg, b_tm = [Buf(), Buf()], [Buf(), Buf()]
        pM = Rot([0, 1, 2, 3, 4, 5, 6, 7])
        wi = 0
        ti = 0
        NEX_ = int(os.environ.get("NEXPS", NEXP))
        MBLKS = [(256, 512), (768, 512), (1280, 512), (1792, 512)] if last else BLKS
        mt0 = 2 if last else 0

        def load_gu(ex_, fc_, wi_):
            k.dma("pool", wg[wi_][:], w_gate[l, ex_, :, fc_ * 128:(fc_ + 1) * 128].rearrange("(ch p) c -> p ch c", p=128), W=[b_wg[wi_]])
            k.dma("pool", wu[wi_][:], w_up[l, ex_, :, fc_ * 128:(fc_ + 1) * 128].rearrange("(ch p) c -> p ch c", p=128), W=[b_wu[wi_]])

        for ex in range(NEX_):
            for bi, (t0, n) in enumerate(MBLKS):
                pb = pM.next()
                k.op("pe", lambda e: e.matmul(bank(pb)[:, 0:n], lhsT=ident32[0:16, ex:ex + 1].broadcast_to([16, 128]), rhs=CT[:, t0:t0 + n], start=True, stop=True),
                     R=[b_cst, b_CT], W=[bbank[pb]])
                k.op("act", lambda e: e.activation(out=CB[:, t0:t0 + n], in_=bank(pb)[:, 0:n], func=AF.Copy), R=[bbank[pb]], W=[b_CB])
            for fc in range(4):
                w_i = wi % 2
                wi += 1
                if ex == 0 and fc == 0:
                    load_gu(ex, fc, w_i)
                nfc, nex = (fc + 1, ex) if fc < 3 else (0, ex + 1)
                if nex < NEX_:
                    load_gu(nex, nfc, 1 - w_i)
                if fc == 1:
                    k.dma("pool", wd[:], w_down[l, ex, :, :].rearrange("(fc p) c -> p fc c", p=128), W=[b_wd])
                for bi, (t0, n) in enumerate(MBLKS):
                    i = ti % 2
                    ti += 1
                    pg = pM.next()
                    for ch in range(8):
                        k.op("pe", lambda e: e.matmul(bank(pg)[:, 0:n], lhsT=wg[w_i][:, ch, :], rhs=XT[:, ch, t0:t0 + n], start=(ch == 0), stop=(ch == 7)),
                             R=[b_wg[w_i]] + xt_bufs(t0, n), W=[bbank[pg]])
                    pu = pM.next()
                    for ch in range(8):
                        k.op("pe", lambda e: e.matmul(bank(pu)[:, 0:n], lhsT=wu[w_i][:, ch, :], rhs=XT[:, ch, t0:t0 + n], start=(ch == 0), stop=(ch == 7)),
                             R=[b_wu[w_i]] + xt_bufs(t0, n), W=[bbank[pu]])
                    k.op("act", lambda e: e.activation(out=sgt[i][:, 0:n], in_=bank(pg)[:, 0:n], func=AF.Silu), R=[bbank[pg]], W=[b_sg[i]])
                    k.op("dve", lambda e: e.tensor_tensor(out=tmt[i][:, 0:n], in0=bank(pu)[:, 0:n], in1=CB[:, t0:t0 + n], op=ALU.mult),
                         R=[bbank[pu], b_CB], W=[b_tm[i]])
                    k.op("pool", lambda e: e.tensor_tensor(out=HID[:, fc, t0:t0 + n], in0=sgt[i][:, 0:n], in1=tmt[i][:, 0:n], op=ALU.mult),
                         R=[b_sg[i], b_tm[i]], W=[b_hid[fc]])
            for tt in range(mt0, NT):
                for hf in range(2):
                    pb = pM.next()
                    for fc in range(4):
                        k.op("pe", lambda e: e.matmul(bank(pb)[:, :], lhsT=HID[:, fc, tt * 128:(tt + 1) * 128], rhs=wd[:, fc, hf * 512:(hf + 1) * 512],
                                                      start=(fc == 0), stop=(fc == 3)), R=[b_hid[fc], b_wd], W=[bbank[pb]])
                    dst = ACC[:, tt, hf * 512:(hf + 1) * 512]
                    if ex == 0:
                        k.op("act", lambda e: e.activation(out=dst, in_=bank(pb)[:, :], func=AF.Copy), R=[bbank[pb]], W=[b_acc[tt]])
                    else:
                        k.op("dve", lambda e: e.tensor_tensor(out=dst, in0=bank(pb)[:, :], in1=dst, op=ALU.add), R=[bbank[pb], b_acc[tt]], W=[b_acc[tt]])
        SM.close()
        dbg_dump("acc3", ACC[:, 3, :], b_acc[3], [128, D])
        ck("M")

        SO = Scope()
        gbc = SO.sb("gbc2", [128, 2, D])
        lngb = SO.sb("lngb2", [128, 2, D])
        b_bc = Buf()
        for j in range(2):
            k.dma("sp", gbc[:, j, :], bass.AP(tensor=modd.tensor, offset=(l * 2 + j) * 6 * D + 5 * D, ap=[[0, 128], [1, D]]), R=[b_modd_all[l]], W=[b_bc])
        k.dma("sp", lngb[:, 0, :], bass.AP(tensor=ln2_g.tensor, offset=l * D, ap=[[0, 128], [1, D]]), W=[b_bc])
        k.dma("sp", lngb[:, 1, :], bass.AP(tensor=ln2_b.tensor, offset=l * D, ap=[[0, 128], [1, D]]), W=[b_bc])
        xr = [SO.sb(f"xr2{i}", [128, D]) for i in range(3)]
        zt = [SO.sb(f"zt2{i}", [128, D]) for i in range(3)]
        x1t = [SO.sb(f"x2t{i}", [128, D]) for i in range(3)]
        b_xr, b_zt, b_x1t = [Buf(), Buf(), Buf()], [Buf(), Buf(), Buf()], [Buf(), Buf(), Buf()]
        lnst = [SO.sb(f"lnst2{i}", [128, 2, 6]) for i in range(3)]
        lnmv = [SO.sb(f"lnmv2{i}", [128, 4]) for i in range(3)]
        b_ln = [Buf(), Buf(), Buf()]
        pendT = [None]
        lt0 = 2 if last else 0
        k.dma("sp", xr[lt0 % 3][:], xs[lt0 * 128:(lt0 + 1) * 128, :], R=[b_xs[lt0]], W=[b_xr[lt0 % 3]])
        for tt in range(lt0, NT):
            i = tt % 3
            j = 1 if tt < 2 else 0
            if tt + 1 < NT:
                k.dma("sp", xr[(tt + 1) % 3][:], xs[(tt + 1) * 128:(tt + 2) * 128, :], R=[b_xs[tt + 1]], W=[b_xr[(tt + 1) % 3]])
            k.op("dve", lambda e: e.tensor_tensor(out=zt[i][:], in0=ACC[:, tt, :], in1=gbc[:, j, :], op=ALU.mult), R=[b_acc[tt], b_bc], W=[b_zt[i]])
            k.op("dve", lambda e: e.scalar_tensor_tensor(out=zt[i][:], in0=xr[i][:], scalar=ALPHA, in1=zt[i][:], op0=ALU.mult, op1=ALU.add),
                 R=[b_xr[i], b_zt[i]], W=[b_zt[i]])
            layer_norm_tile(lnst[i], lnmv[i], b_ln[i], zt[i], b_zt[i], x1t[i], b_x1t[i], lngb[:, 0, :], lngb[:, 1, :], b_bc)
            if last:
                bo = Buf()
                k.dma("sp", out[(tt - 2) * 128:(tt - 1) * 128, :], x1t[i][:], R=[b_x1t[i]], W=[bo])
            else:
                k.dma("sp", xs[tt * 128:(tt + 1) * 128, :], x1t[i][:], R=[b_x1t[i]], W=[b_xs[tt]])
                if l + 1 < nlayers:
                    if pendT[0] is not None:
                        pt_, pi_ = pendT[0]
                        transpose_tile(x1t[pi_], b_x1t[pi_], pt_, l + 1, 1, 0)
                    pendT[0] = (tt, i)
            if tt == 3:
                dbg_dump("x2t3_%d" % l, x1t[i][:], b_x1t[i], [128, D])
        if pendT[0] is not None:
            pt_, pi_ = pendT[0]
            transpose_tile(x1t[pi_], b_x1t[pi_], pt_, l + 1, 1, 0)
        SO.close()
        SML.close()
        ck("L%d" % l)


def make_in_map(inp, b, cst, rope):
    f = lambda a: np.ascontiguousarray(np.asarray(a, dtype=np.float32))
    m = {
        "x": f(inp["x"][b]), "ctx": f(inp["ctx"][b]), "c": f(inp["c"][b]).reshape(1, D),
        "c_ctx": f(inp["c_ctx"]).reshape(1, D), "b_router": f(inp["b_router"]).reshape(1, NEXP),
        "cst": cst, "rope": rope,
    }
    for n in ("w_ada", "b_ada", "w_in", "lb_logits", "a_norm_g", "rpb", "w_out", "ln1_g", "ln1_b", "w_router",
              "w_gate", "w_up", "w_down", "ln2_g", "ln2_b"):
        m[n] = f(inp[n])
    return m


_NC = None


def kernel(**inputs):
    global _NC
    if _NC is None:
        _NC = build()[0]
    nc = _NC
    cst, rope = host_consts()
    nb = np.asarray(inputs["x"]).shape[0]
    shared = make_in_map(inputs, 0, cst, rope)
    in_maps = []
    for b in range(nb):
        m = dict(shared)
        m["x"] = np.ascontiguousarray(np.asarray(inputs["x"][b], dtype=np.float32))
        m["ctx"] = np.ascontiguousarray(np.asarray(inputs["ctx"][b], dtype=np.float32))
        m["c"] = np.ascontiguousarray(np.asarray(inputs["c"][b], dtype=np.float32)).reshape(1, D)
        in_maps.append(m)
    res = run_bass_kernel_spmd(nc, in_maps, core_ids=list(range(nb)))
    return np.stack([np.asarray(r["out"], dtype=np.float32) for r in res.results], axis=0)
```

```python
import os
import numpy as np
import concourse.bass as bass
import concourse.mybir as mybir
from concourse.bass_utils import run_bass_kernel_spmd

F32 = mybir.dt.float32
BF16 = mybir.dt.bfloat16
AF = mybir.ActivationFunctionType
ALU = mybir.AluOpType
AX = mybir.AxisListType

D = 1024
SEQ = 2048
CTX = 256
T = SEQ + CTX
NT = T // 128
NCH = T // 64
DEPTH = 4
A_Q, A_FF, A_FB, A_I, A_G, B_Q, B_K, B_V = 0, 512, 1024, 1536, 2048, 2560, 3072, 3584
ALPHA = (2 * DEPTH) ** 0.25
LN_EPS = 1e-5
RMS_EPS = 1e-6
NEXP = 16


class Buf:
    __slots__ = ("name", "writer", "readers")

    def __init__(self, name=""):
        self.name = name
        self.writer = None
        self.readers = []


class K:
    def __init__(self, nc):
        self.nc = nc
        self.h = {"pe": nc.tensor, "act": nc.scalar, "dve": nc.vector, "pool": nc.gpsimd, "sp": nc.sync}
        self.sem = {}
        self.cnt = {}
        self.seen = {}
        for e in self.h:
            self.sem[e] = nc.semaphore("s_" + e).__enter__()
            self.cnt[e] = 0
            self.seen[e] = {}
        self.ND = 8
        self.dsem = {}
        self.dcnt = {}
        self.dnext = {}
        for q in ("sp", "pool"):
            self.dsem[q] = [nc.semaphore(f"d_{q}{i}").__enter__() for i in range(self.ND)]
            self.dcnt[q] = [0] * self.ND
            self.dnext[q] = 0
        self.ninst = 0

    def _wait(self, e, ev):
        key, sem, val = ev
        if self.seen[e].get(key, 0) >= val:
            return
        self.h[e].wait_ge(sem, val)
        self.seen[e][key] = val

    def _deps(self, e, R, W):
        mx = {}
        for b in R:
            ev = b.writer
            if ev is not None and (ev[0] not in mx or mx[ev[0]][2] < ev[2]):
                mx[ev[0]] = ev
        for b in W:
            ev = b.writer
            if ev is not None and (ev[0] not in mx or mx[ev[0]][2] < ev[2]):
                mx[ev[0]] = ev
            for ev in b.readers:
                if ev[0] not in mx or mx[ev[0]][2] < ev[2]:
                    mx[ev[0]] = ev
        for key, ev in mx.items():
            if e == "pe" and key == "pe":
                continue
            self._wait(e, ev)

    def _mark(self, ev, R, W):
        for b in W:
            b.writer = ev
            b.readers = []
        for b in R:
            rs = b.readers
            for i, r in enumerate(rs):
                if r[0] == ev[0]:
                    rs[i] = ev
                    break
            else:
                rs.append(ev)

    def op(self, e, fn, R=(), W=()):
        self._deps(e, R, W)
        inst = fn(self.h[e])
        self.cnt[e] += 1
        inst.then_inc(self.sem[e], 1)
        self._mark((e, self.sem[e], self.cnt[e]), R, W)
        self.ninst += 1
        return inst

    def dma(self, q, out, in_, R=(), W=(), **kw):
        i = self.dnext[q]
        self.dnext[q] = (i + 1) % self.ND
        key = f"d_{q}{i}"
        sem = self.dsem[q][i]
        if self.dcnt[q][i] > 0:
            self._wait(q, (key, sem, self.dcnt[q][i]))
        self._deps(q, R, W)
        inst = self.h[q].dma_start(out=out, in_=in_, **kw)
        self.dcnt[q][i] += 16
        inst.then_inc(sem, 16)
        self._mark((key, sem, self.dcnt[q][i]), R, W)
        self.ninst += 1
        return inst

    def barrier(self):
        evs = [(e, self.sem[e], self.cnt[e]) for e in self.h if self.cnt[e] > 0]
        for q in self.dsem:
            for i in range(self.ND):
                if self.dcnt[q][i] > 0:
                    evs.append((f"d_{q}{i}", self.dsem[q][i], self.dcnt[q][i]))
        for e in self.h:
            for ev in evs:
                if not (e == "pe" and ev[0] == "pe"):
                    self._wait(e, ev)


class Rot:
    def __init__(self, items):
        self.items = items
        self.i = 0

    def next(self):
        it = self.items[self.i]
        self.i = (self.i + 1) % len(self.items)
        return it


def host_consts():
    cst = np.zeros((128, 704), np.float32)
    cst[:, 0:128] = np.eye(128, dtype=np.float32)
    J = np.zeros((128, 128), np.float32)
    P = np.zeros((128, 128), np.float32)
    for m in range(128):
        blk, q = divmod(m, 64)
        J[blk * 64 + (63 - q), m] = 1.0
        d = m % 64
        partner = d + 16 if (d % 32) < 16 else d - 16
        P[blk * 64 + partner, m] = 1.0
    cst[:, 128:256] = J
    cst[:, 256:384] = P
    s = np.arange(64)[:, None]
    t = np.arange(64)[None, :]
    cst[0:64, 384:448] = (s <= t).astype(np.float32)
    cst[0:64, 448:512] = (s >= t).astype(np.float32)
    c = np.arange(64)
    cs = np.clip(c - 8, 0, 48)
    inwin = (c[None, :] >= cs[:, None]) & (c[None, :] < cs[:, None] + 16)
    wm = np.where(inwin, 0.0, -1e30).astype(np.float32)
    cst[0:64, 512:576] = wm
    cst[64:128, 512:576] = wm
    cst[:, 576:704] = 1.0 / 128.0
    inv = 10000.0 ** (-np.arange(16, dtype=np.float32) / 16.0)
    tt = np.arange(SEQ)
    pos_r = (tt // 64).astype(np.float32)
    pos_c = (tt % 64).astype(np.float32)
    rope = np.zeros((128, 2 * SEQ), np.float32)
    for p in range(128):
        d = p % 64
        j = d % 16
        pos = pos_r if d < 32 else pos_c
        ang = pos * inv[j]
        sign = -1.0 if (d % 32) < 16 else 1.0
        rope[p, :SEQ] = np.cos(ang)
        rope[p, SEQ:] = sign * np.sin(ang)
    return cst, rope


class _Stop(Exception):
    pass


def build(nlayers=DEPTH, stop=None, dbg=()):
    nc = bass.Bass("TRN2", target_bir_lowering=False)
    k = K(nc)
    dbg_out = {}
    try:
        _build(nc, k, dbg_out, nlayers, stop, dbg)
    except _Stop:
        pass
    k.barrier()
    return nc, dbg_out


def _build(nc, k, dbg_out, nlayers, stop, dbg):
    def ck(name):
        if stop == name:
            raise _Stop()

    def dram(name, shape, dt=F32, kind="ExternalInput"):
        return nc.dram_tensor(name, list(shape), dt, kind=kind).ap()

    uid = [0]

    class Scope:
        def __init__(self):
            self.items = []

        def sb(self, name, shape, dt=F32):
            uid[0] += 1
            t = nc.sbuf_tensor(f"{name}_{uid[0]}", list(shape), dt)
            h = t.__enter__()
            self.items.append(t)
            return h

        def close(self):
            k.barrier()
            for t in reversed(self.items):
                t.__exit__(None, None, None)
            self.items = []

    x_in = dram("x", [SEQ, D])
    ctx_in = dram("ctx", [CTX, D])
    c_in = dram("c", [1, D])
    cc_in = dram("c_ctx", [1, D])
    w_ada = dram("w_ada", [DEPTH, D, 6 * D])
    b_ada = dram("b_ada", [DEPTH, 6 * D])
    w_in = dram("w_in", [DEPTH, D, 4096])
    lb_logits = dram("lb_logits", [2, DEPTH, 512])
    a_norm_g = dram("a_norm_g", [DEPTH, 128])
    rpb = dram("rpb", [DEPTH, 8, 15, 31])
    w_out = dram("w_out", [DEPTH, D, D])
    ln1_g = dram("ln1_g", [DEPTH, D])
    ln1_b = dram("ln1_b", [DEPTH, D])
    w_router = dram("w_router", [D, NEXP])
    b_router = dram("b_router", [1, NEXP])
    w_gate = dram("w_gate", [DEPTH, NEXP, D, 512])
    w_up = dram("w_up", [DEPTH, NEXP, D, 512])
    w_down = dram("w_down", [DEPTH, NEXP, 512, D])
    ln2_g = dram("ln2_g", [DEPTH, D])
    ln2_b = dram("ln2_b", [DEPTH, D])
    cst_in = dram("cst", [128, 704])
    rope_in = dram("rope", [128, 2 * SEQ])
    out = dram("out", [SEQ, D], kind="ExternalOutput")
    xs = dram("xs_scr", [T, D], kind="Internal")
    modd = dram("mod_scr", [DEPTH, 2, 6 * D], kind="Internal")
    rpbpad = dram("rpb_scr", [120, 128], kind="Internal")
    combT_d = dram("combT_scr", [16, T], kind="Internal")

    def dbg_dump(name, src_ap, src_buf, shape, dt=F32):
        if name not in dbg:
            return
        o = dram("dbg_" + name, shape, dt=dt, kind="ExternalOutput")
        b = Buf()
        srcs = src_buf if isinstance(src_buf, list) else [src_buf]
        k.dma("sp", o, src_ap, R=srcs, W=[b])
        dbg_out[name] = b

    ps2 = [nc.psum_tensor(f"ps{i}", [128, 1024], F32).__enter__() for i in range(4)]
    bbank = [Buf(f"bank{i}") for i in range(8)]

    def bank(i):
        return ps2[i // 2][:, (i % 2) * 512:(i % 2) * 512 + 512]

    G = Scope()
    cst = G.sb("cst_sb", [128, 704])
    b_cst = Buf()
    ident32 = cst[:, 0:128]
    J2 = cst[:, 128:256]
    cmf = cst[0:64, 384:448]
    cmb = cst[0:64, 448:512]
    winm = cst[:, 512:576]
    ident16 = G.sb("ident16", [128, 128], BF16)
    Pm16 = G.sb("Pm16", [128, 128], BF16)
    onesm = G.sb("onesm", [128, 128], BF16)
    cmi = G.sb("cmi", [64, 2, 64], mybir.dt.int32)
    rmask = G.sb("rmask", [128, 512], BF16)
    XT = G.sb("XT", [128, 8, T], BF16)
    bXT = [Buf(f"xt{i}") for i in range(NT)]
    modc = G.sb("modc", [128, DEPTH, 2, 6, 8])
    b_modc = Buf()
    lbv = G.sb("lbv", [128, 2, DEPTH, 4])
    oml = G.sb("oml", [128, 2, DEPTH, 4])
    noml = G.sb("noml", [128, 2, DEPTH, 4])
    b_lb = Buf()
    ang = G.sb("ang", [128, DEPTH])
    b_ang = Buf()
    wr32 = G.sb("wr32", [128, 8, NEXP])
    b_wr = Buf()
    brb = G.sb("brb", [128, NEXP])
    b_brb = Buf()
    b_combT = Buf()

    k.dma("sp", cst[:], cst_in[:, :], W=[b_cst])
    k.op("dve", lambda e: e.tensor_copy(out=ident16[:], in_=cst[:, 0:128]), R=[b_cst], W=[b_cst])
    k.op("dve", lambda e: e.tensor_copy(out=Pm16[:], in_=cst[:, 256:384]), R=[b_cst], W=[b_cst])
    k.op("dve", lambda e: e.tensor_copy(out=onesm[:], in_=cst[:, 576:704]), R=[b_cst], W=[b_cst])
    k.op("dve", lambda e: e.tensor_copy(out=cmi[:].rearrange("p a b -> p (a b)"), in_=cst[0:64, 384:512]), R=[b_cst], W=[b_cst])
    k.op("pool", lambda e: e.memset(rmask[:], 1.0), W=[b_cst])
    k.op("pool", lambda e: e.memset(rmask[:, 0:512:64], 0.0), R=[b_cst], W=[b_cst])
    k.dma("sp", wr32[:], w_router.rearrange("(ch p) e -> p ch e", p=128), W=[b_wr])
    k.dma("sp", brb[:], bass.AP(tensor=b_router.tensor, offset=0, ap=[[0, 128], [1, NEXP]]), W=[b_brb])

    ck("P0")
    PR = Scope()
    craw = PR.sb("craw", [128, 2, 8])
    csil = PR.sb("csil", [128, 2, 8])
    b_c = Buf()
    k.dma("sp", craw[:, 0, :], c_in.rearrange("o (ch p) -> p (o ch)", p=128), W=[b_c], allow_slow_non_contiguous=True)
    k.dma("sp", craw[:, 1, :], cc_in.rearrange("o (ch p) -> p (o ch)", p=128), W=[b_c], allow_slow_non_contiguous=True)
    k.op("act", lambda e: e.activation(out=csil[:], in_=craw[:], func=AF.Silu), R=[b_c], W=[b_c])
    ck("P1")
    wa = [PR.sb(f"wa{i}", [128, 8, 512]) for i in range(2)]
    b_wa = [Buf(), Buf()]
    modrow = PR.sb("modrow", [2, 6 * D])
    b_modrow = Buf()
    badar = PR.sb("badar", [2, 6 * D])
    b_bada = Buf()
    b_modd_all = []
    mt48 = PR.sb("mt48", [48, 2, 128])
    b_mt48 = Buf()
    for l in range(nlayers):
        for j in range(2):
            k.dma("sp", badar[j:j + 1, :], b_ada[l:l + 1, :], W=[b_bada])
        for cb in range(12):
            i = (l * 12 + cb) % 2
            k.dma("sp", wa[i][:], w_ada[l, :, cb * 512:(cb + 1) * 512].rearrange("(ch p) c -> p ch c", p=128), W=[b_wa[i]])
            pb = 6 + (cb % 2)
            for ch in range(8):
                k.op("pe", lambda e: e.matmul(bank(pb)[0:2, :], lhsT=csil[:, :, ch], rhs=wa[i][:, ch, :],
                                              start=(ch == 0), stop=(ch == 7)), R=[b_c, b_wa[i]], W=[bbank[pb]])
            k.op("dve", lambda e: e.tensor_tensor(out=modrow[:, cb * 512:(cb + 1) * 512], in0=bank(pb)[0:2, :],
                                                  in1=badar[:, cb * 512:(cb + 1) * 512], op=ALU.add),
                 R=[bbank[pb], b_bada], W=[b_modrow])
        ck("P2")
        b_modd = Buf()
        b_modd_all.append(b_modd)
        k.dma("sp", modd[l, :, :], modrow[:], R=[b_modrow], W=[b_modd])
        for j in range(2):
            k.dma("sp", mt48[:, j, :], modd[l, j, :].rearrange("(r p) -> r p", p=128), R=[b_modd], W=[b_mt48])
            k.op("pe", lambda e: e.transpose(out=bank(5)[:, j * 48:(j + 1) * 48], in_=mt48[:, j, :], identity=ident32[0:48, 0:48]),
                 R=[b_mt48, b_cst], W=[bbank[5]])
        k.op("dve", lambda e: e.tensor_copy(out=modc[:, l, :, :, :].rearrange("p j w c -> p (j w c)"), in_=bank(5)[:, 0:96]),
             R=[bbank[5]], W=[b_modc])
        for w_ in (1, 4):
            k.op("dve", lambda e: e.tensor_scalar(out=modc[:, l, :, w_, :], in0=modc[:, l, :, w_, :], scalar1=1.0, scalar2=None, op0=ALU.add),
                 R=[b_modc], W=[b_modc])
    if "modrow" in dbg:
        dbg_dump("modrow", modrow[:], b_modrow, [2, 6 * D])
    ck("P3")
    lraw = PR.sb("lraw", [128, 2, DEPTH, 4])
    lexp = PR.sb("lexp", [128, 2, DEPTH, 4])
    lsum = PR.sb("lsum", [128, 2, 4])
    lmx = PR.sb("lmx", [128, 2, 4])
    b_l = Buf()
    for d_ in range(2):
        for l_ in range(DEPTH):
            k.dma("sp", lraw[:, d_, l_, :], lb_logits[d_, l_, :].rearrange("(h p) -> p h", p=128), W=[b_l], allow_slow_non_contiguous=True)
    k.op("dve", lambda e: e.tensor_tensor(out=lmx[:], in0=lraw[:, :, 0, :], in1=lraw[:, :, 1, :], op=ALU.max), R=[b_l], W=[b_l])
    k.op("dve", lambda e: e.tensor_tensor(out=lmx[:], in0=lmx[:], in1=lraw[:, :, 2, :], op=ALU.max), R=[b_l], W=[b_l])
    k.op("dve", lambda e: e.tensor_tensor(out=lmx[:], in0=lmx[:], in1=lraw[:, :, 3, :], op=ALU.max), R=[b_l], W=[b_l])
    for l_ in range(DEPTH):
        k.op("dve", lambda e: e.tensor_tensor(out=lexp[:, :, l_, :], in0=lraw[:, :, l_, :], in1=lmx[:], op=ALU.subtract), R=[b_l], W=[b_l])
    k.op("act", lambda e: e.activation(out=lexp[:], in_=lexp[:], func=AF.Exp), R=[b_l], W=[b_l])
    k.op("dve", lambda e: e.tensor_tensor(out=lsum[:], in0=lexp[:, :, 0, :], in1=lexp[:, :, 1, :], op=ALU.add), R=[b_l], W=[b_l])
    k.op("dve", lambda e: e.tensor_tensor(out=lsum[:], in0=lsum[:], in1=lexp[:, :, 2, :], op=ALU.add), R=[b_l], W=[b_l])
    k.op("dve", lambda e: e.tensor_tensor(out=lsum[:], in0=lsum[:], in1=lexp[:, :, 3, :], op=ALU.add), R=[b_l], W=[b_l])
    k.op("dve", lambda e: e.reciprocal(out=lsum[:], in_=lsum[:]), R=[b_l], W=[b_l])
    k.op("dve", lambda e: e.memset(lbv[:, :, 0, :], 0.0), R=[b_l], W=[b_lb])
    for l_ in range(1, DEPTH):
        k.op("dve", lambda e: e.tensor_tensor(out=lexp[:, :, l_, :], in0=lexp[:, :, l_, :], in1=lsum[:], op=ALU.mult), R=[b_l], W=[b_l])
        k.op("dve", lambda e: e.tensor_tensor(out=lbv[:, :, l_, :], in0=lbv[:, :, l_ - 1, :], in1=lexp[:, :, l_, :], op=ALU.add), R=[b_l, b_lb], W=[b_lb])
    k.op("dve", lambda e: e.tensor_scalar(out=oml[:], in0=lbv[:], scalar1=-1.0, scalar2=1.0, op0=ALU.mult, op1=ALU.add), R=[b_lb], W=[b_lb])
    k.op("dve", lambda e: e.tensor_scalar(out=noml[:], in0=lbv[:], scalar1=1.0, scalar2=-1.0, op0=ALU.mult, op1=ALU.add), R=[b_lb], W=[b_lb])
    ck("P4")
    k.dma("sp", ang[:], a_norm_g.rearrange("l p -> p l"), W=[b_ang], allow_slow_non_contiguous=True)
    zt = PR.sb("zt", [120, 128])
    b_zt = Buf()
    b_rpbpad = Buf()
    k.op("pool", lambda e: e.memset(zt[:], 0.0), W=[b_zt])
    k.dma("sp", rpbpad[:, :], zt[:], R=[b_zt], W=[b_rpbpad])
    dbg_dump("modc", modc[:].rearrange("p l j w c -> p (l j w c)"), b_modc, [128, DEPTH * 96])
    dbg_dump("lbv", lbv[:].rearrange("p a b c -> p (a b c)"), b_lb, [128, 32])
    PR.close()
    ck("P")

    psT = Rot([0, 1])

    def xt_bufs(t0, n):
        return [bXT[i] for i in range(t0 // 128, (t0 + n + 127) // 128)]

    def transpose_tile(xtile, bx, tt, l, which_sc, which_sh, tf32=None, b_tf=None):
        j = 1 if tt < 2 else 0
        for half in range(2):
            pb = psT.next()
            for q in range(4):
                ch = half * 4 + q
                k.op("pe", lambda e: e.transpose(out=bank(pb)[:, q * 128:(q + 1) * 128], in_=xtile[:, ch * 128:(ch + 1) * 128],
                                                 identity=ident32), R=[bx, b_cst], W=[bbank[pb]])
            for q in range(4):
                ch = half * 4 + q
                sc = modc[:, l, j, which_sc, ch:ch + 1]
                sh = modc[:, l, j, which_sh, ch:ch + 1]
                if tf32 is None:
                    k.op("act", lambda e: e.activation(out=XT[:, ch, tt * 128:(tt + 1) * 128], in_=bank(pb)[:, q * 128:(q + 1) * 128],
                                                       func=AF.Identity, scale=sc, bias=sh), R=[bbank[pb], b_modc], W=[bXT[tt]])
                else:
                    k.op("act", lambda e: e.activation(out=tf32[:, ch, :], in_=bank(pb)[:, q * 128:(q + 1) * 128],
                                                       func=AF.Identity, scale=sc, bias=sh), R=[bbank[pb], b_modc], W=[b_tf])
                    k.op("act", lambda e: e.activation(out=XT[:, ch, tt * 128:(tt + 1) * 128], in_=bank(pb)[:, q * 128:(q + 1) * 128],
                                                       func=AF.Identity, scale=sc, bias=sh), R=[bbank[pb], b_modc], W=[bXT[tt]])

    def proj_fm(pb, W, bW, wsl, t0, n):
        for ch in range(8):
            k.op("pe", lambda e: e.matmul(bank(pb)[:, 0:n], lhsT=W[(slice(None), ch) + tuple(wsl)], rhs=XT[:, ch, t0:t0 + n],
                                          start=(ch == 0), stop=(ch == 7)), R=[bW] + xt_bufs(t0, n), W=[bbank[pb]])

    BLKS = [(0, 512), (512, 512), (1024, 512), (1536, 512), (2048, 256)]

    S0 = Scope()
    xin = [S0.sb(f"xin{i}", [128, D]) for i in range(2)]
    b_xin = [Buf(), Buf()]
    for tt in range(NT):
        i = tt % 2
        src = ctx_in[tt * 128:(tt + 1) * 128, :] if tt < 2 else x_in[(tt - 2) * 128:(tt - 1) * 128, :]
        k.dma("sp", xin[i][:], src, W=[b_xin[i]])
        transpose_tile(xin[i], b_xin[i], tt, 0, 1, 0)
    S0.close()
    if "xt0" in dbg:
        dbg_dump("xt0", XT[:, :, :].rearrange("p c t -> p (c t)"), bXT, [128, 8 * T], dt=BF16)

    ck("T0")
    b_xs = [Buf(f"xs{i}") for i in range(NT)]

    for l in range(nlayers):
        last = (l == DEPTH - 1)
        MS = Scope()
        MIX = MS.sb("MIX", [128, 8, T], BF16)
        b_mix = [Buf(f"mix{i}") for i in range(8)]

        SA = Scope()
        Wh = SA.sb("Wh", [128, 8, 5, 128], BF16)
        b_Wh = Buf()
        nT = 2
        QSb = [SA.sb(f"QSb{i}", [128, 512]) for i in range(nT)]
        Xb2 = [[SA.sb(f"Xb{d}{i}", [128, 512]) for i in range(nT)] for d in range(2)]
        bX2 = [[Buf() for _ in range(nT)] for d in range(2)]
        Fb = [SA.sb(f"Fb{i}", [128, 512]) for i in range(nT)]
        Cb = [SA.sb(f"Cb{i}", [128, 512]) for i in range(nT)]
        Db = [SA.sb(f"Db{i}", [128, 512]) for i in range(nT)]
        Eb = [SA.sb(f"Eb{i}", [128, 512]) for i in range(nT)]
        D2b = [SA.sb(f"D2b{i}", [128, 512]) for i in range(nT)]
        E2b = [SA.sb(f"E2b{i}", [128, 512]) for i in range(nT)]
        bD2 = [Buf() for _ in range(nT)]
        bE2 = [Buf() for _ in range(nT)]
        bQS = [Buf() for _ in range(nT)]
        bX = [Buf() for _ in range(nT)]
        bF = [Buf() for _ in range(nT)]
        bC = [Buf() for _ in range(nT)]
        bD = [Buf() for _ in range(nT)]
        bE = [Buf() for _ in range(nT)]
        QIN = [SA.sb(f"QIN{d}", [128, T], BF16) for d in range(2)]
        QS2 = [SA.sb(f"QS2{d}", [128, T], BF16) for d in range(2)]
        KDEC = [SA.sb(f"KDEC{d}", [128, T], BF16) for d in range(2)]
        KD2 = [SA.sb(f"KD2{d}", [128, T], BF16) for d in range(2)]
        b_q3 = [Buf(), Buf()]
        GS = SA.sb("GS", [128, T], BF16)
        b_gs = Buf()
        TOT = [SA.sb(f"TOT{d}", [128, NCH]) for d in range(2)]
        b_tot = [Buf(), Buf()]
        V64 = SA.sb("V64", [64, NCH, 128], BF16)
        b_v64 = Buf()
        OD = [SA.sb(f"OD{d}", [128, T]) for d in range(2)]
        b_od = [Buf(), Buf()]
        S32 = [SA.sb(f"S32{d}", [128, 128]) for d in range(2)]
        S16 = [SA.sb(f"S16{d}", [128, 128], BF16) for d in range(2)]
        S16b = [[SA.sb(f"S16b{d}{j}", [128, 128], BF16) for j in range(2)] for d in range(2)]
        b_s16b = [[Buf(), Buf()] for d in range(2)]
        b_s32 = [Buf(), Buf()]
        b_s16 = [Buf(), Buf()]
        kdT = [SA.sb(f"kdT{i}", [64, 128], BF16) for i in range(4)]
        b_kdT = [Buf() for _ in range(4)]
        scm = [SA.sb(f"scm{i}", [64, 64], BF16) for i in range(4)]
        b_scm = [Buf() for _ in range(4)]
        sqb = [SA.sb(f"sqb{i}", [128, 512], BF16) for i in range(2)]
        b_sq = [Buf(), Buf()]
        pA = Rot([2, 3, 4, 5])
        for h in range(0 if not os.environ.get('SKIPA') else 4, 4):
            for fi, c0 in enumerate([A_Q, A_G, A_FF, A_FB, A_I]):
                k.dma("pool", Wh[:, :, fi, :], w_in[l, :, c0 + h * 128:c0 + h * 128 + 128].rearrange("(ch p) c -> p ch c", p=128), W=[b_Wh])
            ck("A0")
            for bi, (t0, n) in enumerate(BLKS):
                i = bi % nT
                nch = n // 64
                ck0 = t0 // 64
                if bi == 1:
                    ck("A2")
                pb = pA.next()
                proj_fm(pb, Wh, b_Wh, (0, slice(None)), t0, n)
                k.op("act", lambda e: e.activation(out=QSb[i][:, 0:n], in_=bank(pb)[:, 0:n], func=AF.Silu), R=[bbank[pb]], W=[bQS[i]])
                pb = pA.next()
                proj_fm(pb, Wh, b_Wh, (1, slice(None)), t0, n)
                k.op("act", lambda e: e.activation(out=GS[:, t0:t0 + n], in_=bank(pb)[:, 0:n], func=AF.Silu), R=[bbank[pb]], W=[b_gs])
                ck("A1")
                for d in range(2):
                    pb = pA.next()
                    proj_fm(pb, Wh, b_Wh, (2 + d, slice(None)), t0, n)
                    k.op("act", lambda e: e.activation(out=Xb2[d][i][:, 0:n], in_=bank(pb)[:, 0:n], func=AF.Sigmoid), R=[bbank[pb]], W=[bX2[d][i]])
                for d in range(2):
                    lb_ap = lbv[:, d, l, h:h + 1]
                    oml_ap = oml[:, d, l, h:h + 1]
                    noml_ap = noml[:, d, l, h:h + 1]
                    X_, F_, C_, D_, E_ = Xb2[d][i][:, 0:n], Fb[i][:, 0:n], Cb[i][:, 0:n], Db[i][:, 0:n], Eb[i][:, 0:n]
                    bX = bX2[d]
                    k.op("dve", lambda e: e.tensor_scalar(out=F_, in0=X_, scalar1=1e-6, scalar2=None, op0=ALU.max), R=[bX[i]], W=[bF[i]])
                    k.op("act", lambda e: e.activation(out=F_, in_=F_, func=AF.Ln, scale=oml_ap, bias=lb_ap), R=[bF[i], b_lb], W=[bF[i]])
                    k.op("pool", lambda e: e.tensor_scalar(out=X_, in0=X_, scalar1=noml_ap, scalar2=oml_ap, op0=ALU.mult, op1=ALU.add),
                         R=[bX[i], b_lb], W=[bX[i]])
                    k.op("dve", lambda e: e.tensor_tensor_scan(out=C_, data0=rmask[:, 0:n], data1=F_, initial=0.0, op0=ALU.mult, op1=ALU.add),
                         R=[bF[i], b_cst], W=[bC[i]])
                    C3 = C_.rearrange("p (c s) -> p c s", s=64)
                    Ctb = C3[:, :, 63:64].broadcast_to([128, nch, 64])
                    D3 = D_.rearrange("p (c s) -> p c s", s=64)
                    D2_ = D2b[i][:, 0:n]
                    D23 = D2_.rearrange("p (c s) -> p c s", s=64)
                    if d == 0:
                        Cmb = C3[:, :, 31:32].broadcast_to([128, nch, 64])
                        k.op("dve", lambda e: e.tensor_tensor(out=D3, in0=C3, in1=Ctb, op=ALU.subtract), R=[bC[i]], W=[bD[i]])
                        k.op("pool", lambda e: e.tensor_tensor(out=D23, in0=C3, in1=Cmb, op=ALU.subtract), R=[bC[i]], W=[bD2[i]])
                        a1, a1b, a1s = C_, bC[i], 1.0
                        a3, a3b, a3s = D_, bD[i], -1.0
                    else:
                        k.op("dve", lambda e: e.tensor_tensor(out=F_, in0=C_, in1=F_, op=ALU.subtract), R=[bC[i], bF[i]], W=[bF[i]])
                        F3 = F_.rearrange("p (c s) -> p c s", s=64)
                        Pmb = F3[:, :, 32:33].broadcast_to([128, nch, 64])
                        k.op("dve", lambda e: e.tensor_tensor(out=D3, in0=Ctb, in1=F3, op=ALU.subtract), R=[bC[i], bF[i]], W=[bD[i]])
                        k.op("pool", lambda e: e.tensor_tensor(out=D23, in0=Pmb, in1=F3, op=ALU.subtract), R=[bF[i]], W=[bD2[i]])
                        a1, a1b, a1s = D_, bD[i], 1.0
                        a3, a3b, a3s = F_, bF[i], 1.0
                    k.op("act", lambda e: e.activation(out=TOT[d][:, ck0:ck0 + nch], in_=C_[:, 63:n:64], func=AF.Exp), R=[bC[i]], W=[b_tot[d]])
                    k.op("act", lambda e: e.activation(out=E_, in_=a1, func=AF.Exp, scale=a1s), R=[a1b], W=[bE[i]])
                    k.op("dve", lambda e: e.tensor_tensor(out=QIN[d][:, t0:t0 + n], in0=QSb[i][:, 0:n], in1=E_, op=ALU.mult),
                         R=[bQS[i], bE[i]], W=[b_q3[d]])
                    k.op("act", lambda e: e.activation(out=E_, in_=a3, func=AF.Exp, scale=a3s), R=[a3b], W=[bE[i]])
                    k.op("dve", lambda e: e.tensor_tensor(out=KDEC[d][:, t0:t0 + n], in0=X_, in1=E_, op=ALU.mult),
                         R=[bX[i], bE[i]], W=[b_q3[d]])
                    E2_ = E2b[i][:, 0:n]
                    k.op("act", lambda e: e.activation(out=E2_, in_=D2_, func=AF.Exp, scale=1.0), R=[bD2[i]], W=[bE2[i]])
                    k.op("pool", lambda e: e.tensor_tensor(out=QS2[d][:, t0:t0 + n], in0=QSb[i][:, 0:n], in1=E2_, op=ALU.mult),
                         R=[bQS[i], bE2[i]], W=[b_q3[d]])
                    k.op("act", lambda e: e.activation(out=E_, in_=D2_, func=AF.Exp, scale=-1.0), R=[bD2[i]], W=[bE[i]])
                    k.op("dve", lambda e: e.tensor_tensor(out=KD2[d][:, t0:t0 + n], in0=X_, in1=E_, op=ALU.mult),
                         R=[bX[i], bE[i]], W=[b_q3[d]])
            ck("A3")
            for c4 in range(NCH // 4):
                pb = pA.next()
                for q in range(4):
                    c = c4 * 4 + q
                    for ch in range(8):
                        k.op("pe", lambda e: e.matmul(bank(pb)[0:64, q * 128:(q + 1) * 128], lhsT=XT[:, ch, c * 64:(c + 1) * 64],
                                                      rhs=Wh[:, ch, 4, :], start=(ch == 0), stop=(ch == 7)),
                             R=[b_Wh] + xt_bufs(c * 64, 64), W=[bbank[pb]])
                k.op("act", lambda e: e.activation(out=V64[:, c4 * 4:(c4 + 1) * 4, :].rearrange("p c v -> p (c v)"), in_=bank(pb)[0:64, :],
                                                   func=AF.Copy), R=[bbank[pb]], W=[b_v64])
            ck("A4")
            for d in range(2):
                k.op("pool", lambda e: e.memset(S32[d][:], 0.0), W=[b_s32[d]])
                k.op("pool", lambda e: e.memset(S16[d][:], 0.0), W=[b_s16[d]])
                k.op("pool", lambda e: e.memset(S16b[d][0][:], 0.0), W=[b_s16b[d][0]])
            orders = [list(range(NCH)), [3, 2, 1, 0] + list(range(NCH - 1, 3, -1))]
            cms = [cmf, cmb]
            ri = int(os.environ.get('RI0', 0))
            b_pt = [bbank[0], bbank[4]]
            b_psc = [bbank[1], bbank[5]]
            b_po = [bbank[2], bbank[6]]
            b_pds = [bbank[3], bbank[7]]
            for step in range(int(os.environ.get('NSTEPS', NCH))):
                for d in [int(x) for x in os.environ.get('DIRS', '01')]:
                    c = orders[d][step]
                    cs_ = slice(c * 64, (c + 1) * 64)
                    r4 = ri % 4
                    ri += 1
                    pt_ap = bank(4 * d).bitcast(BF16)[0:64, 0:128]
                    psc_ap = bank(4 * d + 1)[0:64, 0:64]
                    po_ap = bank(4 * d + 2)[:, 0:64]
                    pds_ap = bank(4 * d + 3)[:, 0:128]
                    k.op("pe", lambda e: e.transpose(out=pt_ap, in_=KDEC[d][:, cs_], identity=ident16[:]), R=[b_q3[d], b_cst], W=[b_pt[d]])
                    if step == 0 and d == 0: ck("R0")
                    k.op("act", lambda e: e.activation(out=kdT[r4][:], in_=pt_ap, func=AF.Copy), R=[b_pt[d]], W=[b_kdT[r4]])
                    if step == 0 and d == 0: ck("R1")
                    k.op("pe", lambda e: e.matmul(psc_ap, lhsT=KD2[d][:, cs_], rhs=QS2[d][:, cs_], start=True, stop=True),
                         R=[b_q3[d]], W=[b_psc[d]])
                    if step == 0 and d == 0: ck("R2")
                    k.op("pool", lambda e: e.memset(scm[r4][:], 0.0), W=[b_scm[r4]])
                    k.op("dve", lambda e: e.copy_predicated(out=scm[r4][:], mask=cmi[:, d, :], data=psc_ap), R=[b_psc[d], b_cst, b_scm[r4]], W=[b_scm[r4]])
                    if step == 0 and d == 0: ck("R3")
                    if os.environ.get("S32MASTER"):
                        k.op("pe", lambda e: e.matmul(po_ap, lhsT=S16[d][:], rhs=QIN[d][:, cs_], start=True, stop=False),
                             R=[b_s16[d], b_q3[d]], W=[b_po[d]])
                    else:
                        k.op("pe", lambda e: e.matmul(po_ap, lhsT=S16b[d][step % 2][:], rhs=QIN[d][:, cs_], start=True, stop=False),
                             R=[b_s16b[d][step % 2], b_q3[d]], W=[b_po[d]])
                    if step == 0 and d == 0: ck("R4")
                    k.op("pe", lambda e: e.matmul(po_ap, lhsT=V64[:, c, :], rhs=scm[r4][:], start=False, stop=True),
                         R=[b_v64, b_scm[r4]], W=[b_po[d]])
                    if step == 0 and d == 0: ck("R5")
                    k.op("act", lambda e: e.activation(out=OD[d][:, cs_], in_=po_ap, func=AF.Copy), R=[b_po[d]], W=[b_od[d]])
                    if step == 0 and d == 0: ck("R6")
                    k.op("pe", lambda e: e.matmul(pds_ap, lhsT=kdT[r4][:], rhs=V64[:, c, :], start=True, stop=True),
                         R=[b_kdT[r4], b_v64], W=[b_pds[d]])
                    if step == 0 and d == 0: ck("R7")
                    if os.environ.get("S32MASTER"):
                        k.op("dve", lambda e: e.scalar_tensor_tensor(out=S16[d][:], in0=S32[d][:], scalar=TOT[d][:, c:c + 1], in1=pds_ap,
                                                                     op0=ALU.mult, op1=ALU.add), R=[b_s32[d], b_tot[d], b_pds[d]], W=[b_s16[d]])
                        k.op("dve", lambda e: e.scalar_tensor_tensor(out=S32[d][:], in0=S32[d][:], scalar=TOT[d][:, c:c + 1], in1=pds_ap,
                                                                     op0=ALU.mult, op1=ALU.add), R=[b_s32[d], b_tot[d], b_pds[d]], W=[b_s32[d]])
                    else:
                        sn = S16b[d][(step + 1) % 2]
                        so = S16b[d][step % 2]
                        k.op("dve", lambda e: e.scalar_tensor_tensor(out=sn[:], in0=so[:], scalar=TOT[d][:, c:c + 1], in1=pds_ap,
                                                                     op0=ALU.mult, op1=ALU.add), R=[b_s16b[d][step % 2], b_tot[d], b_pds[d]], W=[b_s16b[d][(step + 1) % 2]])
                    if step == 0 and d == 0: ck("R8")
                    pass
                    if step == 0 and d == 0: ck("R9")
            ck("A5")
            for bi, (t0, n) in enumerate(BLKS):
                i = bi % 2
                O_ = Cb[i][:, 0:n]
                k.op("pool", lambda e: e.tensor_tensor(out=O_, in0=OD[0][:, t0:t0 + n], in1=OD[1][:, t0:t0 + n], op=ALU.add),
                     R=[b_od[0], b_od[1]], W=[bC[i]])
                k.op("act", lambda e: e.activation(out=sqb[i][:, 0:n], in_=O_, func=AF.Square), R=[bC[i]], W=[b_sq[i]])
                pb = pA.next()
                k.op("pe", lambda e: e.matmul(bank(pb)[:, 0:n], lhsT=onesm[:], rhs=sqb[i][:, 0:n], start=True, stop=True),
                     R=[b_sq[i], b_cst], W=[bbank[pb]])
                R_ = Db[i][:, 0:n]
                k.op("act", lambda e: e.activation(out=R_, in_=bank(pb)[:, 0:n], func=AF.Ln, bias=RMS_EPS, scale=1.0), R=[bbank[pb]], W=[bD[i]])
                k.op("act", lambda e: e.activation(out=R_, in_=R_, func=AF.Exp, scale=-0.5), R=[bD[i]], W=[bD[i]])
                k.op("dve", lambda e: e.tensor_tensor(out=O_, in0=O_, in1=R_, op=ALU.mult), R=[bC[i], bD[i]], W=[bC[i]])
                k.op("dve", lambda e: e.scalar_tensor_tensor(out=MIX[:, h, t0:t0 + n], in0=O_, scalar=ang[:, l:l + 1], in1=GS[:, t0:t0 + n],
                                                             op0=ALU.mult, op1=ALU.mult), R=[bC[i], b_ang, b_gs], W=[b_mix[h]])
        SA.close()
        dbg_dump("mixA", MIX[:, 0:4, :].rearrange("p c t -> p (c t)"), b_mix[0:4], [128, 4 * T], dt=BF16)
        ck("A")

        SB = Scope()
        ropeS = SB.sb("rope_sb", [128, 2 * SEQ])
        b_rope = Buf()
        k.dma("sp", ropeS[:], rope_in[:, :], W=[b_rope])
        HB = []
        for par in range(2):
            HB.append(dict(
                Wq=SB.sb(f"Wq{par}", [128, 8, 3, 128], BF16), b_Wq=Buf(),
                Qp=SB.sb(f"Qp{par}", [128, T], BF16), Kp=SB.sb(f"Kp{par}", [128, T], BF16),
                Qr=SB.sb(f"Qr{par}", [128, SEQ], BF16), Kr=SB.sb(f"Kr{par}", [128, SEQ], BF16),
                b_Qp=Buf(), b_Kp=Buf(), b_Qr=Buf(), b_Kr=Buf(),
                Va=SB.sb(f"Va{par}", [128, NT, 128], BF16), Vb=SB.sb(f"Vb{par}", [128, NT - 1, 128], BF16),
                b_Va=Buf(), b_Vb=Buf(),
                Hh=SB.sb(f"Hh{par}", [128, 15, 64]), b_Hh=Buf(),
                Tb=SB.sb(f"Tb{par}", [128, 960], BF16), b_Tb=Buf()))
        rp_sb = SB.sb("rp_sb", [120, 31])
        b_rp = Buf()
        rt1 = [SB.sb(f"rt1{i}", [128, 512]) for i in range(2)]
        rt2 = [SB.sb(f"rt2{i}", [128, 512]) for i in range(2)]
        b_rt1 = [Buf(), Buf()]
        b_rt2 = [Buf(), Buf()]
        Pt_ = [SB.sb(f"Pt{i}", [128, 768], BF16) for i in range(3)]
        PTs = [SB.sb(f"PTs{i}", [128, 768], BF16) for i in range(3)]
        b_P = [Buf(), Buf(), Buf()]
        b_PTs = [Buf(), Buf(), Buf()]
        stt_ = [SB.sb(f"stt{i}", [128, 4]) for i in range(3)]
        b_st = [Buf(), Buf(), Buf()]
        Dg = [SB.sb(f"Dg{i}", [128, 128], BF16) for i in range(3)]
        b_Dg = [Buf(), Buf(), Buf()]
        pB = Rot([6, 7])
        k.dma("sp", rp_sb[:], rpb[l].rearrange("h r c -> (h r) c"), W=[b_rp])
        k.dma("sp", rpbpad[:, 48:79], rp_sb[:], R=[b_rp], W=[b_rpbpad])
        rope_ctr = [0]

        def setup_gen(hp):
            hb = HB[hp % 2]
            Wq, b_Wq, Qp, Kp, Qr, Kr = hb["Wq"], hb["b_Wq"], hb["Qp"], hb["Kp"], hb["Qr"], hb["Kr"]
            b_Qp, b_Kp, b_Qr, b_Kr = hb["b_Qp"], hb["b_Kp"], hb["b_Qr"], hb["b_Kr"]
            Va, Vb, b_Va, b_Vb, Hh, b_Hh, Tb, b_Tb = hb["Va"], hb["Vb"], hb["b_Va"], hb["b_Vb"], hb["Hh"], hb["b_Hh"], hb["Tb"], hb["b_Tb"]
            for fi, c0 in enumerate([B_Q, B_K, B_V]):
                k.dma("pool", Wq[:, :, fi, :], w_in[l, :, c0 + hp * 128:c0 + hp * 128 + 128].rearrange("(ch p) c -> p ch c", p=128), W=[b_Wq])
            for hh in range(2):
                src = bass.AP(tensor=rpbpad.tensor, offset=((2 * hp + hh) * 15) * 128, ap=[[1, 64], [128, 15], [1, 64]])
                k.dma("sp", Hh[hh * 64:(hh + 1) * 64, :, :], src, R=[b_rpbpad], W=[b_Hh])
            Hf = Hh[:].rearrange("p a b -> p (a b)")
            for (c0, ncol) in ((0, 512), (512, 448)):
                pb = pB.next()
                k.op("pe", lambda e: e.matmul(bank(pb)[:, 0:ncol], lhsT=J2, rhs=Hf[:, c0:c0 + ncol], start=True, stop=True),
                     R=[b_cst, b_Hh], W=[bbank[pb]])
                ndr = ncol // 64
                k.op("dve", lambda e: e.tensor_tensor(out=Tb[:, c0:c0 + ncol].rearrange("p (a b) -> p a b", b=64),
                                                      in0=bank(pb)[:, 0:ncol].rearrange("p (a b) -> p a b", b=64),
                                                      in1=winm.unsqueeze(1).broadcast_to([128, ndr, 64]), op=ALU.add),
                     R=[bbank[pb], b_cst], W=[b_Tb])
                yield
            for bi, (t0, n) in enumerate(BLKS):
                pb = pB.next()
                proj_fm(pb, Wq, b_Wq, (0, slice(None)), t0, n)
                k.op("act", lambda e: e.activation(out=Qp[:, t0:t0 + n], in_=bank(pb)[:, 0:n], func=AF.Copy, scale=0.125), R=[bbank[pb]], W=[b_Qp])
                pb = pB.next()
                proj_fm(pb, Wq, b_Wq, (1, slice(None)), t0, n)
                k.op("dve", lambda e: e.tensor_copy(out=Kp[:, t0:t0 + n], in_=bank(pb)[:, 0:n]), R=[bbank[pb]], W=[b_Kp])
                yield
            for lbk in range(4):
                t0 = CTX + lbk * 512
                tl = lbk * 512
                for (src_, bsrc, dst_, bdst) in ((Qp, b_Qp, Qr, b_Qr), (Kp, b_Kp, Kr, b_Kr)):
                    i = rope_ctr[0] % 2
                    rope_ctr[0] += 1
                    pb = pB.next()
                    k.op("pe", lambda e: e.matmul(bank(pb)[:, :], lhsT=Pm16[:], rhs=src_[:, t0:t0 + 512], start=True, stop=True),
                         R=[b_cst, bsrc], W=[bbank[pb]])
                    k.op("pool", lambda e: e.tensor_tensor(out=rt1[i][:], in0=src_[:, t0:t0 + 512], in1=ropeS[:, tl:tl + 512], op=ALU.mult),
                         R=[bsrc, b_rope], W=[b_rt1[i]])
                    k.op("dve", lambda e: e.tensor_tensor(out=rt2[i][:], in0=bank(pb)[:, :], in1=ropeS[:, SEQ + tl:SEQ + tl + 512], op=ALU.mult),
                         R=[bbank[pb], b_rope], W=[b_rt2[i]])
                    k.op("pool", lambda e: e.tensor_tensor(out=dst_[:, tl:tl + 512], in0=rt1[i][:], in1=rt2[i][:], op=ALU.add),
                         R=[b_rt1[i], b_rt2[i]], W=[bdst])
                    yield
            for (Vt, bV, ntile, off) in ((Va, b_Va, NT, 0), (Vb, b_Vb, NT - 1, 64)):
                j0 = 0
                while j0 < ntile:
                    g = min(4, ntile - j0)
                    pb = pB.next()
                    for q in range(g):
                        tk = off + (j0 + q) * 128
                        for ch in range(8):
                            k.op("pe", lambda e: e.matmul(bank(pb)[:, q * 128:(q + 1) * 128], lhsT=XT[:, ch, tk:tk + 128], rhs=Wq[:, ch, 2, :],
                                                          start=(ch == 0), stop=(ch == 7)), R=[b_Wq] + xt_bufs(tk, 128), W=[bbank[pb]])
                    k.op("act", lambda e: e.activation(out=Vt[:, j0:j0 + g, :].rearrange("p a b -> p (a b)"), in_=bank(pb)[:, 0:g * 128], func=AF.Copy),
                         R=[bbank[pb]], W=[bV])
                    j0 += g
                    yield

        gens = [setup_gen(hp_) for hp_ in range(4)]
        for hp in range(4):
            for _ in gens[hp]:
                pass
            hb = HB[hp % 2]
            Qp, Kp, Qr, Kr = hb["Qp"], hb["Kp"], hb["Qr"], hb["Kr"]
            b_Qp, b_Kp, b_Qr, b_Kr = hb["b_Qp"], hb["b_Kp"], hb["b_Qr"], hb["b_Kr"]
            Va, Vb, b_Va, b_Vb, Tb, b_Tb = hb["Va"], hb["Vb"], hb["b_Va"], hb["b_Vb"], hb["Tb"], hb["b_Tb"]
            gnext = None
            tiles = [("ctx", qb) for qb in range(4)] + [("lat", r) for r in range(32)]
            NTL = len(tiles)
            tst = [None] * NTL

            def ph_a(t):
                kind, idx = tiles[t]
                i3 = t % 3
                S2 = ps2[i3]
                bS = [bbank[2 * i3], bbank[2 * i3 + 1]]
                if kind == "lat":
                    r = idx
                    rs = min(max(r - 4, 0), 24)
                    dr0 = rs - r + 7
                    qt = CTX + r * 64
                    nk = 768
                    k.op("pe", lambda e: e.matmul(S2[:, 0:512], lhsT=ident16[:], rhs=Tb[:, dr0 * 64:dr0 * 64 + 512], start=True, stop=False),
                         R=[b_Tb, b_cst], W=[bS[0]])
                    for hh in range(2):
                        hs = slice(hh * 64, (hh + 1) * 64)
                        k.op("pe", lambda e: e.matmul(S2[hs, 0:512], lhsT=Qr[hs, r * 64:(r + 1) * 64], rhs=Kr[hs, rs * 64:rs * 64 + 512],
                                                      start=False, stop=True), R=[b_Qr, b_Kr], W=[bS[0]])
                    for hh in range(2):
                        hs = slice(hh * 64, (hh + 1) * 64)
                        k.op("pe", lambda e: e.matmul(S2[hs, 512:768], lhsT=Qp[hs, qt:qt + 64], rhs=Kp[hs, 0:CTX], start=True, stop=True),
                             R=[b_Qp, b_Kp], W=[bS[1]])
                    vts = []
                    for b in range(4):
                        if rs % 2 == 0:
                            vts.append((Va, 2 + rs // 2 + b, b_Va))
                        else:
                            vts.append((Vb, (3 + rs) // 2 + b, b_Vb))
                    vts += [(Va, 0, b_Va), (Va, 1, b_Va)]
                else:
                    qt = idx * 64
                    nk = 256
                    for hh in range(2):
                        hs = slice(hh * 64, (hh + 1) * 64)
                        k.op("pe", lambda e: e.matmul(S2[hs, 0:256], lhsT=Qp[hs, qt:qt + 64], rhs=Kp[hs, 0:CTX], start=True, stop=True),
                             R=[b_Qp, b_Kp], W=[bS[0]])
                    vts = [(Va, 0, b_Va), (Va, 1, b_Va)]
                tst[t] = (S2, bS, qt, nk, vts)

            def ph_b(t):
                S2, bS, qt, nk, vts = tst[t]
                i = t % 3
                bSr = bS if nk > 512 else bS[0:1]
                st_ = stt_[i]
                k.op("dve", lambda e: e.tensor_reduce(out=st_[:, 0:1], in_=S2[:, 0:nk], axis=AX.X, op=ALU.max), R=bSr, W=[b_st[i]])
                k.op("dve", lambda e: e.tensor_scalar(out=st_[:, 1:2], in0=st_[:, 0:1], scalar1=-1.0, scalar2=None, op0=ALU.mult), R=[b_st[i]], W=[b_st[i]])
                k.op("act", lambda e: e.activation(out=Pt_[i][:, 0:nk], in_=S2[:, 0:nk], func=AF.Exp, bias=st_[:, 1:2], scale=1.0, accum_out=st_[:, 2:3]),
                     R=bSr + [b_st[i]], W=[b_P[i], b_st[i]])
                k.op("dve", lambda e: e.reciprocal(out=st_[:, 3:4], in_=st_[:, 2:3]), R=[b_st[i]], W=[b_st[i]])
                k.op("dve", lambda e: e.tensor_scalar(out=Dg[i][:], in0=ident32, scalar1=st_[:, 3:4], scalar2=None, op0=ALU.mult), R=[b_st[i], b_cst], W=[b_Dg[i]])

            def ph_c(t):
                S2, bS, qt, nk, vts = tst[t]
                i = t % 3
                nb = nk // 128
                PT2 = ps2[3]
                for b in range(nb):
                    k.op("pe", lambda e: e.matmul(PT2[:, b * 128:(b + 1) * 128], lhsT=Pt_[i][:, b * 128:(b + 1) * 128], rhs=Dg[i][:], start=True, stop=True),
                         R=[b_P[i], b_Dg[i]], W=[bbank[6 + b // 4]])
                bPT = [bbank[6], bbank[7]] if nb > 4 else [bbank[6]]
                if t % 2 == 0:
                    k.op("act", lambda e: e.activation(out=PTs[i][:, 0:nk], in_=PT2[:, 0:nk], func=AF.Copy), R=bPT, W=[b_PTs[i]])
                else:
                    k.op("dve", lambda e: e.tensor_copy(out=PTs[i][:, 0:nk], in_=PT2[:, 0:nk]), R=bPT, W=[b_PTs[i]])

            def ph_d(t):
                S2, bS, qt, nk, vts = tst[t]
                i = t % 3
                nb = nk // 128
                for hh in range(2):
                    hs = slice(hh * 64, (hh + 1) * 64)
                    for b in range(nb):
                        Vt, vj, bV = vts[b]
                        k.op("pe", lambda e: e.matmul(S2[hs, 768:832], lhsT=Vt[:, vj, hs], rhs=PTs[i][:, b * 128 + hh * 64:b * 128 + hh * 64 + 64],
                                                      start=(b == 0), stop=(b == nb - 1)), R=[bV, b_PTs[i]], W=[bS[1]])
                k.op("act", lambda e: e.activation(out=MIX[:, 4 + hp, qt:qt + 64], in_=S2[:, 768:832], func=AF.Copy), R=[bS[1]], W=[b_mix[4 + hp]])

            for t in range(NTL + 2):
                if t < NTL:
                    ph_a(t)
                if gnext is not None:
                    next(gnext, None)
                if 1 <= t <= NTL:
                    ph_b(t - 1)
                    ph_c(t - 1)
                if t >= 2:
                    ph_d(t - 2)
            if gnext is not None:
                for _ in gnext:
                    pass
        SB.close()
        dbg_dump("mixB", MIX[:, 4:8, :].rearrange("p c t -> p (c t)"), b_mix[4:8], [128, 4 * T], dt=BF16)
        ck("B")

        SO = Scope()
        Wo = SO.sb("Wo", [128, 8, D], BF16)
        b_Wo = Buf()
        for hf in range(2):
            k.dma("pool", Wo[:, hf * 4:(hf + 1) * 4, :], w_out[l, hf * 512:(hf + 1) * 512, :].rearrange("(ch p) c -> p ch c", p=128), W=[b_Wo])
        gbc = SO.sb("gbc", [128, 2, D])
        lngb = SO.sb("lngb", [128, 2, D])
        b_bc = Buf()
        for j in range(2):
            k.dma("sp", gbc[:, j, :], bass.AP(tensor=modd.tensor, offset=(l * 2 + j) * 6 * D + 2 * D, ap=[[0, 128], [1, D]]), R=[b_modd_all[l]], W=[b_bc])
        k.dma("sp", lngb[:, 0, :], bass.AP(tensor=ln1_g.tensor, offset=l * D, ap=[[0, 128], [1, D]]), W=[b_bc])
        k.dma("sp", lngb[:, 1, :], bass.AP(tensor=ln1_b.tensor, offset=l * D, ap=[[0, 128], [1, D]]), W=[b_bc])
        xr = [SO.sb(f"xr{i}", [128, D]) for i in range(3)]
        zt = [SO.sb(f"zt{i}", [128, D]) for i in range(3)]
        x1t = [SO.sb(f"x1t{i}", [128, D]) for i in range(3)]
        tf32 = [SO.sb(f"tf32{i}", [128, 8, 128]) for i in range(3)]
        b_xr, b_zt, b_x1t, b_tf = [Buf(), Buf(), Buf()], [Buf(), Buf(), Buf()], [Buf(), Buf(), Buf()], [Buf(), Buf(), Buf()]
        lnst = [SO.sb(f"lnst{i}", [128, 2, 6]) for i in range(3)]
        lnmv = [SO.sb(f"lnmv{i}", [128, 4]) for i in range(3)]
        b_ln = [Buf(), Buf(), Buf()]
        rt = [SO.sb(f"rt{i}", [128, 8, 16]) for i in range(3)]
        rcmp = [SO.sb(f"rcmp{i}", [128, 4, 4, 4]) for i in range(3)]
        b_rt = [Buf(), Buf(), Buf()]
        cTs = [SO.sb(f"cTs{i}", [16, 128]) for i in range(3)]
        b_cTs = [Buf(), Buf(), Buf()]
        pY = Rot([2, 4])

        def layer_norm_tile(st, mv, bln, z, bz, xo, bxo, g_ap, b_ap, bgb):
            for hf in range(2):
                k.op("dve", lambda e: e.bn_stats(out=st[:, hf, :], in_=z[:, hf * 512:(hf + 1) * 512]), R=[bz], W=[bln])
            k.op("dve", lambda e: e.bn_aggr(out=mv[:, 0:2], in_=st[:].rearrange("p a b -> p (a b)")), R=[bln], W=[bln])
            k.op("act", lambda e: e.activation(out=mv[:, 2:3], in_=mv[:, 1:2], func=AF.Ln, bias=LN_EPS, scale=1.0), R=[bln], W=[bln])
            k.op("act", lambda e: e.activation(out=mv[:, 2:3], in_=mv[:, 2:3], func=AF.Exp, scale=-0.5), R=[bln], W=[bln])
            k.op("dve", lambda e: e.tensor_scalar(out=mv[:, 3:4], in0=mv[:, 0:1], scalar1=mv[:, 2:3], scalar2=-1.0, op0=ALU.mult, op1=ALU.mult),
                 R=[bln], W=[bln])
            k.op("act", lambda e: e.activation(out=xo[:], in_=z[:], func=AF.Identity, scale=mv[:, 2:3], bias=mv[:, 3:4]), R=[bz, bln], W=[bxo])
            k.op("dve", lambda e: e.tensor_tensor(out=xo[:], in0=xo[:], in1=g_ap, op=ALU.mult), R=[bxo, bgb], W=[bxo])
            k.op("pool", lambda e: e.tensor_tensor(out=xo[:], in0=xo[:], in1=b_ap, op=ALU.add), R=[bxo, bgb], W=[bxo])

        tt0 = 2 if last else 0

        def load_res(t_):
            i_ = t_ % 3
            if l == 0:
                src = ctx_in[t_ * 128:(t_ + 1) * 128, :] if t_ < 2 else x_in[(t_ - 2) * 128:(t_ - 1) * 128, :]
                k.dma("sp", xr[i_][:], src, W=[b_xr[i_]])
            else:
                k.dma("sp", xr[i_][:], xs[t_ * 128:(t_ + 1) * 128, :], R=[b_xs[t_]], W=[b_xr[i_]])

        def stageO_p1(tt):
            i = tt % 3
            j = 1 if tt < 2 else 0
            pb0 = pY.next()
            if tt == tt0:
                load_res(tt)
            if tt + 1 < NT:
                load_res(tt + 1)
            for hf in range(2):
                pb = pb0 + hf
                for ch in range(8):
                    k.op("pe", lambda e: e.matmul(bank(pb)[:, :], lhsT=MIX[:, ch, tt * 128:(tt + 1) * 128], rhs=Wo[:, ch, hf * 512:(hf + 1) * 512],
                                                  start=(ch == 0), stop=(ch == 7)), R=[b_mix[ch], b_Wo], W=[bbank[pb]])
                k.op("dve", lambda e: e.tensor_tensor(out=zt[i][:, hf * 512:(hf + 1) * 512], in0=bank(pb)[:, :], in1=gbc[:, j, hf * 512:(hf + 1) * 512], op=ALU.mult),
                     R=[bbank[pb], b_bc], W=[b_zt[i]])
            k.op("dve", lambda e: e.scalar_tensor_tensor(out=zt[i][:], in0=xr[i][:], scalar=ALPHA, in1=zt[i][:], op0=ALU.mult, op1=ALU.add),
                 R=[b_xr[i], b_zt[i]], W=[b_zt[i]])
            yield
            layer_norm_tile(lnst[i], lnmv[i], b_ln[i], zt[i], b_zt[i], x1t[i], b_x1t[i], lngb[:, 0, :], lngb[:, 1, :], b_bc)
            k.dma("sp", xs[tt * 128:(tt + 1) * 128, :], x1t[i][:], R=[b_x1t[i]], W=[b_xs[tt]])

        def stageO_p2(tt):
            i = tt % 3
            j = 1 if tt < 2 else 0
            transpose_tile(x1t[i], b_x1t[i], tt, l, 4, 3, tf32=tf32[i], b_tf=b_tf[i])
            yield
            for ch in range(8):
                k.op("pe", lambda e: e.matmul(bank(6)[:, 0:16], lhsT=tf32[i][:, ch, :], rhs=wr32[:, ch, :], start=(ch == 0), stop=(ch == 7)),
                     R=[b_tf[i], b_wr], W=[bbank[6]])
            yield
            R_ = rt[i]
            e1, aff, sel, m2, gs4, gm4, asel = R_[:, 0, :], R_[:, 1, :], R_[:, 2, :], R_[:, 3, :], R_[:, 4, 0:4], R_[:, 4, 4:8], R_[:, 5, :]
            gmx, den = R_[:, 4, 8:9], R_[:, 4, 9:10]
            k.op("act", lambda e: e.activation(out=e1, in_=bank(6)[:, 0:16], func=AF.Exp, scale=-1.0), R=[bbank[6]], W=[b_rt[i]])
            k.op("dve", lambda e: e.tensor_scalar(out=e1, in0=e1, scalar1=1.0, scalar2=None, op0=ALU.add), R=[b_rt[i]], W=[b_rt[i]])
            k.op("dve", lambda e: e.reciprocal(out=aff, in_=e1), R=[b_rt[i]], W=[b_rt[i]])
            k.op("dve", lambda e: e.tensor_tensor(out=sel, in0=aff, in1=brb[:], op=ALU.add), R=[b_rt[i], b_brb], W=[b_rt[i]])
            selv = sel.rearrange("p (g i) -> p g i", g=4)
            k.op("dve", lambda e: e.tensor_tensor(out=rcmp[i][:], in0=selv.unsqueeze(2).broadcast_to([128, 4, 4, 4]),
                                                  in1=selv.unsqueeze(3).broadcast_to([128, 4, 4, 4]), op=ALU.is_gt), R=[b_rt[i]], W=[b_rt[i]])
            m2v = m2.rearrange("p (g i) -> p g i", g=4)
            k.op("dve", lambda e: e.tensor_reduce(out=m2v, in_=rcmp[i][:], axis=AX.X, op=ALU.add), R=[b_rt[i]], W=[b_rt[i]])
            k.op("dve", lambda e: e.tensor_scalar(out=m2, in0=m2, scalar1=2.0, scalar2=None, op0=ALU.is_lt), R=[b_rt[i]], W=[b_rt[i]])
            k.op("dve", lambda e: e.tensor_tensor(out=asel, in0=m2, in1=sel, op=ALU.mult), R=[b_rt[i]], W=[b_rt[i]])
            k.op("dve", lambda e: e.tensor_reduce(out=gs4, in_=asel.rearrange("p (g i) -> p g i", g=4), axis=AX.X, op=ALU.add), R=[b_rt[i]], W=[b_rt[i]])
            k.op("dve", lambda e: e.tensor_reduce(out=gmx, in_=gs4, axis=AX.X, op=ALU.max), R=[b_rt[i]], W=[b_rt[i]])
            k.op("dve", lambda e: e.tensor_scalar(out=gm4, in0=gs4, scalar1=gmx, scalar2=None, op0=ALU.is_ge), R=[b_rt[i]], W=[b_rt[i]])
            k.op("dve", lambda e: e.tensor_tensor(out=m2v, in0=m2v, in1=gm4.unsqueeze(2).broadcast_to([128, 4, 4]), op=ALU.mult), R=[b_rt[i]], W=[b_rt[i]])
            k.op("dve", lambda e: e.tensor_tensor(out=asel, in0=m2, in1=aff, op=ALU.mult), R=[b_rt[i]], W=[b_rt[i]])
            k.op("dve", lambda e: e.tensor_reduce(out=den, in_=asel, axis=AX.X, op=ALU.add), R=[b_rt[i]], W=[b_rt[i]])
            k.op("dve", lambda e: e.reciprocal(out=den, in_=den), R=[b_rt[i]], W=[b_rt[i]])
            k.op("dve", lambda e: e.tensor_scalar(out=asel, in0=asel, scalar1=den, scalar2=None, op0=ALU.mult), R=[b_rt[i]], W=[b_rt[i]])
            yield
            k.op("pe", lambda e: e.transpose(out=bank(7)[0:16, 0:128], in_=asel, identity=ident32), R=[b_rt[i], b_cst], W=[bbank[7]])
            k.op("act", lambda e: e.activation(out=cTs[i][:], in_=bank(7)[0:16, 0:128], func=AF.Copy), R=[bbank[7]], W=[b_cTs[i]])
            k.dma("sp", combT_d[:, tt * 128:(tt + 1) * 128], cTs[i][:], R=[b_cTs[i]], W=[b_combT])
            if tt == 3:
                dbg_dump("x1t3", x1t[i][:], b_x1t[i], [128, D])
                dbg_dump("comb3", asel, b_rt[i], [128, 16])
        gens2 = {}
        for tt in range(tt0, NT + 2):
            g1 = stageO_p1(tt) if tt < NT else None
            if tt0 <= tt - 1 < NT:
                gens2[tt - 1] = stageO_p2(tt - 1)
            g2 = gens2.get(tt - 1) if tt - 1 < NT else None
            g3 = gens2.get(tt - 2)
            if g2:
                next(g2)
            if g1:
                next(g1)
            if g2:
                next(g2)
            if g3:
                next(g3, None)
            if g1:
                next(g1, None)
            if g2:
                next(g2)
        SO.close()
        dbg_dump("xt2", XT[:, :, :].rearrange("p c t -> p (c t)"), bXT, [128, 8 * T], dt=BF16)
        ck("O")
        MS.close()

        SML = Scope()
        ACC = SML.sb("ACC", [128, NT, D])
        b_acc = [Buf(f"acc{i}") for i in range(NT)]
        SM = Scope()
        CT = SM.sb("CT", [16, T])
        b_CT = Buf()
        k.dma("sp", CT[:], combT_d[:, :], R=[b_combT], W=[b_CT])
        CB = SM.sb("CB", [128, T])
        b_CB = Buf()
        wg = [SM.sb(f"wg{i}", [128, 8, 128], BF16) for i in range(2)]
        wu = [SM.sb(f"wu{i}", [128, 8, 128], BF16) for i in range(2)]
        b_wg, b_wu = [Buf(), Buf()], [Buf(), Buf()]
        wd = SM.sb("wd", [128, 4, D], BF16)
        b_wd = Buf()
        HID = SM.sb("HID", [128, 4, T], BF16)
        b_hid = [Buf() for _ in range(4)]
        sgt = [SM.sb(f"sgt{i}", [128, 512]) for i in range(2)]
        tmt = [SM.sb(f"tmt{i}", [128, 512]) for i in range(2)]
        b_sg, b_tm = [Buf(), Buf()], [Buf(), Buf()]
        pM = Rot([0, 1, 2, 3, 4, 5, 6, 7])
        wi = 0
        ti = 0
        NEX_ = int(os.environ.get("NEXPS", NEXP))
        MBLKS = [(256, 512), (768, 512), (1280, 512), (1792, 512)] if last else BLKS
        mt0 = 2 if last else 0

        def load_gu(ex_, fc_, wi_):
            k.dma("pool", wg[wi_][:], w_gate[l, ex_, :, fc_ * 128:(fc_ + 1) * 128].rearrange("(ch p) c -> p ch c", p=128), W=[b_wg[wi_]])
            k.dma("pool", wu[wi_][:], w_up[l, ex_, :, fc_ * 128:(fc_ + 1) * 128].rearrange("(ch p) c -> p ch c", p=128), W=[b_wu[wi_]])

        for ex in range(NEX_):
            for bi, (t0, n) in enumerate(MBLKS):
                pb = pM.next()
                k.op("pe", lambda e: e.matmul(bank(pb)[:, 0:n], lhsT=ident32[0:16, ex:ex + 1].broadcast_to([16, 128]), rhs=CT[:, t0:t0 + n], start=True, stop=True),
                     R=[b_cst, b_CT], W=[bbank[pb]])
                k.op("act", lambda e: e.activation(out=CB[:, t0:t0 + n], in_=bank(pb)[:, 0:n], func=AF.Copy), R=[bbank[pb]], W=[b_CB])
            for fc in range(4):
                w_i = wi % 2
                wi += 1
                if ex == 0 and fc == 0:
                    load_gu(ex, fc, w_i)
                nfc, nex = (fc + 1, ex) if fc < 3 else (0, ex + 1)
                if nex < NEX_:
                    load_gu(nex, nfc, 1 - w_i)
                if fc == 1:
                    k.dma("pool", wd[:], w_down[l, ex, :, :].rearrange("(fc p) c -> p fc c", p=128), W=[b_wd])
                for bi, (t0, n) in enumerate(MBLKS):
                    i = ti % 2
                    ti += 1
                    pg = pM.next()
                    for ch in range(8):
                        k.op("pe", lambda e: e.matmul(bank(pg)[:, 0:n], lhsT=wg[w_i][:, ch, :], rhs=XT[:, ch, t0:t0 + n], start=(ch == 0), stop=(ch == 7)),
                             R=[b_wg[w_i]] + xt_bufs(t0, n), W=[bbank[pg]])
                    pu = pM.next()
                    for ch in range(8):
                        k.op("pe", lambda e: e.matmul(bank(pu)[:, 0:n], lhsT=wu[w_i][:, ch, :], rhs=XT[:, ch, t0:t0 + n], start=(ch == 0), stop=(ch == 7)),
                             R=[b_wu[w_i]] + xt_bufs(t0, n), W=[bbank[pu]])
                    k.op("act", lambda e: e.activation(out=sgt[i][:, 0:n], in_=bank(pg)[:, 0:n], func=AF.Silu), R=[bbank[pg]], W=[b_sg[i]])
                    k.op("dve", lambda e: e.tensor_tensor(out=tmt[i][:, 0:n], in0=bank(pu)[:, 0:n], in1=CB[:, t0:t0 + n], op=ALU.mult),
                         R=[bbank[pu], b_CB], W=[b_tm[i]])
                    k.op("pool", lambda e: e.tensor_tensor(out=HID[:, fc, t0:t0 + n], in0=sgt[i][:, 0:n], in1=tmt[i][:, 0:n], op=ALU.mult),
                         R=[b_sg[i], b_tm[i]], W=[b_hid[fc]])
            for tt in range(mt0, NT):
                for hf in range(2):
                    pb = pM.next()
                    for fc in range(4):
                        k.op("pe", lambda e: e.matmul(bank(pb)[:, :], lhsT=HID[:, fc, tt * 128:(tt + 1) * 128], rhs=wd[:, fc, hf * 512:(hf + 1) * 512],
                                                      start=(fc == 0), stop=(fc == 3)), R=[b_hid[fc], b_wd], W=[bbank[pb]])
                    dst = ACC[:, tt, hf * 512:(hf + 1) * 512]
                    if ex == 0:
                        k.op("act", lambda e: e.activation(out=dst, in_=bank(pb)[:, :], func=AF.Copy), R=[bbank[pb]], W=[b_acc[tt]])
                    else:
                        k.op("dve", lambda e: e.tensor_tensor(out=dst, in0=bank(pb)[:, :], in1=dst, op=ALU.add), R=[bbank[pb], b_acc[tt]], W=[b_acc[tt]])
        SM.close()
        dbg_dump("acc3", ACC[:, 3, :], b_acc[3], [128, D])
        ck("M")

        SO = Scope()
        gbc = SO.sb("gbc2", [128, 2, D])
        lngb = SO.sb("lngb2", [128, 2, D])
        b_bc = Buf()
        for j in range(2):
            k.dma("sp", gbc[:, j, :], bass.AP(tensor=modd.tensor, offset=(l * 2 + j) * 6 * D + 5 * D, ap=[[0, 128], [1, D]]), R=[b_modd_all[l]], W=[b_bc])
        k.dma("sp", lngb[:, 0, :], bass.AP(tensor=ln2_g.tensor, offset=l * D, ap=[[0, 128], [1, D]]), W=[b_bc])
        k.dma("sp", lngb[:, 1, :], bass.AP(tensor=ln2_b.tensor, offset=l * D, ap=[[0, 128], [1, D]]), W=[b_bc])
        xr = [SO.sb(f"xr2{i}", [128, D]) for i in range(3)]
        zt = [SO.sb(f"zt2{i}", [128, D]) for i in range(3)]
        x1t = [SO.sb(f"x2t{i}", [128, D]) for i in range(3)]
        b_xr, b_zt, b_x1t = [Buf(), Buf(), Buf()], [Buf(), Buf(), Buf()], [Buf(), Buf(), Buf()]
        lnst = [SO.sb(f"lnst2{i}", [128, 2, 6]) for i in range(3)]
        lnmv = [SO.sb(f"lnmv2{i}", [128, 4]) for i in range(3)]
        b_ln = [Buf(), Buf(), Buf()]
        pendT = [None]
        lt0 = 2 if last else 0
        k.dma("sp", xr[lt0 % 3][:], xs[lt0 * 128:(lt0 + 1) * 128, :], R=[b_xs[lt0]], W=[b_xr[lt0 % 3]])
        for tt in range(lt0, NT):
            i = tt % 3
            j = 1 if tt < 2 else 0
            if tt + 1 < NT:
                k.dma("sp", xr[(tt + 1) % 3][:], xs[(tt + 1) * 128:(tt + 2) * 128, :], R=[b_xs[tt + 1]], W=[b_xr[(tt + 1) % 3]])
            k.op("dve", lambda e: e.tensor_tensor(out=zt[i][:], in0=ACC[:, tt, :], in1=gbc[:, j, :], op=ALU.mult), R=[b_acc[tt], b_bc], W=[b_zt[i]])
            k.op("dve", lambda e: e.scalar_tensor_tensor(out=zt[i][:], in0=xr[i][:], scalar=ALPHA, in1=zt[i][:], op0=ALU.mult, op1=ALU.add),
                 R=[b_xr[i], b_zt[i]], W=[b_zt[i]])
            layer_norm_tile(lnst[i], lnmv[i], b_ln[i], zt[i], b_zt[i], x1t[i], b_x1t[i], lngb[:, 0, :], lngb[:, 1, :], b_bc)
            if last:
                bo = Buf()
                k.dma("sp", out[(tt - 2) * 128:(tt - 1) * 128, :], x1t[i][:], R=[b_x1t[i]], W=[bo])
            else:
                k.dma("sp", xs[tt * 128:(tt + 1) * 128, :], x1t[i][:], R=[b_x1t[i]], W=[b_xs[tt]])
                if l + 1 < nlayers:
                    if pendT[0] is not None:
                        pt_, pi_ = pendT[0]
                        transpose_tile(x1t[pi_], b_x1t[pi_], pt_, l + 1, 1, 0)
                    pendT[0] = (tt, i)
            if tt == 3:
                dbg_dump("x2t3_%d" % l, x1t[i][:], b_x1t[i], [128, D])
        if pendT[0] is not None:
            pt_, pi_ = pendT[0]
            transpose_tile(x1t[pi_], b_x1t[pi_], pt_, l + 1, 1, 0)
        SO.close()
        SML.close()
        ck("L%d" % l)


def make_in_map(inp, b, cst, rope):
    f = lambda a: np.ascontiguousarray(np.asarray(a, dtype=np.float32))
    m = {
        "x": f(inp["x"][b]), "ctx": f(inp["ctx"][b]), "c": f(inp["c"][b]).reshape(1, D),
        "c_ctx": f(inp["c_ctx"]).reshape(1, D), "b_router": f(inp["b_router"]).reshape(1, NEXP),
        "cst": cst, "rope": rope,
    }
    for n in ("w_ada", "b_ada", "w_in", "lb_logits", "a_norm_g", "rpb", "w_out", "ln1_g", "ln1_b", "w_router",
              "w_gate", "w_up", "w_down", "ln2_g", "ln2_b"):
        m[n] = f(inp[n])
    return m


_NC = None


def kernel(**inputs):
    global _NC
    if _NC is None:
        _NC = build()[0]
    nc = _NC
    cst, rope = host_consts()
    nb = np.asarray(inputs["x"]).shape[0]
    shared = make_in_map(inputs, 0, cst, rope)
    in_maps = []
    for b in range(nb):
        m = dict(shared)
        m["x"] = np.ascontiguousarray(np.asarray(inputs["x"][b], dtype=np.float32))
        m["ctx"] = np.ascontiguousarray(np.asarray(inputs["ctx"][b], dtype=np.float32))
        m["c"] = np.ascontiguousarray(np.asarray(inputs["c"][b], dtype=np.float32)).reshape(1, D)
        in_maps.append(m)
    res = run_bass_kernel_spmd(nc, in_maps, core_ids=list(range(nb)))
    return np.stack([np.asarray(r["out"], dtype=np.float32) for r in res.results], axis=0)
```
